# Optimizing a Trainium2 kernel written in Bass

```python
import jax, jax.numpy as jnp
from jax import lax
import numpy as np

D_MODEL = 2048
BATCH = 2
SEQ = 8192
DEPTH = 2

N_MIXERS = 2
N_A_LAYERS = (DEPTH + 1) // 2
N_B_LAYERS = DEPTH // 2
ROPE_THETA = 10000.0
EPS = 1e-6

MLA_HEADS = 16
MLA_Q_RANK = 512
MLA_KV_RANK = 512
MLA_NOPE = 128
MLA_ROPE = 64
MLA_QK = MLA_NOPE + MLA_ROPE
MLA_V = 128
MLA_IN = MLA_Q_RANK + MLA_KV_RANK + MLA_ROPE
Q_BLOCK = 128

DIL_GROUPS = ((128, 1), (512, 4), (2048, 16))
N_DIL_GROUPS = len(DIL_GROUPS)
DIL_HEADS = 16
DIL_HEAD_DIM = 128
DIL_GROUP_W = DIL_HEADS * DIL_HEAD_DIM
DIL_IN = N_DIL_GROUPS * 3 * DIL_GROUP_W

PEER_HEADS = 8
PEER_NKEYS = 128
PEER_EXPERTS = PEER_NKEYS * PEER_NKEYS
PEER_QDIM = 256
PEER_HALF = PEER_QDIM // 2
PEER_TOPK = 16
PEER_CHUNK = 128

kernel_name = 'hybrid_mla_dilated_peer'


def rms_norm(x, gain):
    xf = x.astype(jnp.float32)
    y = xf * lax.rsqrt(jnp.mean(xf * xf, axis=-1, keepdims=True) + EPS)
    return (y * gain.astype(jnp.float32)).astype(x.dtype)


def rope(x, positions):
    half = x.shape[-1] // 2
    inv_freq = ROPE_THETA ** (-jnp.arange(half, dtype=jnp.float32) / half)
    ang = positions.astype(jnp.float32)[..., None] * inv_freq
    ang = ang.reshape(ang.shape[:2] + (1,) * (x.ndim - 3) + (half,))
    cos, sin = jnp.cos(ang), jnp.sin(ang)
    xf = x.astype(jnp.float32)
    x1, x2 = xf[..., :half], xf[..., half:]
    return jnp.concatenate([x1 * cos - x2 * sin, x2 * cos + x1 * sin], axis=-1).astype(x.dtype)


def causal_block_attention(q, k, v, scale):
    B, S, H, Dq = q.shape
    nb = S // Q_BLOCK
    qb = q.reshape(B, nb, Q_BLOCK, H, Dq).transpose(1, 0, 2, 3, 4)
    k_pos = jnp.arange(S)

    def one_block(args):
        qi, bi = args
        s = jnp.einsum('bqhd,bkhd->bhqk', qi, k, preferred_element_type=jnp.float32) * scale
        q_pos = bi * Q_BLOCK + jnp.arange(Q_BLOCK)
        s = jnp.where(k_pos[None, :] <= q_pos[:, None], s, -jnp.inf)
        p = jax.nn.softmax(s, axis=-1)
        return jnp.einsum('bhqk,bkhd->bqhd', p.astype(v.dtype), v)

    o = lax.map(one_block, (qb, jnp.arange(nb)))
    return o.transpose(1, 0, 2, 3, 4).reshape(B, S, H, v.shape[-1])


def mla_mixer(h, positions, w_in, g_q, w_uq, g_kv, w_ukv, g_qn, g_kn, w_o):
    B, S, _ = h.shape
    z = h @ w_in
    c_q, c_kv, k_r = jnp.split(z, [MLA_Q_RANK, MLA_Q_RANK + MLA_KV_RANK], axis=-1)
    q = (rms_norm(c_q, g_q) @ w_uq).reshape(B, S, MLA_HEADS, MLA_QK)
    kv = (rms_norm(c_kv, g_kv) @ w_ukv).reshape(B, S, MLA_HEADS, MLA_NOPE + MLA_V)
    k_nope, v = kv[..., :MLA_NOPE], kv[..., MLA_NOPE:]
    k_rope = jnp.broadcast_to(k_r[:, :, None, :], (B, S, MLA_HEADS, MLA_ROPE))
    k = jnp.concatenate([k_nope, k_rope], axis=-1)
    q = rms_norm(q, g_qn)
    k = rms_norm(k, g_kn)
    q = jnp.concatenate([q[..., :MLA_NOPE], rope(q[..., MLA_NOPE:], positions)], axis=-1)
    k = jnp.concatenate([k[..., :MLA_NOPE], rope(k[..., MLA_NOPE:], positions)], axis=-1)
    o = causal_block_attention(q, k, v, MLA_QK ** -0.5)
    return o.reshape(B, S, MLA_HEADS * MLA_V) @ w_o


def dilated_group_attention(q, k, v, window, dilation):
    B, S, H, dh = q.shape
    steps = window // dilation
    L = S // dilation
    nb = -(-L // steps)
    Lp = nb * steps

    def to_sub(t):
        t = t.reshape(B, L, dilation, H, dh).transpose(0, 2, 3, 1, 4)
        t = jnp.pad(t, ((0, 0), (0, 0), (0, 0), (0, Lp - L), (0, 0)))
        return t.reshape(B, dilation, H, nb, steps, dh)

    def with_prev(t):
        prev = jnp.pad(t, ((0, 0), (0, 0), (0, 0), (1, 0), (0, 0), (0, 0)))[:, :, :, :-1]
        return jnp.concatenate([prev, t], axis=4)

    qs, ks, vs = to_sub(q), to_sub(k), to_sub(v)
    kb, vb = with_prev(ks), with_prev(vs)
    s = jnp.einsum('brhnqd,brhnkd->brhnqk', qs, kb, preferred_element_type=jnp.float32) * dh ** -0.5
    qi = jnp.arange(steps)[:, None]
    kj = jnp.arange(2 * steps)[None, :]
    dist = qi + steps - kj
    blk = jnp.arange(nb)[:, None, None]
    valid = (dist >= 0) & (dist <= steps) & (blk * steps + kj - steps >= 0)
    s = jnp.where(valid, s, -jnp.inf)
    lse = jax.nn.logsumexp(s, axis=-1)
    p = jnp.exp(s - lse[..., None])
    o = jnp.einsum('brhnqk,brhnkd->brhnqd', p.astype(v.dtype), vb)
    o = o.reshape(B, dilation, H, Lp, dh)[:, :, :, :L].transpose(0, 3, 1, 2, 4).reshape(B, S, H, dh)
    lse = lse.reshape(B, dilation, H, Lp)[:, :, :, :L].transpose(0, 3, 1, 2).reshape(B, S, H)
    return o, lse


def dilated_mixer(h, positions, w_in, g_qn, g_kn, w_o):
    B, S, _ = h.shape
    z = (h @ w_in).reshape(B, S, N_DIL_GROUPS, 3, DIL_HEADS, DIL_HEAD_DIM)
    q_all = rope(rms_norm(z[:, :, :, 0], g_qn[:, None, :]), positions)
    k_all = rope(rms_norm(z[:, :, :, 1], g_kn[:, None, :]), positions)
    v_all = z[:, :, :, 2]
    outs, lses = [], []
    for gi, (window, dilation) in enumerate(DIL_GROUPS):
        o, lse = dilated_group_attention(q_all[:, :, gi], k_all[:, :, gi], v_all[:, :, gi], window, dilation)
        outs.append(o)
        lses.append(lse)
    w = jax.nn.softmax(jnp.stack(lses, axis=0), axis=0)
    o = jnp.einsum('gbsh,gbshd->bshd', w.astype(v_all.dtype), jnp.stack(outs, axis=0))
    return o.reshape(B, S, DIL_GROUP_W) @ w_o


def peer_ffn(h, w_q, sub_keys, u_tab, v_tab):
    B, S, D = h.shape
    T = B * S
    x = h.reshape(T, D)
    q = (x @ w_q).reshape(T, PEER_HEADS, 2, PEER_HALF)
    s = jnp.einsum('thpc,hpnc->thpn', q, sub_keys, preferred_element_type=jnp.float32)
    top_s, top_i = lax.top_k(s, PEER_TOPK)
    cand = top_s[:, :, 0, :, None] + top_s[:, :, 1, None, :]
    best_s, best_c = lax.top_k(cand.reshape(T, PEER_HEADS, PEER_TOPK * PEER_TOPK), PEER_TOPK)
    i1 = jnp.take_along_axis(top_i[:, :, 0], best_c // PEER_TOPK, axis=-1)
    i2 = jnp.take_along_axis(top_i[:, :, 1], best_c % PEER_TOPK, axis=-1)
    idx = (i1 * PEER_NKEYS + i2).reshape(T, PEER_HEADS * PEER_TOPK)
    gate = jax.nn.softmax(best_s, axis=-1).reshape(T, PEER_HEADS * PEER_TOPK)
    nc = T // PEER_CHUNK

    def chunk(args):
        xc, ic, gc = args
        u = jnp.take(u_tab, ic, axis=0)
        a = jnp.einsum('cd,ced->ce', xc, u, preferred_element_type=jnp.float32)
        act = (jax.nn.gelu(a, approximate=False) * gc).astype(xc.dtype)
        return jnp.einsum('ce,ced->cd', act, jnp.take(v_tab, ic, axis=0))

    y = lax.map(chunk, (x.reshape(nc, PEER_CHUNK, D), idx.reshape(nc, PEER_CHUNK, -1), gate.reshape(nc, PEER_CHUNK, -1)))
    return y.reshape(B, S, D)


def setup_inputs(seed: int = 0) -> dict:
    key = jax.random.key(seed)
    ks = jax.random.split(key, 24)
    f32 = jnp.float32
    D = D_MODEL

    def nrm(k, shape, scale):
        return jax.random.normal(k, shape, f32) * scale

    def gain(k, shape):
        return 1.0 + 0.05 * jax.random.normal(k, shape, f32)

    return {
        'x': nrm(ks[0], (BATCH, SEQ, D), 1.0),
        'c': nrm(ks[1], (BATCH, D), 1.0),
        'positions': jnp.arange(SEQ, dtype=jnp.int32)[None, :] + jax.random.randint(ks[2], (BATCH, 1), 0, 4096, dtype=jnp.int32),
        'ada_w': nrm(ks[3], (DEPTH, D, 6 * D), 0.5 * D ** -0.5),
        'ada_b': nrm(ks[4], (DEPTH, 6 * D), 0.01),
        'norm_g': gain(ks[5], (DEPTH, 2, D)),
        'mla_w_in': nrm(ks[6], (N_A_LAYERS, D, MLA_IN), D ** -0.5),
        'mla_g_q': gain(ks[7], (N_A_LAYERS, MLA_Q_RANK)),
        'mla_w_uq': nrm(ks[8], (N_A_LAYERS, MLA_Q_RANK, MLA_HEADS * MLA_QK), MLA_Q_RANK ** -0.5),
        'mla_g_kv': gain(ks[9], (N_A_LAYERS, MLA_KV_RANK)),
        'mla_w_ukv': nrm(ks[10], (N_A_LAYERS, MLA_KV_RANK, MLA_HEADS * (MLA_NOPE + MLA_V)), MLA_KV_RANK ** -0.5),
        'mla_g_qn': gain(ks[11], (N_A_LAYERS, MLA_QK)),
        'mla_g_kn': gain(ks[12], (N_A_LAYERS, MLA_QK)),
        'mla_w_o': nrm(ks[13], (N_A_LAYERS, MLA_HEADS * MLA_V, D), (MLA_HEADS * MLA_V) ** -0.5),
        'dil_w_in': nrm(ks[14], (N_B_LAYERS, D, DIL_IN), D ** -0.5),
        'dil_g_qn': gain(ks[15], (N_B_LAYERS, N_DIL_GROUPS, DIL_HEAD_DIM)),
        'dil_g_kn': gain(ks[16], (N_B_LAYERS, N_DIL_GROUPS, DIL_HEAD_DIM)),
        'dil_w_o': nrm(ks[17], (N_B_LAYERS, DIL_GROUP_W, D), DIL_GROUP_W ** -0.5),
        'peer_w_q': nrm(ks[18], (DEPTH, D, PEER_HEADS * PEER_QDIM), D ** -0.5),
        'peer_sub_keys': nrm(ks[19], (DEPTH, PEER_HEADS, 2, PEER_NKEYS, PEER_HALF), PEER_HALF ** -0.5),
        'peer_u': nrm(ks[20], (DEPTH, PEER_EXPERTS, D), D ** -0.5),
        'peer_v': nrm(ks[21], (DEPTH, PEER_EXPERTS, D), PEER_HEADS ** -0.5),
    }


def reference(x, c, positions, ada_w, ada_b, norm_g, mla_w_in, mla_g_q, mla_w_uq, mla_g_kv, mla_w_ukv, mla_g_qn, mla_g_kn, mla_w_o, dil_w_in, dil_g_qn, dil_g_kn, dil_w_o, peer_w_q, peer_sub_keys, peer_u, peer_v):
    mod = jnp.einsum('bd,lde->lbe', jax.nn.silu(c), ada_w) + ada_b[:, None, :]
    for layer in range(DEPTH):
        shift1, scale1, gate1, shift2, scale2, gate2 = jnp.split(mod[layer][:, None, :], 6, axis=-1)
        h = rms_norm(x, norm_g[layer, 0]) * (1.0 + scale1) + shift1
        if layer % N_MIXERS == 0:
            a = layer // N_MIXERS
            y = mla_mixer(h, positions, mla_w_in[a], mla_g_q[a], mla_w_uq[a], mla_g_kv[a], mla_w_ukv[a], mla_g_qn[a], mla_g_kn[a], mla_w_o[a])
        else:
            b = layer // N_MIXERS
            y = dilated_mixer(h, positions, dil_w_in[b], dil_g_qn[b], dil_g_kn[b], dil_w_o[b])
        x = x + gate1 * y
        h = rms_norm(x, norm_g[layer, 1]) * (1.0 + scale2) + shift2
        x = x + gate2 * peer_ffn(h, peer_w_q[layer], peer_sub_keys[layer], peer_u[layer], peer_v[layer])
    return x
```

```python
import numpy as np
import ml_dtypes
from contextlib import ExitStack
import concourse.bass as bass
import concourse.mybir as mybir
from concourse.bass_utils import run_bass_kernel_spmd

F32 = mybir.dt.float32
BF16 = mybir.dt.bfloat16
I32 = mybir.dt.int32
ALU = mybir.AluOpType
AF = mybir.ActivationFunctionType
AX = mybir.AxisListType
NPBF = ml_dtypes.bfloat16

D = 2048
NT = 2048
NTILE = 16
SEQ = 8192
EPS = 1e-6
TWO_PI = float(2 * np.pi)


class Eng:
    def __init__(self, S, name, handle):
        self.S = S
        self.name = name
        self.h = handle
        self.sem = S.es.enter_context(S.nc.semaphore("sem_" + name))
        self.count = 0
        self.seen = {}
        self.seen_dma = {}


class Buf:
    def __init__(self, S, name):
        self.S = S
        self.name = name
        self.w = None
        self.r = []
        self.dsem = None
        self.dval = 0
        self.excl = False

    def dma_sem(self):
        if self.dsem is None:
            self.dsem = self.S.es.enter_context(self.S.nc.semaphore("d_" + self.name))
        return self.dsem


class Sched:
    def __init__(self, nc, es):
        self.nc = nc
        self.es = es
        self.pe = Eng(self, "pe", nc.tensor)
        self.dve = Eng(self, "dve", nc.vector)
        self.act = Eng(self, "act", nc.scalar)
        self.pool = Eng(self, "pool", nc.gpsimd)
        self.sp = Eng(self, "sp", nc.sync)
        self.nbuf = 0

    def buf(self, name=None):
        self.nbuf += 1
        return Buf(self, name or ("b%d" % self.nbuf))

    def bufs(self, n):
        return [self.buf() for _ in range(n)]

    def _wait(self, X, ev):
        if ev is None:
            return
        if ev[0] == 'e':
            _, E, n = ev
            if X.seen.get(E.name, 0) >= n:
                return
            X.h.wait_ge(E.sem, n)
            X.seen[E.name] = n
        else:
            _, sem, val, key = ev
            if X.seen_dma.get(key, 0) >= val:
                return
            X.h.wait_ge(sem, val)
            X.seen_dma[key] = val

    def _deps(self, X, reads, writes):
        for b in reads:
            self._wait(X, b.w)
            if b.excl:
                for ev in b.r:
                    if not (ev[0] == 'e' and ev[1] is X):
                        self._wait(X, ev)
        for b in writes:
            self._wait(X, b.w)
            for ev in b.r:
                self._wait(X, ev)

    def op(self, X, fn, reads=(), writes=()):
        self._deps(X, reads, writes)
        ins = fn()
        X.count += 1
        ins.then_inc(X.sem, 1)
        ev = ('e', X, X.count)
        for b in reads:
            b.r.append(ev)
            if len(b.r) > 24:
                b.r = self._compact(b.r)
        for b in writes:
            b.w = ev
            b.r = []
        return ins

    @staticmethod
    def _compact(evs):
        last = {}
        for ev in evs:
            k = ('e', ev[1].name) if ev[0] == 'e' else ('d', ev[3])
            if k not in last or ev[2] > last[k][2]:
                last[k] = ev
        return list(last.values())

    def dma(self, Q, out, in_, reads=(), writes=(), **kw):
        self._deps(Q, reads, writes)
        owner = writes[0] if writes else reads[0]
        sem = owner.dma_sem()
        owner.dval += 16
        Q.h.dma_start(out=out, in_=in_, **kw).then_inc(sem, 16)
        ev = ('d', sem, owner.dval, owner.name)
        for b in reads:
            b.r.append(ev)
            if len(b.r) > 24:
                b.r = self._compact(b.r)
        for b in writes:
            b.w = ev
            b.r = []
        return ev

    def finish(self, bufs):
        for b in bufs:
            self._wait(self.sp, b.w)
            for ev in b.r:
                self._wait(self.sp, ev)


def V(ap, *dims):
    return bass.AP(ap.tensor, ap.offset, [list(ap.ap[0])] + [list(d) for d in dims])


def pbc(dram_ap_row, n, parts=128):
    return bass.AP(dram_ap_row.tensor, dram_ap_row.offset, [[0, parts], [1, n]])


class Ctx:
    def __init__(self):
        self.nc = bass.Bass("TRN2", target_bir_lowering=False)
        self.es = ExitStack()
        self.S = None
        self.n = 0

    def start(self):
        self.S = Sched(self.nc, self.es)
        self.ps = [self.es.enter_context(self.nc.psum_tensor("ps%d" % i, [128, 512], F32)) for i in range(8)]
        self.psb = [self.S.buf("psb%d" % i) for i in range(8)]
        for b in self.psb:
            b.excl = True

    def din(self, name, shape, dt=F32):
        return self.nc.dram_tensor(name, list(shape), dt, kind="ExternalInput").ap()

    def dout(self, name, shape, dt=F32):
        return self.nc.dram_tensor(name, list(shape), dt, kind="ExternalOutput").ap()

    def dint(self, name, shape, dt=F32):
        return self.nc.dram_tensor(name, list(shape), dt, kind="Internal").ap()

    def sb(self, shape, dt=F32, name=None):
        self.n += 1
        t = self.es.enter_context(self.nc.sbuf_tensor(name or ("t%d" % self.n), list(shape), dt))
        return t

    def act(self, out, in_, func, r, w, **kw):
        nc = self.nc
        return self.S.op(self.S.act, lambda: nc.scalar.activation(out=out, in_=in_, func=func, **kw), r, w)

    def tt(self, out, in0, in1, op, r, w, eng=None):
        e = eng or self.S.dve
        return self.S.op(e, lambda: e.h.tensor_tensor(out=out, in0=in0, in1=in1, op=op), r, w)

    def ts(self, out, in0, s1, op0, r, w, s2=None, op1=None, eng=None):
        e = eng or self.S.dve
        if op1 is None:
            return self.S.op(e, lambda: e.h.tensor_scalar(out=out, in0=in0, scalar1=s1, scalar2=None, op0=op0), r, w)
        return self.S.op(e, lambda: e.h.tensor_scalar(out=out, in0=in0, scalar1=s1, scalar2=s2, op0=op0, op1=op1), r, w)

    def stt(self, out, in0, scalar, in1, op0, op1, r, w, eng=None):
        e = eng or self.S.dve
        return self.S.op(e, lambda: e.h.scalar_tensor_tensor(out=out, in0=in0, scalar=scalar, in1=in1, op0=op0, op1=op1), r, w)

    def cp(self, out, in_, r, w, eng=None):
        e = eng or self.S.dve
        return self.S.op(e, lambda: e.h.tensor_copy(out=out, in_=in_), r, w)

    def red(self, out, in_, r, w, op=ALU.add):
        nc = self.nc
        return self.S.op(self.S.dve, lambda: nc.vector.tensor_reduce(out=out, in_=in_, axis=AX.X, op=op), r, w)

    def recip(self, out, in_, r, w):
        nc = self.nc
        return self.S.op(self.S.dve, lambda: nc.vector.reciprocal(out=out, in_=in_), r, w)

    def memset(self, ap, val, w, eng=None):
        e = eng or self.S.dve
        return self.S.op(e, lambda: e.h.memset(ap, val), (), w)

    def mm(self, out, lhsT, rhs, start, stop, r, w):
        nc = self.nc
        return self.S.op(self.S.pe, lambda: nc.tensor.matmul(out, lhsT=lhsT, rhs=rhs, start=start, stop=stop), r, w)

    def tr(self, out, in_, ident, r, w):
        nc = self.nc
        return self.S.op(self.S.pe, lambda: nc.tensor.transpose(out, in_, ident), r, w)

    def load(self, out, in_, w, r=(), q=None, **kw):
        return self.S.dma(q or self.S.sp, out, in_, reads=r, writes=w, **kw)

    def store(self, out, in_, r, w, q=None, **kw):
        return self.S.dma(q or self.S.pool, out, in_, reads=r, writes=w, **kw)

    def rstd(self, out, ss, scale, r, w):
        self.act(out, ss, AF.Sqrt, list(r) + [self.b_eps], w, bias=self.eps_ap, scale=scale)
        self.recip(out, out, w, w)

    def consts(self, ident_dram):
        self.idf = self.sb([128, 128], F32, "idf")
        self.idb = self.sb([128, 128], BF16, "idb")
        self.b_id = self.S.buf("ident")
        self.load(self.idf[:], ident_dram[:, :], [self.b_id])
        self.cp(self.idb[:], self.idf[:], [self.b_id], [self.b_id])
        self.epst = self.sb([128, 1], F32, "epst")
        self.b_eps = self.S.buf("eps")
        self.memset(self.epst[:], EPS, [self.b_eps])
        self.eps_ap = self.epst[:, 0:1]


def run(ctx, in_maps):
    res = run_bass_kernel_spmd(ctx.nc, in_maps, core_ids=list(range(8)))
    return res.results


IDENT = np.eye(128, dtype=np.float32)


def build_mod():
    C = Ctx()
    nc = C.nc
    NCOL = 6144
    ccol = C.din("ccol", [128, 16])
    W = C.din("W", [D, NCOL])
    bias = C.din("bias", [1, NCOL])
    out = C.dout("mod", [1, NCOL])
    with C.es:
        C.start()
        S = C.S
        ct = C.sb([128, 16]); sc = C.sb([128, 16])
        bt = C.sb([1, NCOL]); ot = C.sb([1, NCOL])
        slabs = [C.sb([128, 16 * 512]) for _ in range(2)]
        b_c, b_sc, b_b, b_o, b_out = S.bufs(5)
        b_sl = S.bufs(2)
        C.load(ct[:], ccol[:, :], [b_c])
        C.load(bt[:], bias[:, :], [b_b])
        C.act(sc[:], ct[:], AF.Silu, [b_c], [b_sc])
        Wv = W.rearrange("(k p) n -> p k n", p=128)
        for n in range(NCOL // 512):
            sl = slabs[n % 2]
            C.load(sl[:].rearrange("p (k n) -> p k n", k=16), Wv[:, :, n * 512:(n + 1) * 512], [b_sl[n % 2]])
            pb = n % 2
            for k in range(16):
                C.mm(C.ps[pb][0:1, :], sc[:, k:k + 1], sl[:, k * 512:(k + 1) * 512], k == 0, k == 15,
                     [b_sc, b_sl[n % 2]], [C.psb[pb]])
            C.tt(ot[:, n * 512:(n + 1) * 512], C.ps[pb][0:1, :], bt[:, n * 512:(n + 1) * 512], ALU.add,
                 [C.psb[pb], b_b], [b_o])
        C.store(out[:, :], ot[:], [b_o], [b_out])
        S.finish([b_out])
    return C


def run_mod(inputs):
    c = np.asarray(inputs["c"], np.float32)
    ada_w = np.asarray(inputs["ada_w"], np.float32)
    ada_b = np.asarray(inputs["ada_b"], np.float32)
    C = build_mod()
    maps = []
    for core in range(8):
        b, j = core // 4, core % 4
        cols = slice(6144 * j, 6144 * (j + 1))
        if j < 2:
            Wc = ada_w[0][:, 6144 * j:6144 * (j + 1)]
            bc = ada_b[0][6144 * j:6144 * (j + 1)]
        else:
            Wc = ada_w[1][:, 6144 * (j - 2):6144 * (j - 1)]
            bc = ada_b[1][6144 * (j - 2):6144 * (j - 1)]
        maps.append({"ccol": np.ascontiguousarray(c[b].reshape(16, 128).T),
                     "W": np.ascontiguousarray(Wc), "bias": np.ascontiguousarray(bc.reshape(1, -1))})
    res = run(C, maps)
    mod = np.zeros((2, 2, 12288), np.float32)
    for core in range(8):
        b, j = core // 4, core % 4
        l, jj = j // 2, j % 2
        mod[l, b, 6144 * jj:6144 * (jj + 1)] = res[core]["mod"][0]
    return mod


def rope_tables(C, pos_d, invf_d, half, b_out):
    S = C.S
    n = 16 * half
    posi = C.sb([128, 16], I32); posf = C.sb([128, 16])
    invf = C.sb([128, half])
    ang = C.sb([128, n]); kf = C.sb([128, n]); ki = C.sb([128, n], I32)
    cos = C.sb([128, n]); sin = C.sb([128, n])
    b_p, b_i, b_a, b_k = S.bufs(4)
    C.load(posi[:], pos_d[:, :], [b_p])
    C.load(invf[:], pbc(invf_d, half), [b_i])
    C.cp(posf[:], posi[:], [b_p], [b_p])
    C.tt(V(ang[:], (half, 16), (1, half)), V(posf[:], (1, 16), (0, half)), V(invf[:], (0, 16), (1, half)),
         ALU.mult, [b_p, b_i], [b_a])
    for (dst, shift) in ((sin, 0.0), (cos, float(np.pi / 2))):
        if shift != 0.0:
            C.ts(dst[:], ang[:], shift, ALU.add, [b_a], [b_out])
            src = dst
        else:
            src = ang
        C.ts(kf[:], src[:], float(1.0 / TWO_PI), ALU.mult, [b_a, b_out], [b_k])
        C.cp(ki[:], kf[:], [b_k], [b_k])
        C.cp(kf[:], ki[:], [b_k], [b_k])
        C.stt(dst[:], kf[:], -TWO_PI, src[:], ALU.mult, ALU.add, [b_k, b_a, b_out], [b_out])
        C.ts(dst[:], dst[:], float(np.pi), ALU.min, [b_out], [b_out], s2=float(-np.pi), op1=ALU.max)
        C.act(dst[:], dst[:], AF.Sin, [b_out], [b_out])
    return cos, sin


def rope_apply(C, out1, out2, x1, x2, cosv, sinv, ta, tb, r, w, b_t):
    C.tt(ta, x1, cosv, ALU.mult, r, [b_t])
    C.tt(tb, x2, sinv, ALU.mult, r, [b_t])
    C.tt(out1, ta, tb, ALU.subtract, [b_t], w)
    C.tt(ta, x2, cosv, ALU.mult, r, [b_t])
    C.tt(tb, x1, sinv, ALU.mult, r, [b_t])
    C.tt(out2, ta, tb, ALU.add, [b_t], w)


def load_w_bf16(C, dram_w, K, N, b):
    kc = K // 128
    t = C.sb([128, kc * N], BF16)
    src = dram_w.rearrange("(k p) n -> p k n", p=128)
    for k in range(kc):
        C.load(t[:, k * N:(k + 1) * N], src[:, k, :], [b], q=C.S.pool)
    return t


def norm_modulate_setup(C, g_d, scale_d, shift_d, tmp, b_t):
    S = C.S
    gmod = C.sb([128, D]); shiftb = C.sb([128, D])
    b_g, b_s = S.bufs(2)
    C.load(gmod[:], pbc(g_d, D), [b_g])
    C.load(tmp, pbc(scale_d, D), [b_t])
    C.load(shiftb[:], pbc(shift_d, D), [b_s])
    C.stt(gmod[:], tmp, 1.0, gmod[:], ALU.add, ALU.mult, [b_t, b_g], [b_g])
    return gmod, shiftb, b_g, b_s


def build_mla_pre(ntile=NTILE, dbg=0):
    C = Ctx()
    nc = C.nc
    x = C.din("x", [NT, D]); pos = C.din("pos", [128, 16], I32)
    shift_d = C.din("shift", [1, D]); scale_d = C.din("scale", [1, D]); g_d = C.din("g", [1, D])
    w_in = C.din("w_in", [D, 1088]); g_q = C.din("g_q", [1, 512]); w_uq = C.din("w_uq", [512, 3072])
    g_kv = C.din("g_kv", [1, 512]); w_ukv = C.din("w_ukv", [512, 4096])
    g_qn = C.din("g_qn", [1, 192]); g_kn = C.din("g_kn", [1, 192])
    invf = C.din("invf", [1, 32]); ident = C.din("ident", [128, 128])
    QT = C.dout("QT", [16, 192, NT], BF16); KT = C.dout("KT", [16, 192, NT], BF16)
    Vo = C.dout("V", [NT, D], BF16)
    with C.es:
        C.start()
        S = C.S
        C.consts(ident)
        b_w = S.buf("w")
        w_in_sb = load_w_bf16(C, w_in, D, 1088, b_w)
        w_uq_sb = load_w_bf16(C, w_uq, 512, 3072, b_w)
        w_ukv_sb = load_w_bf16(C, w_ukv, 512, 4096, b_w)
        junk = C.sb([128, 2048], BF16); b_junk = S.buf()
        tmpf = C.sb([128, 3072]); b_tmpf = S.buf()
        gmod, shiftb, b_g, b_s = norm_modulate_setup(C, g_d, scale_d, shift_d, tmpf[:, 0:D], b_tmpf)
        gq = C.sb([128, 512]); gkv = C.sb([128, 512]); gqn = C.sb([128, 192]); gkn = C.sb([128, 192])
        b_gs = S.buf("gains")
        for t_, d_, n_ in ((gq, g_q, 512), (gkv, g_kv, 512), (gqn, g_qn, 192), (gkn, g_kn, 192)):
            C.load(t_[:], pbc(d_, n_), [b_gs])
        b_cs = S.buf("cossin")
        cos, sin = rope_tables(C, pos, invf, 32, b_cs)

        xt = [C.sb([128, D]) for _ in range(2)]
        b_x = S.bufs(2)
        st = [C.sb([128, 64]) for _ in range(2)]; b_st = S.bufs(2)
        hb = [C.sb([128, D], BF16)] * 2; b_hb = [S.buf()] * 2
        hT = [C.sb([128, D], BF16)] * 2; b_hT = [S.buf()] * 2
        cn = [C.sb([128, 1024], BF16)] * 2; b_cn = [S.buf()] * 2
        cT = [C.sb([128, 1024], BF16)] * 2; b_cT = [S.buf()] * 2
        kr = [C.sb([128, 64]) for _ in range(2)]; b_kr = S.bufs(2)
        qsb = C.sb([128, 3072]); b_q = S.buf()
        qf = C.sb([128, 3072], BF16); b_qf = S.buf()
        knsb = qsb; b_kn = b_q
        kfn = qf; kfr = qf; b_kf = b_qf
        vsb = [C.sb([128, 2048], BF16)] * 2; b_v = [S.buf()] * 2
        krr = C.sb([128, 64]); ra = C.sb([128, 512]); rb = C.sb([128, 512]); b_r = S.buf()
        Tn = [C.sb([128, 2048], BF16)] * 2; Tr = [C.sb([64, 2048], BF16)] * 2
        b_T = [S.buf()] * 2
        b_QT, b_KT, b_V = S.bufs(3)
        ps, psb = C.ps, C.psb

        def ps_bf(i):
            return ps[i][:].bitcast(BF16)

        for i in range(ntile):
            p = i % 2
            s_ = st[p]
            C.load(xt[p][:], x[i * 128:(i + 1) * 128, :], [b_x[p]])
            if dbg == 1:
                continue
            C.memset(s_[:], 0.0, [b_st[p]])
            C.act(junk[:, 0:D], xt[p][:], AF.Square, [b_x[p]], [b_junk, b_st[p]], accum_out=s_[:, 0:1])
            C.rstd(s_[:, 1:2], s_[:, 0:1], 1.0 / D, [b_st[p]], [b_st[p]])
            C.stt(tmpf[:, 0:D], xt[p][:], s_[:, 1:2], gmod[:], ALU.mult, ALU.mult, [b_x[p], b_st[p], b_g], [b_tmpf])
            C.tt(hb[p][:], tmpf[:, 0:D], shiftb[:], ALU.add, [b_tmpf, b_s], [b_hb[p]])
            for k in range(16):
                bk = k // 8
                C.tr(ps_bf(bk)[:, (k % 8) * 128:(k % 8 + 1) * 128], hb[p][:, k * 128:(k + 1) * 128], C.idb[:],
                     [b_hb[p], C.b_id], [psb[bk]])
            for bk in range(2):
                C.act(hT[p][:, bk * 1024:(bk + 1) * 1024], ps_bf(bk), AF.Copy, [psb[bk]], [b_hT[p]])
            if dbg == 2:
                continue
            for (bk, n0, n1) in ((2, 0, 512), (3, 512, 1024), (4, 1024, 1088)):
                for k in range(16):
                    C.mm(ps[bk][:, 0:n1 - n0], hT[p][:, k * 128:(k + 1) * 128],
                         w_in_sb[:, k * 1088 + n0:k * 1088 + n1], k == 0, k == 15, [b_hT[p], b_w], [psb[bk]])
            for (bk, col, gt) in ((2, 2, gq), (3, 4, gkv)):
                C.act(junk[:, 0:512], ps[bk][:], AF.Square, [psb[bk]], [b_junk, b_st[p]], accum_out=s_[:, col:col + 1])
                C.rstd(s_[:, col + 1:col + 2], s_[:, col:col + 1], 1.0 / 512, [b_st[p]], [b_st[p]])
                o = (bk - 2) * 512
                C.stt(cn[p][:, o:o + 512], ps[bk][:], s_[:, col + 1:col + 2], gt[:], ALU.mult, ALU.mult,
                      [psb[bk], b_st[p], b_gs], [b_cn[p]])
            C.act(kr[p][:], ps[4][:, 0:64], AF.Copy, [psb[4]], [b_kr[p]])
            for k in range(8):
                C.tr(ps_bf(5)[:, k * 128:(k + 1) * 128], cn[p][:, k * 128:(k + 1) * 128], C.idb[:],
                     [b_cn[p], C.b_id], [psb[5]])
            C.act(cT[p][:], ps_bf(5), AF.Copy, [psb[5]], [b_cT[p]])
            if dbg == 3:
                continue
            for c in range(8):
                bk = 6 + c % 2
                for k in range(4):
                    C.mm(ps[bk][:, 0:384], cT[p][:, k * 128:(k + 1) * 128],
                         w_uq_sb[:, k * 3072 + c * 384:k * 3072 + (c + 1) * 384], k == 0, k == 3,
                         [b_cT[p], b_w], [psb[bk]])
                C.act(qsb[:, c * 384:(c + 1) * 384], ps[bk][:, 0:384], AF.Copy, [psb[bk]], [b_q])
            C.act(tmpf[:, 0:3072], qsb[:], AF.Square, [b_q], [b_tmpf])
            C.red(s_[:, 16:32], tmpf[:, 0:3072].rearrange("p (h d) -> p h d", h=16), [b_tmpf], [b_st[p]])
            C.rstd(s_[:, 16:32], s_[:, 16:32], 1.0 / 192, [b_st[p]], [b_st[p]])
            q3 = qsb[:].rearrange("p (h d) -> p h d", h=16)
            C.tt(q3, q3, V(s_[:, 16:17], (1, 16), (0, 192)), ALU.mult, [b_q, b_st[p]], [b_q])
            C.tt(q3, q3, V(gqn[:], (0, 16), (1, 192)), ALU.mult, [b_q, b_gs], [b_q])
            qf3 = qf[:].rearrange("p (h d) -> p h d", h=16)
            C.cp(qf3[:, :, 0:128], q3[:, :, 0:128], [b_q], [b_qf], eng=S.pool)
            cosv = V(cos[:, i * 32:i * 32 + 1], (0, 16), (1, 32)); sinv = V(sin[:, i * 32:i * 32 + 1], (0, 16), (1, 32))
            ra3 = V(ra[:], (32, 16), (1, 32)); rb3 = V(rb[:], (32, 16), (1, 32))
            rope_apply(C, qf3[:, :, 128:160], qf3[:, :, 160:192], q3[:, :, 128:160], q3[:, :, 160:192],
                       cosv, sinv, ra3, rb3, [b_q, b_cs], [b_qf], b_r)
            if dbg == 4:
                continue
            tp = i % 2
            for h in range(16):
                C.tr(ps_bf(h // 8)[:, (h % 8) * 128:(h % 8 + 1) * 128], qf3[:, h, 0:128], C.idb[:],
                     [b_qf, C.b_id], [psb[h // 8]])
                C.tr(ps_bf(2 + h // 8)[0:64, (h % 8) * 128:(h % 8 + 1) * 128], qf3[:, h, 128:192], C.idb[:],
                     [b_qf, C.b_id], [psb[2 + h // 8]])
            for bk in range(2):
                C.act(Tn[tp][:, bk * 1024:(bk + 1) * 1024], ps_bf(bk), AF.Copy, [psb[bk]], [b_T[tp]])
                C.cp(Tr[tp][:, bk * 1024:(bk + 1) * 1024], ps_bf(2 + bk)[0:64, :], [psb[2 + bk]], [b_T[tp]])
            C.store(QT[:, 0:128, i * 128:(i + 1) * 128].rearrange("h d t -> d h t"),
                    Tn[tp][:].rearrange("p (h t) -> p h t", h=16), [b_T[tp]], [b_QT])
            C.store(QT[:, 128:192, i * 128:(i + 1) * 128].rearrange("h d t -> d h t"),
                    Tr[tp][:].rearrange("p (h t) -> p h t", h=16), [b_T[tp]], [b_QT])
            if dbg == 5:
                continue
            kn3 = knsb[:, 0:2048].rearrange("p (h d) -> p h d", h=16)
            v3 = vsb[p][:].rearrange("p (h d) -> p h d", h=16)
            for c in range(8):
                bk = 6 + c % 2
                for k in range(4):
                    C.mm(ps[bk][:], cT[p][:, 512 + k * 128:512 + (k + 1) * 128],
                         w_ukv_sb[:, k * 4096 + c * 512:k * 4096 + (c + 1) * 512], k == 0, k == 3,
                         [b_cT[p], b_w], [psb[bk]])
                pv = ps[bk][:].rearrange("p (h d) -> p h d", h=2)
                C.act(kn3[:, 2 * c:2 * c + 2, :], pv[:, :, 0:128], AF.Copy, [psb[bk]], [b_kn])
                C.cp(v3[:, 2 * c:2 * c + 2, :], pv[:, :, 128:256], [psb[bk]], [b_v[p]])
            C.store(Vo[i * 128:(i + 1) * 128, :], vsb[p][:], [b_v[p]], [b_V])
            if dbg == 6:
                continue
            C.act(tmpf[:, 0:2048], knsb[:, 0:2048], AF.Square, [b_kn], [b_tmpf])
            C.red(s_[:, 32:48], tmpf[:, 0:2048].rearrange("p (h d) -> p h d", h=16), [b_tmpf], [b_st[p]])
            C.act(junk[:, 0:64], kr[p][:], AF.Square, [b_kr[p]], [b_junk, b_st[p]], accum_out=s_[:, 6:7])
            C.ts(s_[:, 32:48], s_[:, 32:48], s_[:, 6:7], ALU.add, [b_st[p]], [b_st[p]])
            C.rstd(s_[:, 32:48], s_[:, 32:48], 1.0 / 192, [b_st[p]], [b_st[p]])
            kfn3 = kfn[:, 0:2048].rearrange("p (h d) -> p h d", h=16)
            C.tt(kn3, kn3, V(s_[:, 32:33], (1, 16), (0, 128)), ALU.mult, [b_kn, b_st[p]], [b_kn])
            C.tt(kfn3, kn3, V(gkn[:], (0, 16), (1, 128)), ALU.mult, [b_kn, b_gs], [b_kf])
            C.tt(kr[p][:], kr[p][:], gkn[:, 128:192], ALU.mult, [b_kr[p], b_gs], [b_kr[p]])
            rope_apply(C, krr[:, 0:32], krr[:, 32:64], kr[p][:, 0:32], kr[p][:, 32:64],
                       cos[:, i * 32:(i + 1) * 32], sin[:, i * 32:(i + 1) * 32], ra[:, 0:32], rb[:, 0:32],
                       [b_kr[p], b_cs], [b_r], b_r)
            C.tt(V(kfr[:, 2048:2049], (64, 16), (1, 64)), V(krr[:], (0, 16), (1, 64)), V(s_[:, 32:33], (1, 16), (0, 64)),
                 ALU.mult, [b_r, b_st[p]], [b_kf])
            if dbg == 7:
                continue
            tp = (i + 1) % 2
            kfr3 = V(kfr[:, 2048:2049], (64, 16), (1, 64))
            for h in range(16):
                C.tr(ps_bf(h // 8)[:, (h % 8) * 128:(h % 8 + 1) * 128], kfn3[:, h, :], C.idb[:],
                     [b_kf, C.b_id], [psb[h // 8]])
                C.tr(ps_bf(2 + h // 8)[0:64, (h % 8) * 128:(h % 8 + 1) * 128], kfr3[:, h, :], C.idb[:],
                     [b_kf, C.b_id], [psb[2 + h // 8]])
            for bk in range(2):
                C.act(Tn[tp][:, bk * 1024:(bk + 1) * 1024], ps_bf(bk), AF.Copy, [psb[bk]], [b_T[tp]])
                C.cp(Tr[tp][:, bk * 1024:(bk + 1) * 1024], ps_bf(2 + bk)[0:64, :], [psb[2 + bk]], [b_T[tp]])
            C.store(KT[:, 0:128, i * 128:(i + 1) * 128].rearrange("h d t -> d h t"),
                    Tn[tp][:].rearrange("p (h t) -> p h t", h=16), [b_T[tp]], [b_KT])
            C.store(KT[:, 128:192, i * 128:(i + 1) * 128].rearrange("h d t -> d h t"),
                    Tr[tp][:].rearrange("p (h t) -> p h t", h=16), [b_T[tp]], [b_KT])
        S.finish([b_QT, b_KT, b_V])
    return C


def core_tokens(core):
    b, j = core // 4, core % 4
    return b, slice(NT * j, NT * (j + 1))


def pos_layout(positions, core):
    b, sl = core_tokens(core)
    return np.ascontiguousarray(np.asarray(positions[b, sl], np.int32).reshape(16, 128).T)


def inv_freq(half):
    return (10000.0 ** (-np.arange(half, dtype=np.float32) / np.float32(half))).astype(np.float32).reshape(1, half)


def row(v):
    return np.ascontiguousarray(np.asarray(v, np.float32).reshape(1, -1))


def build_mla_attn(nqb=4, nheads=16):
    C = Ctx()
    nc = C.nc
    QT = C.din("QT", [16, 192, NT], BF16); KT = C.din("KT", [16, 192, SEQ], BF16)
    Vp = C.din("Vp", [16, 128, 64 * 128], BF16)
    maskb_d = C.din("maskb", [128, 64]); tri_d = C.din("tri", [128, 128])
    x = C.din("x", [NT, D]); gate_d = C.din("gate", [1, D]); w_o = C.din("w_o", [D, D])
    ident = C.din("ident", [128, 128])
    x1 = C.dout("x1", [NT, D])
    SCALE = float(192 ** -0.5)
    with C.es:
        C.start()
        S = C.S
        ps, psb = C.ps, C.psb
        b_w = S.buf("w")
        w_o_sb = load_w_bf16(C, w_o, D, D, b_w)
        maskb = C.sb([128, 64]); trif = C.sb([128, 128]); trib = C.sb([128, 128], BF16)
        ones = C.sb([128, 128], BF16); gate = C.sb([128, D])
        b_c = S.buf("consts")
        C.load(maskb[:], maskb_d[:, :], [b_c]); C.load(trif[:], tri_d[:, :], [b_c])
        C.load(gate[:], pbc(gate_d, D), [b_c])
        C.cp(trib[:], trif[:], [b_c], [b_c])
        C.memset(ones[:], 1.0, [b_c])
        NS = 3
        ktn = [C.sb([128, 2048], BF16) for _ in range(NS)]; ktr = [C.sb([64, 2048], BF16) for _ in range(NS)]
        vs = [C.sb([128, 2048], BF16) for _ in range(NS)]; b_kv = S.bufs(NS)
        qn = [C.sb([128, 512], BF16) for _ in range(2)]; qr = [C.sb([64, 512], BF16) for _ in range(2)]; b_qq = S.bufs(2)
        pt = [C.sb([128, 512], BF16) for _ in range(3)]; b_pt = S.bufs(3)
        ot = C.sb([128, 16 * 512], BF16); b_ot = S.buf()
        rden = C.sb([128, 512]); b_rd = S.buf()
        xt = [C.sb([128, D]) for _ in range(2)]; b_x = S.bufs(2)
        yt = C.sb([128, D]); b_y = S.buf()
        b_out = S.buf("out")
        slot = 0
        npt = 0
        for qb in range(nqb):
            nch = 48 + 4 * (qb + 1)
            for h in range(nheads):
                qp = (qb * nheads + h) % 2
                C.load(qn[qp][:], QT[h, 0:128, qb * 512:(qb + 1) * 512], [b_qq[qp]])
                C.load(qr[qp][:], QT[h, 128:192, qb * 512:(qb + 1) * 512], [b_qq[qp]])
                bo = 2 + qp
                bd = 4 + qp
                for seg in range(4):
                    sl = slot % NS
                    slot += 1
                    c0 = seg * 16
                    c1 = min(nch, c0 + 16)
                    nk = (c1 - c0) * 128
                    C.load(ktn[sl][:, 0:nk], KT[h, 0:128, c0 * 128:c0 * 128 + nk], [b_kv[sl]])
                    C.load(ktr[sl][:, 0:nk], KT[h, 128:192, c0 * 128:c0 * 128 + nk], [b_kv[sl]])
                    C.load(vs[sl][:, 0:nk], Vp[h, :, c0 * 128:c0 * 128 + nk], [b_kv[sl]])
                    for c in range(c0, c1):
                        lc = c - c0
                        dc = c - (nch - 4)
                        q0 = 128 * dc if dc > 0 else 0
                        bs = c % 2
                        pi = npt % 3
                        npt += 1
                        C.mm(ps[bs][:, q0:512], ktn[sl][:, lc * 128:(lc + 1) * 128], qn[qp][:, q0:512], True, False,
                             [b_kv[sl], b_qq[qp]], [psb[bs]])
                        C.mm(ps[bs][:, q0:512], ktr[sl][:, lc * 128:(lc + 1) * 128], qr[qp][:, q0:512], False, True,
                             [b_kv[sl], b_qq[qp]], [psb[bs]])
                        C.act(pt[pi][:, q0:512], ps[bs][:, q0:512], AF.Exp, [psb[bs], b_c], [b_pt[pi]],
                              bias=maskb[:, c:c + 1], scale=SCALE)
                        if dc >= 0:
                            C.tt(pt[pi][:, q0:q0 + 128], pt[pi][:, q0:q0 + 128], trib[:], ALU.mult,
                                 [b_pt[pi], b_c], [b_pt[pi]])
                        C.mm(ps[bo][:, q0:512], vs[sl][:, lc * 128:(lc + 1) * 128], pt[pi][:, q0:512], c == 0, c == nch - 1,
                             [b_kv[sl], b_pt[pi]], [psb[bo]])
                        C.mm(ps[bd][:, q0:512], ones[:], pt[pi][:, q0:512], c == 0, c == nch - 1,
                             [b_c, b_pt[pi]], [psb[bd]])
                C.recip(rden[:], ps[bd][:], [psb[bd]], [b_rd])
                C.tt(ot[:, h * 512:(h + 1) * 512], ps[bo][:], rden[:], ALU.mult, [psb[bo], b_rd], [b_ot])
            for t4 in range(4):
                ti = qb * 4 + t4
                xp = ti % 2
                C.load(xt[xp][:], x[ti * 128:(ti + 1) * 128, :], [b_x[xp]])
                for half in range(2):
                    for nn in range(2):
                        bk = 6 + nn
                        n0 = half * 1024 + nn * 512
                        for hh in range(nheads):
                            C.mm(ps[bk][:], ot[:, hh * 512 + t4 * 128:hh * 512 + (t4 + 1) * 128],
                                 w_o_sb[:, hh * 2048 + n0:hh * 2048 + n0 + 512], hh == 0, hh == nheads - 1,
                                 [b_ot, b_w], [psb[bk]])
                        C.tt(yt[:, n0:n0 + 512], ps[bk][:], gate[:, n0:n0 + 512], ALU.mult, [psb[bk], b_c], [b_y])
                C.tt(yt[:], yt[:], xt[xp][:], ALU.add, [b_y, b_x[xp]], [b_y])
                C.store(x1[ti * 128:(ti + 1) * 128, :], yt[:], [b_y], [b_out])
        S.finish([b_out])
    return C


def tri_mask():
    p = np.arange(128)[:, None]
    i = np.arange(128)[None, :]
    return (p <= i).astype(np.float32)


def mla_exchange(p1res):
    maps = []
    for core in range(8):
        b, j = core // 4, core % 4
        KT_all = np.concatenate([np.asarray(p1res[b * 4 + jj]["KT"]) for jj in range(4)], axis=2)
        V_all = np.concatenate([np.asarray(p1res[b * 4 + jj]["V"]) for jj in range(4)], axis=0)
        nvalid = NT * (j + 1)
        KTp = np.zeros((16, 192, SEQ), dtype=KT_all.dtype)
        KTp[:, :, SEQ - nvalid:] = KT_all[:, :, :nvalid]
        Vs = np.zeros((SEQ, D), dtype=V_all.dtype)
        Vs[SEQ - nvalid:] = V_all[:nvalid]
        Vp = np.ascontiguousarray(Vs.reshape(64, 128, 16, 128).transpose(2, 1, 0, 3)).reshape(16, 128, 64 * 128)
        maskb = np.zeros((128, 64), np.float32)
        maskb[:, :(SEQ - nvalid) // 128] = -30000.0
        maps.append(dict(KT=KTp, Vp=Vp, maskb=maskb))
    return maps


def fence(old, new):
    evs = []
    for b in old:
        if b.w is not None:
            evs.append(b.w)
        evs.extend(b.r)
    evs = Sched._compact(evs) if evs else []
    for b in new:
        b.w = None
        b.r = list(evs)


def build_peer(ntb=4, ngroups=32):
    C = Ctx()
    nc = C.nc
    x = C.din("x", [NT, D])
    shift_d = C.din("shift", [1, D]); scale_d = C.din("scale", [1, D]); gate_d = C.din("gate", [1, D]); g_d = C.din("g", [1, D])
    w_q = C.din("w_q", [D, D]); skT_d = C.din("skT", [128, 16 * 128])
    UT = C.din("UT", [D, 16384]); Vt = C.din("Vt", [16384, D])
    ident = C.din("ident", [128, 128])
    out = C.dout("x2", [NT, D])
    MASK_T = 1.0 - 2e-4
    with C.es:
        S = Sched(nc, C.es)
        C.S = S
        psY = C.es.enter_context(nc.psum_tensor("psY", [128, 2048], F32)); b_psY = S.buf("psY"); b_psY.excl = True
        ps = [C.es.enter_context(nc.psum_tensor("psq%d" % i, [128, 512], F32)) for i in range(4)]
        psb = S.bufs(4)
        for b in psb:
            b.excl = True
        psYb = [psY[:, i * 512:(i + 1) * 512] for i in range(4)]
        C.consts(ident)
        skT = C.sb([128, 2048]); b_sk = S.buf()
        C.load(skT[:], skT_d[:, :], [b_sk])
        yacc = [C.sb([128, D]) for _ in range(4)]; b_ya = S.bufs(4)
        hTb = C.sb([128, 16 * 512], BF16); b_hTb = S.buf()
        a1 = [C.sb([128, 1024]) for _ in range(4)]; a2 = [C.sb([128, 1024]) for _ in range(4)]; b_a = S.bufs(4)
        diag = C.sb([128, 32 * 128], BF16); b_dg = S.buf()
        st = C.sb([128, 64]); b_st = S.buf()
        tk = C.sb([128, 4 * 64]); b_tk = S.buf()
        cand = C.sb([128, 512]); b_cd = S.buf()
        junk = C.sb([128, 2048], BF16); b_junk = S.buf()
        AW = 25600
        arena = C.sb([128, AW])
        o = 0
        def carve(n, dt=F32):
            nonlocal o
            v = arena[:, o:o + n]
            o += n
            return v.bitcast(dt) if dt != F32 else v
        gmod = carve(2048); shiftb = carve(2048); xt = carve(2048); h2 = carve(2048)
        hTf = carve(8192); wq = [carve(2048), carve(2048)]; qTc = [carve(512), carve(512)]
        s_sb = [carve(512), carve(512)]
        assert o <= AW
        pb_g, pb_x, pb_h2, pb_hTf, pb_s = S.bufs(5)
        pb_wq = S.bufs(2); pb_qT = S.bufs(2)
        pro_bufs = [pb_g, pb_x, pb_h2, pb_hTf, pb_s] + pb_wq + pb_qT
        o = 0
        utg = [carve(4096, BF16), carve(4096, BF16)]
        vg = [carve(4096, BF16), carve(4096, BF16)]
        Pp = carve(2048)
        Mp = [carve(1024, BF16), carve(1024, BF16)]
        gel = [carve(512), carve(512)]
        actT = [carve(1024, BF16), carve(1024, BF16)]
        ytmp = [carve(2048)] * 2
        assert o <= AW, o
        mb_ut = S.bufs(2); mb_v = S.bufs(2); mb_P = S.buf(); mb_M = S.bufs(2); mb_gel = S.bufs(2)
        mb_act = S.bufs(2); mb_yt = [S.buf()] * 2
        main_bufs = mb_ut + mb_v + [mb_P] + mb_M + mb_gel + mb_act + mb_yt
        o = 0
        gateb = carve(2048); ext = [carve(2048), carve(2048)]; eo = [carve(2048), carve(2048)]
        eb_g = S.buf(); eb_x = S.bufs(2); eb_o = S.bufs(2)
        epi_bufs = [eb_g] + eb_x + eb_o
        b_out = S.buf("out")
        UTv = UT.rearrange("(k p) e -> p k e", p=128)
        wqv = w_q.rearrange("(k p) n -> p k n", p=128)
        a1v = [t[:] for t in a1]; a2v = [t[:] for t in a2]

        for tb in range(ntb):
            fence(epi_bufs + main_bufs, pro_bufs)
            C.load(gmod, pbc(g_d, D), [pb_g]); C.load(h2, pbc(scale_d, D), [pb_h2]); C.load(shiftb, pbc(shift_d, D), [pb_g])
            C.stt(gmod, h2, 1.0, gmod, ALU.add, ALU.mult, [pb_h2, pb_g], [pb_g])
            for tt in range(4):
                ti = tb * 4 + tt
                C.load(xt, x[ti * 128:(ti + 1) * 128, :], [pb_x])
                C.memset(st[:, 0:1], 0.0, [b_st])
                C.act(junk[:], xt, AF.Square, [pb_x], [b_junk, b_st], accum_out=st[:, 0:1])
                C.rstd(st[:, 1:2], st[:, 0:1], 1.0 / D, [b_st], [b_st])
                C.stt(h2, xt, st[:, 1:2], gmod, ALU.mult, ALU.mult, [pb_x, b_st, pb_g], [pb_h2])
                C.tt(h2, h2, shiftb, ALU.add, [pb_h2, pb_g], [pb_h2])
                for bk in range(4):
                    for kk in range(4):
                        k = bk * 4 + kk
                        C.tr(ps[bk][:, kk * 128:(kk + 1) * 128], h2[:, k * 128:(k + 1) * 128], C.idf[:],
                             [pb_h2, C.b_id], [psb[bk]])
                    src = ps[bk][:].rearrange("p (k t) -> p k t", k=4)
                    C.act(V(hTf[:, bk * 4 * 512 + tt * 128:bk * 4 * 512 + tt * 128 + 1], (512, 4), (1, 128)), src, AF.Copy,
                          [psb[bk]], [pb_hTf])
                    C.cp(V(hTb[:, bk * 4 * 512 + tt * 128:bk * 4 * 512 + tt * 128 + 1], (512, 4), (1, 128)), src,
                         [psb[bk]], [b_hTb])
            for cc in range(16):
                hd, pp = cc // 2, cc % 2
                w = wq[cc % 2]
                C.load(w.rearrange("p (k n) -> p k n", k=16), wqv[:, :, cc * 128:(cc + 1) * 128], [pb_wq[cc % 2]])
                bq = cc % 2
                for k in range(16):
                    C.mm(ps[bq][:], w[:, k * 128:(k + 1) * 128], hTf[:, k * 512:(k + 1) * 512], k == 0, k == 15,
                         [pb_wq[cc % 2], pb_hTf], [psb[bq]])
                C.act(qTc[cc % 2], ps[bq][:], AF.Copy, [psb[bq]], [pb_qT[cc % 2]])
                bs = 2 + pp
                for tt in range(4):
                    C.mm(ps[bs][:, tt * 128:(tt + 1) * 128], qTc[cc % 2][:, tt * 128:(tt + 1) * 128],
                         skT[:, cc * 128:(cc + 1) * 128], True, True, [pb_qT[cc % 2], b_sk], [psb[bs]])
                C.act(s_sb[pp], ps[bs][:], AF.Copy, [psb[bs]], [pb_s])
                if pp == 0:
                    continue
                for tt in range(4):
                    T = tk[:, tt * 64:(tt + 1) * 64]
                    for half in range(2):
                        sv = s_sb[half][:, tt * 128:(tt + 1) * 128]
                        C.S.op(S.dve, lambda: nc.vector.max(out=T[:, half * 16:half * 16 + 8], in_=sv), [pb_s], [b_tk])
                        C.S.op(S.dve, lambda: nc.vector.match_replace(out=cand[:, 0:128], in_to_replace=T[:, half * 16:half * 16 + 8],
                                                                      in_values=sv, imm_value=-1e30), [pb_s, b_tk], [b_cd])
                        C.S.op(S.dve, lambda: nc.vector.max(out=T[:, half * 16 + 8:half * 16 + 16], in_=cand[:, 0:128]), [b_cd], [b_tk])
                    C.tt(V(cand[:, 0:1], (16, 16), (1, 16)), V(T[:, 0:1], (1, 16), (0, 16)), V(T[:, 16:17], (0, 16), (1, 16)),
                         ALU.add, [b_tk], [b_cd])
                    C.S.op(S.dve, lambda: nc.vector.max(out=T[:, 32:40], in_=cand[:, 0:256]), [b_cd], [b_tk])
                    C.S.op(S.dve, lambda: nc.vector.match_replace(out=cand[:, 256:512], in_to_replace=T[:, 32:40],
                                                                  in_values=cand[:, 0:256], imm_value=-1e30), [b_cd, b_tk], [b_cd])
                    C.S.op(S.dve, lambda: nc.vector.max(out=T[:, 40:48], in_=cand[:, 256:512]), [b_cd], [b_tk])
                    C.ts(T[:, 48:49], T[:, 47:48], -1.0, ALU.mult, [b_tk], [b_tk])
                    C.ts(T[:, 49:50], T[:, 47:48], -0.5, ALU.mult, [b_tk], [b_tk])
                    C.memset(T[:, 50:51], 0.0, [b_tk])
                    C.act(T[:, 52:64][:, 0:12], T[:, 32:44], AF.Exp, [b_tk], [b_tk], bias=T[:, 48:49], scale=1.0)
                    C.red(T[:, 50:51], T[:, 52:64], [b_tk], [b_tk])
                    C.act(T[:, 52:56], T[:, 44:48], AF.Exp, [b_tk], [b_tk], bias=T[:, 48:49], scale=1.0)
                    C.red(T[:, 51:52], T[:, 52:56], [b_tk], [b_tk])
                    C.tt(T[:, 50:51], T[:, 50:51], T[:, 51:52], ALU.add, [b_tk], [b_tk])
                    C.recip(T[:, 51:52], T[:, 50:51], [b_tk], [b_tk])
                    di = tt * 8 + hd
                    C.ts(diag[:, di * 128:(di + 1) * 128], C.idf[:], T[:, 51:52], ALU.mult, [C.b_id, b_tk], [b_dg])
                    C.act(a1[tt][:, hd * 128:(hd + 1) * 128], s_sb[0][:, tt * 128:(tt + 1) * 128], AF.Exp, [pb_s, b_tk], [b_a[tt]],
                          bias=T[:, 49:50], scale=1.0)
                    C.act(a2[tt][:, hd * 128:(hd + 1) * 128], s_sb[1][:, tt * 128:(tt + 1) * 128], AF.Exp, [pb_s, b_tk], [b_a[tt]],
                          bias=T[:, 49:50], scale=1.0)
            fence(pro_bufs, main_bufs)
            for tt in range(4):
                C.memset(yacc[tt][:], 0.0, [b_ya[tt]], eng=S.pool)
            nsub = 0
            for g in range(ngroups):
                sl = g % 2
                C.load(utg[sl].rearrange("p (k e) -> p k e", k=16), UTv[:, :, g * 512:(g + 1) * 512], [mb_ut[sl]], q=S.pool)
                C.load(vg[sl].rearrange("p (c d) -> p c d", c=4), Vt[g * 512:(g + 1) * 512, :].rearrange("(c p) d -> p c d", p=128),
                       [mb_v[sl]], q=S.pool)
                at = actT[g % 2]
                for sg in range(2):
                    for c2 in range(2):
                        c = sg * 2 + c2
                        for k in range(16):
                            C.mm(ps[c2][:], utg[sl][:, k * 512 + c * 128:k * 512 + (c + 1) * 128], hTb[:, k * 512:(k + 1) * 512],
                                 k == 0, k == 15, [mb_ut[sl], b_hTb], [psb[c2]])
                    for tt in range(4):
                        mi = nsub % 2
                        nsub += 1
                        i1 = g * 4 + sg * 2
                        P3 = V(Pp[:, 0:1], (256, 8), (128, 2), (1, 128))
                        C.tt(P3, V(a1v[tt][:, i1:i1 + 1], (128, 8), (1, 2), (0, 128)), V(a2v[tt][:, 0:1], (128, 8), (0, 2), (1, 128)),
                             ALU.mult, [b_a[tt]], [mb_P])
                        C.stt(Mp[mi], Pp, MASK_T, Pp, ALU.is_ge, ALU.mult, [mb_P], [mb_M[mi]])
                        for c2 in range(2):
                            for hd in range(8):
                                di = tt * 8 + hd
                                C.mm(ps[2 + c2][:, tt * 128:(tt + 1) * 128], Mp[mi][:, hd * 256 + c2 * 128:hd * 256 + (c2 + 1) * 128],
                                     diag[:, di * 128:(di + 1) * 128], hd == 0, hd == 7, [mb_M[mi], b_dg], [psb[2 + c2]])
                    for c2 in range(2):
                        c = sg * 2 + c2
                        C.act(gel[c2], ps[c2][:], AF.Gelu, [psb[c2]], [mb_gel[c2]])
                        C.tt(at[:, c * 512:(c + 1) * 512], gel[c2], ps[2 + c2][:], ALU.mult, [mb_gel[c2], psb[2 + c2]], [mb_act[g % 2]])
                for tt in range(4):
                    for dn in range(4):
                        for c in range(4):
                            C.mm(psYb[dn], at[:, c * 512 + tt * 128:c * 512 + (tt + 1) * 128],
                                 vg[sl][:, c * 2048 + dn * 512:c * 2048 + (dn + 1) * 512], c == 0, c == 3,
                                 [mb_act[g % 2], mb_v[sl]], [b_psY])
                    yi = (g * 4 + tt) % 2
                    C.act(ytmp[yi], psY[:], AF.Copy, [b_psY], [mb_yt[yi]])
                    C.tt(yacc[tt][:], yacc[tt][:], ytmp[yi], ALU.add, [mb_yt[yi], b_ya[tt]], [b_ya[tt]], eng=S.pool)
            fence(main_bufs, epi_bufs)
            C.load(gateb, pbc(gate_d, D), [eb_g])
            for tt in range(4):
                ti = tb * 4 + tt
                C.load(ext[tt % 2], x[ti * 128:(ti + 1) * 128, :], [eb_x[tt % 2]])
                C.tt(eo[tt % 2], yacc[tt][:], gateb, ALU.mult, [b_ya[tt], eb_g], [eb_o[tt % 2]])
                C.tt(eo[tt % 2], eo[tt % 2], ext[tt % 2], ALU.add, [eb_o[tt % 2], eb_x[tt % 2]], [eb_o[tt % 2]])
                C.store(out[ti * 128:(ti + 1) * 128, :], eo[tt % 2], [eb_o[tt % 2]], [b_out])
        S.finish([b_out])
    return C


def peer_inputs(inputs, layer):
    sk = np.asarray(inputs["peer_sub_keys"][layer], np.float32)
    skT = np.ascontiguousarray(sk.reshape(16, 128, 128).transpose(2, 0, 1)).reshape(128, 16 * 128)
    UT = np.ascontiguousarray(np.asarray(inputs["peer_u"][layer], np.float32).T)
    Vt = np.ascontiguousarray(np.asarray(inputs["peer_v"][layer], np.float32))
    return dict(skT=skT, UT=UT, Vt=Vt, w_q=np.ascontiguousarray(np.asarray(inputs["peer_w_q"][layer], np.float32)))


def build_dil_pre(nchunks=36):
    C = Ctx()
    nc = C.nc
    x = C.din("x", [NT, D]); pos = C.din("pos", [128, 16], I32)
    shift_d = C.din("shift", [1, D]); scale_d = C.din("scale", [1, D]); g_d = C.din("g", [1, D])
    w_in = C.din("w_in", [D, 18432]); gqk_d = C.din("gqk", [1, 6 * 128])
    invf = C.din("invf", [1, 64]); ident = C.din("ident", [128, 128])
    Z = C.dout("Z", [NT, 18432], BF16)
    with C.es:
        C.start()
        S = C.S
        ps, psb = C.ps, C.psb
        C.consts(ident)
        tmpf = C.sb([128, D]); b_tmpf = S.buf()
        gmod, shiftb, b_g, b_s = norm_modulate_setup(C, g_d, scale_d, shift_d, tmpf[:], b_tmpf)
        gqk = C.sb([128, 768]); b_gs = S.buf()
        C.load(gqk[:], pbc(gqk_d, 768), [b_gs])
        b_cs = S.buf("cossin")
        cos, sin = rope_tables(C, pos, invf, 64, b_cs)
        hT = C.sb([128, 16 * NT], BF16); b_hT = S.buf()
        xt = [C.sb([128, D]) for _ in range(2)]; b_x = S.bufs(2)
        junk = C.sb([128, D], BF16); b_junk = S.buf()
        hb = C.sb([128, D], BF16); b_hb = S.buf()
        st = C.sb([128, 16]); b_st = S.buf()
        wch = [C.sb([128, 16 * 512], BF16) for _ in range(2)]; b_w = S.bufs(2)
        zn = C.sb([128, 512]); b_zn = S.buf()
        ra = C.sb([128, 256]); rb = C.sb([128, 256]); b_r = S.buf()
        ot = [C.sb([128, 512], BF16) for _ in range(3)]; b_o = S.bufs(3)
        b_Z = S.buf("Z")

        def ps_bf(i):
            return ps[i][:].bitcast(BF16)

        hT3 = hT[:].rearrange("p (k t) -> p k t", k=16)
        for i in range(NTILE):
            p = i % 2
            C.load(xt[p][:], x[i * 128:(i + 1) * 128, :], [b_x[p]])
            C.memset(st[:, 0:1], 0.0, [b_st])
            C.act(junk[:], xt[p][:], AF.Square, [b_x[p]], [b_junk, b_st], accum_out=st[:, 0:1])
            C.rstd(st[:, 1:2], st[:, 0:1], 1.0 / D, [b_st], [b_st])
            C.stt(tmpf[:], xt[p][:], st[:, 1:2], gmod[:], ALU.mult, ALU.mult, [b_x[p], b_st, b_g], [b_tmpf])
            C.tt(hb[:], tmpf[:], shiftb[:], ALU.add, [b_tmpf, b_s], [b_hb])
            for k in range(16):
                bk = k // 8
                C.tr(ps_bf(bk)[:, (k % 8) * 128:(k % 8 + 1) * 128], hb[:, k * 128:(k + 1) * 128], C.idb[:],
                     [b_hb, C.b_id], [psb[bk]])
            for bk in range(2):
                C.act(hT3[:, bk * 8:(bk + 1) * 8, i * 128:(i + 1) * 128],
                      ps_bf(bk).rearrange("p (k t) -> p k t", k=8), AF.Copy, [psb[bk]], [b_hT])
        wv = w_in.rearrange("(k p) n -> p k n", p=128)
        no = 0
        for c in range(nchunks):
            blk = c // 4
            g, r = blk // 3, blk % 3
            w = wch[c % 2]
            C.load(w[:].rearrange("p (k n) -> p k n", k=16), wv[:, :, c * 512:(c + 1) * 512], [b_w[c % 2]], q=S.pool)
            for i in range(NTILE):
                bk = 2 + (c * NTILE + i) % 6
                for k in range(16):
                    C.mm(ps[bk][:], hT[:, k * NT + i * 128:k * NT + (i + 1) * 128], w[:, k * 512:(k + 1) * 512],
                         k == 0, k == 15, [b_hT, b_w[c % 2]], [psb[bk]])
                oi = no % 3
                no += 1
                o_ = ot[oi]
                if r == 2:
                    C.act(o_[:], ps[bk][:], AF.Copy, [psb[bk]], [b_o[oi]])
                else:
                    gain = gqk[:, (r * 3 + g) * 128:(r * 3 + g + 1) * 128]
                    C.act(junk[:, 0:512], ps[bk][:], AF.Square, [psb[bk]], [b_junk])
                    C.red(st[:, 4:8], junk[:, 0:512].rearrange("p (h d) -> p h d", h=4), [b_junk], [b_st])
                    C.rstd(st[:, 4:8], st[:, 4:8], 1.0 / 128, [b_st], [b_st])
                    z3 = zn[:].rearrange("p (h d) -> p h d", h=4)
                    C.tt(z3, ps[bk][:].rearrange("p (h d) -> p h d", h=4), V(st[:, 4:5], (1, 4), (0, 128)), ALU.mult,
                         [psb[bk], b_st], [b_zn])
                    C.tt(z3, z3, V(gain, (0, 4), (1, 128)), ALU.mult, [b_zn, b_gs], [b_zn])
                    o3 = o_[:].rearrange("p (h d) -> p h d", h=4)
                    cosv = V(cos[:, i * 64:i * 64 + 1], (0, 4), (1, 64)); sinv = V(sin[:, i * 64:i * 64 + 1], (0, 4), (1, 64))
                    rope_apply(C, o3[:, :, 0:64], o3[:, :, 64:128], z3[:, :, 0:64], z3[:, :, 64:128], cosv, sinv,
                               V(ra[:], (64, 4), (1, 64)), V(rb[:], (64, 4), (1, 64)), [b_zn, b_cs], [b_o[oi]], b_r)
                C.store(Z[i * 128:(i + 1) * 128, c * 512:(c + 1) * 512], o_[:], [b_o[oi]], [b_Z])
        S.finish([b_Z])
    return C


DIL = (1, 4, 16)


def build_dil_attn(ngh=48):
    C = Ctx()
    nc = C.nc
    QT = C.din("QT", [48, 128, NT], BF16); KT = C.din("KT", [48, 128, 4096], BF16); Vb = C.din("Vb", [48, 128, 4096], BF16)
    halo_d = C.din("halo", [128, 1]); tri_d = C.din("tri2", [128, 256])
    x = C.din("x", [NT, D]); gate_d = C.din("gate", [1, D]); w_o = C.din("w_o", [D, D])
    ident = C.din("ident", [128, 128])
    x1 = C.dout("x1", [NT, D])
    SCALE = float(128 ** -0.5)
    with C.es:
        C.start()
        S = C.S
        ps, psb = C.ps, C.psb
        b_w = S.buf("w")
        w_o_sb = load_w_bf16(C, w_o, D, D, b_w)
        trif = C.sb([128, 256]); tri = C.sb([128, 256], BF16); trih = C.sb([128, 256], BF16); halo = C.sb([128, 1])
        ones = C.sb([128, 128], BF16); gate = C.sb([128, D])
        b_c = S.buf("consts")
        C.load(trif[:], tri_d[:, :], [b_c]); C.load(halo[:], halo_d[:, :], [b_c]); C.load(gate[:], pbc(gate_d, D), [b_c])
        C.cp(tri[:], trif[:], [b_c], [b_c])
        C.cp(trih[:], trif[:], [b_c], [b_c])
        C.ts(trih[:, 0:128], trif[:, 0:128], halo[:, 0:1], ALU.mult, [b_c], [b_c])
        C.memset(ones[:], 1.0, [b_c])
        qt = [C.sb([128, NT], BF16) for _ in range(2)]; kt = [C.sb([128, 4096], BF16) for _ in range(2)]
        vb = [C.sb([128, 4096], BF16) for _ in range(2)]; b_in = S.bufs(2)
        pt = [C.sb([128, 256], BF16) for _ in range(3)]; b_pt = S.bufs(3)
        accO = C.sb([128, NT]); accD = C.sb([128, NT]); b_acc = S.buf()
        otall = C.sb([128, 16 * NT], BF16); b_ot = S.buf()
        xt = [accD] * 2; b_x = [b_acc] * 2
        yt = accO; b_y = b_acc
        b_out = S.buf("out")
        npt = 0
        nbank = 0
        for h in range(16):
            for g in range(3):
                gh = g * 16 + h
                if gh >= ngh:
                    continue
                d = DIL[g]
                nb = 16 // d
                ip = (h * 3 + g) % 2
                C.load(qt[ip][:], QT[gh, :, :], [b_in[ip]])
                C.load(kt[ip][:, 0:NT + 128 * d], KT[gh, :, 0:NT + 128 * d], [b_in[ip]])
                C.load(vb[ip][:, 0:NT + 128 * d], Vb[gh, :, 0:NT + 128 * d], [b_in[ip]])
                for t4 in range(4):
                    bo = 2 + (nbank % 2)
                    bd = 4 + (nbank % 2)
                    nbank += 1
                    for tq in range(4):
                        tile = t4 * 4 + tq
                        r, n = tile // nb, tile % nb
                        kb_prev = r * (nb + 1) + n
                        kb_cur = kb_prev + 1
                        bs = tile % 2
                        pi = npt % 3
                        npt += 1
                        qv = qt[ip][:, tile * 128:(tile + 1) * 128]
                        C.mm(ps[bs][:, 0:128], kt[ip][:, kb_prev * 128:(kb_prev + 1) * 128], qv, True, True,
                             [b_in[ip]], [psb[bs]])
                        C.mm(ps[bs][:, 128:256], kt[ip][:, kb_cur * 128:(kb_cur + 1) * 128], qv, True, True,
                             [b_in[ip]], [psb[bs]])
                        C.act(pt[pi][:], ps[bs][:, 0:256], AF.Exp, [psb[bs]], [b_pt[pi]], scale=SCALE)
                        C.tt(pt[pi][:], pt[pi][:], (trih if n == 0 else tri)[:], ALU.mult, [b_pt[pi], b_c], [b_pt[pi]])
                        oc = slice(tq * 128, (tq + 1) * 128)
                        C.mm(ps[bo][:, oc], vb[ip][:, kb_prev * 128:(kb_prev + 1) * 128], pt[pi][:, 0:128], True, False,
                             [b_in[ip], b_pt[pi]], [psb[bo]])
                        C.mm(ps[bo][:, oc], vb[ip][:, kb_cur * 128:(kb_cur + 1) * 128], pt[pi][:, 128:256], False, True,
                             [b_in[ip], b_pt[pi]], [psb[bo]])
                        C.mm(ps[bd][:, oc], ones[:], pt[pi][:, 0:128], True, False, [b_c, b_pt[pi]], [psb[bd]])
                        C.mm(ps[bd][:, oc], ones[:], pt[pi][:, 128:256], False, True, [b_c, b_pt[pi]], [psb[bd]])
                    if d == 1:
                        dO = accO[:, t4 * 512:(t4 + 1) * 512]; dD = accD[:, t4 * 512:(t4 + 1) * 512]
                        sO = ps[bo][:]; sD = ps[bd][:]
                    elif d == 4:
                        r0 = t4
                        dO = V(accO[:, r0:r0 + 1], (4, 512)); dD = V(accD[:, r0:r0 + 1], (4, 512))
                        sO = ps[bo][:]; sD = ps[bd][:]
                    else:
                        r0 = t4 * 4
                        dO = V(accO[:, r0:r0 + 1], (1, 4), (16, 128)); dD = V(accD[:, r0:r0 + 1], (1, 4), (16, 128))
                        sO = ps[bo][:].rearrange("p (r i) -> p r i", r=4); sD = ps[bd][:].rearrange("p (r i) -> p r i", r=4)
                    if g == 0:
                        C.cp(dO, sO, [psb[bo]], [b_acc])
                        C.act(dD, sD, AF.Copy, [psb[bd]], [b_acc])
                    else:
                        C.tt(dO, dO, sO, ALU.add, [psb[bo], b_acc], [b_acc])
                        C.tt(dD, dD, sD, ALU.add, [psb[bd], b_acc], [b_acc])
            C.recip(accD[:], accD[:], [b_acc], [b_acc])
            C.tt(otall[:, h * NT:(h + 1) * NT], accO[:], accD[:], ALU.mult, [b_acc], [b_ot])
        for ti in range(NTILE):
            xp = ti % 2
            C.load(xt[xp][:], x[ti * 128:(ti + 1) * 128, :], [b_x[xp]])
            for nn in range(4):
                bk = 6 + nn % 2
                n0 = nn * 512
                for hh in range(16):
                    C.mm(ps[bk][:], otall[:, hh * NT + ti * 128:hh * NT + (ti + 1) * 128],
                         w_o_sb[:, hh * 2048 + n0:hh * 2048 + n0 + 512], hh == 0, hh == 15, [b_ot, b_w], [psb[bk]])
                C.tt(yt[:, n0:n0 + 512], ps[bk][:], gate[:, n0:n0 + 512], ALU.mult, [psb[bk], b_c], [b_y])
            C.tt(yt[:], yt[:], xt[xp][:], ALU.add, [b_y, b_x[xp]], [b_y])
            C.store(x1[ti * 128:(ti + 1) * 128, :], yt[:], [b_y], [b_out])
        S.finish([b_out])
    return C


def tri2_mask():
    a = np.arange(128)[:, None]
    qi = np.arange(128)[None, :]
    return np.concatenate([(a >= qi), (a <= qi)], axis=1).astype(np.float32)


def dil_exchange(p4res):
    maps = []
    for core in range(8):
        b, j = core // 4, core % 4
        Zc = np.asarray(p4res[core]["Z"]).reshape(NT, 3, 3, 16, 128)
        if j > 0:
            Zp = np.asarray(p4res[core - 1]["Z"]).reshape(NT, 3, 3, 16, 128)
        else:
            Zp = np.zeros_like(Zc)
        dt = Zc.dtype
        QT = np.zeros((48, 128, NT), dt); KT = np.zeros((48, 128, 4096), dt); Vb = np.zeros((48, 128, 4096), dt)
        for g, d in enumerate(DIL):
            nb = 16 // d
            def sub(Zx, which):
                a = Zx[:, g, which]
                return a.reshape(NT // d, d, 16, 128).transpose(1, 0, 2, 3).reshape(d, nb, 128, 16, 128)
            q = sub(Zc, 0); k = sub(Zc, 1); v = sub(Zc, 2)
            kp = sub(Zp, 1)[:, nb - 1:nb]; vp = sub(Zp, 2)[:, nb - 1:nb]
            kk = np.concatenate([kp, k], axis=1)
            vv = np.concatenate([vp, v], axis=1)
            QT[g * 16:(g + 1) * 16] = q.transpose(3, 4, 0, 1, 2).reshape(16, 128, NT)
            KT[g * 16:(g + 1) * 16, :, :NT + 128 * d] = kk.transpose(3, 4, 0, 1, 2).reshape(16, 128, NT + 128 * d)
            Vb[g * 16:(g + 1) * 16, :, :NT + 128 * d] = vv.transpose(3, 2, 0, 1, 4).reshape(16, 128, NT + 128 * d)
        halo = np.full((128, 1), 1.0 if j > 0 else 0.0, np.float32)
        maps.append(dict(QT=QT, KT=KT, Vb=Vb, halo=halo))
    return maps


def _peer_layer(inputs, layer, x_cores, mod):
    C = build_peer()
    pin = peer_inputs(inputs, layer)
    maps = []
    for core in range(8):
        b = core // 4
        m = mod[layer, b]
        d = dict(pin)
        d.update(x=x_cores[core], shift=row(m[6144:8192]), scale=row(m[8192:10240]), gate=row(m[10240:12288]),
                 g=row(inputs["norm_g"][layer, 1]), ident=IDENT)
        maps.append(d)
    res = run(C, maps)
    return [np.asarray(res[c]["x2"]) for c in range(8)]


def kernel(**inputs):
    inputs = {k: np.asarray(v) for k, v in inputs.items()}
    x = inputs["x"].astype(np.float32, copy=False)
    positions = inputs["positions"]
    mod = run_mod(inputs)
    x_cores = []
    for core in range(8):
        b, sl = core_tokens(core)
        x_cores.append(np.ascontiguousarray(x[b, sl]))
    f32 = lambda a: np.ascontiguousarray(np.asarray(a, np.float32))

    C = build_mla_pre()
    maps = []
    for core in range(8):
        b = core // 4
        m = mod[0, b]
        maps.append(dict(x=x_cores[core], pos=pos_layout(positions, core),
                         shift=row(m[0:2048]), scale=row(m[2048:4096]), g=row(inputs["norm_g"][0, 0]),
                         w_in=f32(inputs["mla_w_in"][0]), g_q=row(inputs["mla_g_q"][0]), w_uq=f32(inputs["mla_w_uq"][0]),
                         g_kv=row(inputs["mla_g_kv"][0]), w_ukv=f32(inputs["mla_w_ukv"][0]),
                         g_qn=row(inputs["mla_g_qn"][0]), g_kn=row(inputs["mla_g_kn"][0]),
                         invf=inv_freq(32), ident=IDENT))
    p1 = run(C, maps)
    ex = mla_exchange(p1)
    C = build_mla_attn()
    maps = []
    for core in range(8):
        b = core // 4
        m = mod[0, b]
        d = dict(ex[core])
        d.update(QT=np.asarray(p1[core]["QT"]), tri=tri_mask(), x=x_cores[core], gate=row(m[4096:6144]),
                 w_o=f32(inputs["mla_w_o"][0]), ident=IDENT)
        maps.append(d)
    res = run(C, maps)
    del p1, ex, maps
    x_cores = [np.asarray(res[c]["x1"]) for c in range(8)]
    x_cores = _peer_layer(inputs, 0, x_cores, mod)

    gqk = np.concatenate([np.asarray(inputs["dil_g_qn"][0], np.float32).reshape(-1),
                          np.asarray(inputs["dil_g_kn"][0], np.float32).reshape(-1)]).reshape(1, 768)
    C = build_dil_pre()
    maps = []
    for core in range(8):
        b = core // 4
        m = mod[1, b]
        maps.append(dict(x=x_cores[core], pos=pos_layout(positions, core),
                         shift=row(m[0:2048]), scale=row(m[2048:4096]), g=row(inputs["norm_g"][1, 0]),
                         w_in=f32(inputs["dil_w_in"][0]), gqk=gqk, invf=inv_freq(64), ident=IDENT))
    p4 = run(C, maps)
    ex = dil_exchange(p4)
    del p4
    C = build_dil_attn()
    maps = []
    for core in range(8):
        b = core // 4
        m = mod[1, b]
        d = dict(ex[core])
        d.update(tri2=tri2_mask(), x=x_cores[core], gate=row(m[4096:6144]), w_o=f32(inputs["dil_w_o"][0]), ident=IDENT)
        maps.append(d)
    res = run(C, maps)
    del ex, maps
    x_cores = [np.asarray(res[c]["x1"]) for c in range(8)]
    x_cores = _peer_layer(inputs, 1, x_cores, mod)

    out = np.zeros((2, SEQ, D), np.float32)
    for core in range(8):
        b, sl = core_tokens(core)
        out[b, sl] = x_cores[core]
    return out
```

```python
import numpy as np
import ml_dtypes
from contextlib import ExitStack
import concourse.bass as bass
import concourse.mybir as mybir
from concourse.bass_utils import run_bass_kernel_spmd

F32 = mybir.dt.float32
BF16 = mybir.dt.bfloat16
I32 = mybir.dt.int32
ALU = mybir.AluOpType
AF = mybir.ActivationFunctionType
AX = mybir.AxisListType
NPBF = ml_dtypes.bfloat16

D = 2048
NT = 2048
NTILE = 16
SEQ = 8192
EPS = 1e-6
TWO_PI = float(2 * np.pi)


class Eng:
    def __init__(self, S, name, handle):
        self.S = S
        self.name = name
        self.h = handle
        self.sem = S.es.enter_context(S.nc.semaphore("sem_" + name))
        self.count = 0
        self.seen = {}
        self.seen_dma = {}


class Buf:
    def __init__(self, S, name):
        self.S = S
        self.name = name
        self.w = None
        self.r = []
        self.dsem = None
        self.dval = 0
        self.excl = False

    def dma_sem(self):
        if self.dsem is None:
            self.dsem = self.S.es.enter_context(self.S.nc.semaphore("d_" + self.name))
        return self.dsem


class Sched:
    def __init__(self, nc, es):
        self.nc = nc
        self.es = es
        self.pe = Eng(self, "pe", nc.tensor)
        self.dve = Eng(self, "dve", nc.vector)
        self.act = Eng(self, "act", nc.scalar)
        self.pool = Eng(self, "pool", nc.gpsimd)
        self.sp = Eng(self, "sp", nc.sync)
        self.nbuf = 0

    def buf(self, name=None):
        self.nbuf += 1
        return Buf(self, name or ("b%d" % self.nbuf))

    def bufs(self, n):
        return [self.buf() for _ in range(n)]

    def _wait(self, X, ev):
        if ev is None:
            return
        if ev[0] == 'e':
            _, E, n = ev
            if X.seen.get(E.name, 0) >= n:
                return
            if E is X and n > E.count:
                return
            assert n <= E.count, "waiting on an un-signalled instruction of %s" % E.name
            X.h.wait_ge(E.sem, n)
            X.seen[E.name] = n
        else:
            _, sem, val, key = ev
            if X.seen_dma.get(key, 0) >= val:
                return
            X.h.wait_ge(sem, val)
            X.seen_dma[key] = val

    def _deps(self, X, reads, writes):
        for b in reads:
            self._wait(X, b.w)
            if b.excl:
                for ev in b.r:
                    if not (ev[0] == 'e' and ev[1] is X):
                        self._wait(X, ev)
        for b in writes:
            self._wait(X, b.w)
            for ev in b.r:
                self._wait(X, ev)

    def op(self, X, fn, reads=(), writes=(), signal=True):
        self._deps(X, reads, writes)
        ins = fn()
        if signal:
            X.count += 1
            ins.then_inc(X.sem, 1)
            ev = ('e', X, X.count)
        else:
            ev = ('e', X, X.count + 1)
        for b in reads:
            b.r.append(ev)
            if len(b.r) > 24:
                b.r = self._compact(b.r)
        for b in writes:
            b.w = ev
            b.r = []
        return ins

    @staticmethod
    def _compact(evs):
        last = {}
        for ev in evs:
            k = ('e', ev[1].name) if ev[0] == 'e' else ('d', ev[3])
            if k not in last or ev[2] > last[k][2]:
                last[k] = ev
        return list(last.values())

    def dma(self, Q, out, in_, reads=(), writes=(), **kw):
        self._deps(Q, reads, writes)
        owner = writes[0] if writes else reads[0]
        sem = owner.dma_sem()
        owner.dval += 16
        Q.h.dma_start(out=out, in_=in_, **kw).then_inc(sem, 16)
        ev = ('d', sem, owner.dval, owner.name)
        for b in reads:
            b.r.append(ev)
            if len(b.r) > 24:
                b.r = self._compact(b.r)
        for b in writes:
            b.w = ev
            b.r = []
        return ev

    def finish(self, bufs):
        for b in bufs:
            self._wait(self.sp, b.w)
            for ev in b.r:
                self._wait(self.sp, ev)


def V(ap, *dims):
    return bass.AP(ap.tensor, ap.offset, [list(ap.ap[0])] + [list(d) for d in dims])


def pbc(dram_ap_row, n, parts=128):
    return bass.AP(dram_ap_row.tensor, dram_ap_row.offset, [[0, parts], [1, n]])


class Ctx:
    def __init__(self):
        self.nc = bass.Bass("TRN2", target_bir_lowering=False)
        self.es = ExitStack()
        self.S = None
        self.n = 0

    def start(self):
        self.S = Sched(self.nc, self.es)
        self.ps = [self.es.enter_context(self.nc.psum_tensor("ps%d" % i, [128, 512], F32)) for i in range(8)]
        self.psb = [self.S.buf("psb%d" % i) for i in range(8)]
        for b in self.psb:
            b.excl = True

    def din(self, name, shape, dt=F32):
        return self.nc.dram_tensor(name, list(shape), dt, kind="ExternalInput").ap()

    def dout(self, name, shape, dt=F32):
        return self.nc.dram_tensor(name, list(shape), dt, kind="ExternalOutput").ap()

    def dint(self, name, shape, dt=F32):
        return self.nc.dram_tensor(name, list(shape), dt, kind="Internal").ap()

    def sb(self, shape, dt=F32, name=None):
        self.n += 1
        t = self.es.enter_context(self.nc.sbuf_tensor(name or ("t%d" % self.n), list(shape), dt))
        return t

    def act(self, out, in_, func, r, w, **kw):
        nc = self.nc
        return self.S.op(self.S.act, lambda: nc.scalar.activation(out=out, in_=in_, func=func, **kw), r, w)

    def tt(self, out, in0, in1, op, r, w, eng=None):
        e = eng or self.S.dve
        return self.S.op(e, lambda: e.h.tensor_tensor(out=out, in0=in0, in1=in1, op=op), r, w)

    def ts(self, out, in0, s1, op0, r, w, s2=None, op1=None, eng=None):
        e = eng or self.S.dve
        if op1 is None:
            return self.S.op(e, lambda: e.h.tensor_scalar(out=out, in0=in0, scalar1=s1, scalar2=None, op0=op0), r, w)
        return self.S.op(e, lambda: e.h.tensor_scalar(out=out, in0=in0, scalar1=s1, scalar2=s2, op0=op0, op1=op1), r, w)

    def stt(self, out, in0, scalar, in1, op0, op1, r, w, eng=None):
        e = eng or self.S.dve
        return self.S.op(e, lambda: e.h.scalar_tensor_tensor(out=out, in0=in0, scalar=scalar, in1=in1, op0=op0, op1=op1), r, w)

    def cp(self, out, in_, r, w, eng=None):
        e = eng or self.S.dve
        return self.S.op(e, lambda: e.h.tensor_copy(out=out, in_=in_), r, w)

    def red(self, out, in_, r, w, op=ALU.add):
        nc = self.nc
        return self.S.op(self.S.dve, lambda: nc.vector.tensor_reduce(out=out, in_=in_, axis=AX.X, op=op), r, w)

    def recip(self, out, in_, r, w):
        nc = self.nc
        return self.S.op(self.S.dve, lambda: nc.vector.reciprocal(out=out, in_=in_), r, w)

    def memset(self, ap, val, w, eng=None):
        e = eng or self.S.dve
        return self.S.op(e, lambda: e.h.memset(ap, val), (), w)

    def mm(self, out, lhsT, rhs, start, stop, r, w, sig=None):
        nc = self.nc
        if sig is None:
            sig = bool(stop)
        return self.S.op(self.S.pe, lambda: nc.tensor.matmul(out, lhsT=lhsT, rhs=rhs, start=start, stop=stop), r, w, signal=sig)

    def tr(self, out, in_, ident, r, w, sig=True):
        nc = self.nc
        return self.S.op(self.S.pe, lambda: nc.tensor.transpose(out, in_, ident), r, w, signal=sig)

    def load(self, out, in_, w, r=(), q=None, **kw):
        return self.S.dma(q or self.S.sp, out, in_, reads=r, writes=w, **kw)

    def store(self, out, in_, r, w, q=None, **kw):
        return self.S.dma(q or self.S.pool, out, in_, reads=r, writes=w, **kw)

    def rstd(self, out, ss, scale, r, w):
        self.act(out, ss, AF.Sqrt, list(r) + [self.b_eps], w, bias=self.eps_ap, scale=scale)
        self.recip(out, out, w, w)

    def consts(self, ident_dram):
        self.idf = self.sb([128, 128], F32, "idf")
        self.idb = self.sb([128, 128], BF16, "idb")
        self.b_id = self.S.buf("ident")
        self.load(self.idf[:], ident_dram[:, :], [self.b_id])
        self.cp(self.idb[:], self.idf[:], [self.b_id], [self.b_id])
        self.epst = self.sb([128, 1], F32, "epst")
        self.b_eps = self.S.buf("eps")
        self.memset(self.epst[:], EPS, [self.b_eps])
        self.eps_ap = self.epst[:, 0:1]


def run(ctx, in_maps):
    res = run_bass_kernel_spmd(ctx.nc, in_maps, core_ids=list(range(8)))
    if getattr(res, "exec_time_ns", None):
        print("[kernel] launch exec_time_ns", res.exec_time_ns)
    return res.results


IDENT = np.eye(128, dtype=np.float32)


def build_mod():
    C = Ctx()
    nc = C.nc
    NCOL = 6144
    ccol = C.din("ccol", [128, 16])
    W = C.din("W", [D, NCOL])
    bias = C.din("bias", [1, NCOL])
    out = C.dout("mod", [1, NCOL])
    with C.es:
        C.start()
        S = C.S
        ct = C.sb([128, 16]); sc = C.sb([128, 16])
        bt = C.sb([1, NCOL]); ot = C.sb([1, NCOL])
        slabs = [C.sb([128, 16 * 512]) for _ in range(2)]
        b_c, b_sc, b_b, b_o, b_out = S.bufs(5)
        b_sl = S.bufs(2)
        C.load(ct[:], ccol[:, :], [b_c])
        C.load(bt[:], bias[:, :], [b_b])
        C.act(sc[:], ct[:], AF.Silu, [b_c], [b_sc])
        Wv = W.rearrange("(k p) n -> p k n", p=128)
        for n in range(NCOL // 512):
            sl = slabs[n % 2]
            C.load(sl[:].rearrange("p (k n) -> p k n", k=16), Wv[:, :, n * 512:(n + 1) * 512], [b_sl[n % 2]])
            pb = n % 2
            for k in range(16):
                C.mm(C.ps[pb][0:1, :], sc[:, k:k + 1], sl[:, k * 512:(k + 1) * 512], k == 0, k == 15,
                     [b_sc, b_sl[n % 2]], [C.psb[pb]])
            C.tt(ot[:, n * 512:(n + 1) * 512], C.ps[pb][0:1, :], bt[:, n * 512:(n + 1) * 512], ALU.add,
                 [C.psb[pb], b_b], [b_o])
        C.store(out[:, :], ot[:], [b_o], [b_out])
        S.finish([b_out])
    return C


def run_mod(inputs):
    c = np.asarray(inputs["c"], np.float32)
    ada_w = np.asarray(inputs["ada_w"], np.float32)
    ada_b = np.asarray(inputs["ada_b"], np.float32)
    C = build_mod()
    maps = []
    for core in range(8):
        b, j = core // 4, core % 4
        cols = slice(6144 * j, 6144 * (j + 1))
        if j < 2:
            Wc = ada_w[0][:, 6144 * j:6144 * (j + 1)]
            bc = ada_b[0][6144 * j:6144 * (j + 1)]
        else:
            Wc = ada_w[1][:, 6144 * (j - 2):6144 * (j - 1)]
            bc = ada_b[1][6144 * (j - 2):6144 * (j - 1)]
        maps.append({"ccol": np.ascontiguousarray(c[b].reshape(16, 128).T),
                     "W": np.ascontiguousarray(Wc), "bias": np.ascontiguousarray(bc.reshape(1, -1))})
    res = run(C, maps)
    mod = np.zeros((2, 2, 12288), np.float32)
    for core in range(8):
        b, j = core // 4, core % 4
        l, jj = j // 2, j % 2
        mod[l, b, 6144 * jj:6144 * (jj + 1)] = res[core]["mod"][0]
    return mod


def rope_tables(C, pos_d, invf_d, half, b_out):
    S = C.S
    n = 16 * half
    posi = C.sb([128, 16], I32); posf = C.sb([128, 16])
    invf = C.sb([128, half])
    ang = C.sb([128, n]); kf = C.sb([128, n]); ki = C.sb([128, n], I32)
    cos = C.sb([128, n]); sin = C.sb([128, n])
    b_p, b_i, b_a, b_k = S.bufs(4)
    C.load(posi[:], pos_d[:, :], [b_p])
    C.load(invf[:], pbc(invf_d, half), [b_i])
    C.cp(posf[:], posi[:], [b_p], [b_p])
    C.tt(V(ang[:], (half, 16), (1, half)), V(posf[:], (1, 16), (0, half)), V(invf[:], (0, 16), (1, half)),
         ALU.mult, [b_p, b_i], [b_a])
    for (dst, shift) in ((sin, 0.0), (cos, float(np.pi / 2))):
        if shift != 0.0:
            C.ts(dst[:], ang[:], shift, ALU.add, [b_a], [b_out])
            src = dst
        else:
            src = ang
        C.ts(kf[:], src[:], float(1.0 / TWO_PI), ALU.mult, [b_a, b_out], [b_k])
        C.cp(ki[:], kf[:], [b_k], [b_k])
        C.cp(kf[:], ki[:], [b_k], [b_k])
        C.stt(dst[:], kf[:], -TWO_PI, src[:], ALU.mult, ALU.add, [b_k, b_a, b_out], [b_out])
        C.ts(dst[:], dst[:], float(np.pi), ALU.min, [b_out], [b_out], s2=float(-np.pi), op1=ALU.max)
        C.act(dst[:], dst[:], AF.Sin, [b_out], [b_out])
    return cos, sin


def rope_apply(C, out1, out2, x1, x2, cosv, sinv, ta, tb, r, w, b_t):
    C.tt(ta, x1, cosv, ALU.mult, r, [b_t])
    C.tt(tb, x2, sinv, ALU.mult, r, [b_t])
    C.tt(out1, ta, tb, ALU.subtract, [b_t], w)
    C.tt(ta, x2, cosv, ALU.mult, r, [b_t])
    C.tt(tb, x1, sinv, ALU.mult, r, [b_t])
    C.tt(out2, ta, tb, ALU.add, [b_t], w)


def load_w_bf16(C, dram_w, K, N, b):
    kc = K // 128
    t = C.sb([128, kc * N], BF16)
    src = dram_w.rearrange("(k p) n -> p k n", p=128)
    for k in range(kc):
        C.load(t[:, k * N:(k + 1) * N], src[:, k, :], [b], q=C.S.pool)
    return t


def norm_modulate_setup(C, g_d, scale_d, shift_d, tmp, b_t):
    S = C.S
    gmod = C.sb([128, D]); shiftb = C.sb([128, D])
    b_g, b_s = S.bufs(2)
    C.load(gmod[:], pbc(g_d, D), [b_g])
    C.load(tmp, pbc(scale_d, D), [b_t])
    C.load(shiftb[:], pbc(shift_d, D), [b_s])
    C.stt(gmod[:], tmp, 1.0, gmod[:], ALU.add, ALU.mult, [b_t, b_g], [b_g])
    return gmod, shiftb, b_g, b_s


def build_mla_pre(ntile=NTILE, dbg=0):
    C = Ctx()
    nc = C.nc
    x = C.din("x", [NT, D]); pos = C.din("pos", [128, 16], I32)
    shift_d = C.din("shift", [1, D]); scale_d = C.din("scale", [1, D]); g_d = C.din("g", [1, D])
    w_in = C.din("w_in", [D, 1088]); g_q = C.din("g_q", [1, 512]); w_uq = C.din("w_uq", [512, 3072])
    g_kv = C.din("g_kv", [1, 512]); w_ukv = C.din("w_ukv", [512, 4096])
    g_qn = C.din("g_qn", [1, 192]); g_kn = C.din("g_kn", [1, 192])
    invf = C.din("invf", [1, 32]); ident = C.din("ident", [128, 128])
    QT = C.dout("QT", [16, 192, NT], BF16); KT = C.dout("KT", [16, 192, NT], BF16)
    Vo = C.dout("V", [NT, D], BF16)
    with C.es:
        C.start()
        S = C.S
        C.consts(ident)
        b_w = S.buf("w")
        w_in_sb = load_w_bf16(C, w_in, D, 1088, b_w)
        w_uq_sb = load_w_bf16(C, w_uq, 512, 3072, b_w)
        w_ukv_sb = load_w_bf16(C, w_ukv, 512, 4096, b_w)
        junk = C.sb([128, 2048], BF16); b_junk = S.buf()
        tmpf = C.sb([128, 3072]); b_tmpf = S.buf()
        gmod, shiftb, b_g, b_s = norm_modulate_setup(C, g_d, scale_d, shift_d, tmpf[:, 0:D], b_tmpf)
        gq = C.sb([128, 512]); gkv = C.sb([128, 512]); gqn = C.sb([128, 192]); gkn = C.sb([128, 192])
        b_gs = S.buf("gains")
        for t_, d_, n_ in ((gq, g_q, 512), (gkv, g_kv, 512), (gqn, g_qn, 192), (gkn, g_kn, 192)):
            C.load(t_[:], pbc(d_, n_), [b_gs])
        b_cs = S.buf("cossin")
        cos, sin = rope_tables(C, pos, invf, 32, b_cs)

        xt = [C.sb([128, D]) for _ in range(2)]
        b_x = S.bufs(2)
        st = [C.sb([128, 64]) for _ in range(2)]; b_st = S.bufs(2)
        hb = [C.sb([128, D], BF16)] * 2; b_hb = [S.buf()] * 2
        hT = [C.sb([128, D], BF16)] * 2; b_hT = [S.buf()] * 2
        cn = [C.sb([128, 1024], BF16)] * 2; b_cn = [S.buf()] * 2
        cT = [C.sb([128, 1024], BF16)] * 2; b_cT = [S.buf()] * 2
        kr = [C.sb([128, 64]) for _ in range(2)]; b_kr = S.bufs(2)
        qsb = C.sb([128, 3072]); b_q = S.buf()
        qf = C.sb([128, 3072], BF16); b_qf = S.buf()
        knsb = qsb; b_kn = b_q
        kfn = qf; kfr = qf; b_kf = b_qf
        vsb = [C.sb([128, 2048], BF16)] * 2; b_v = [S.buf()] * 2
        krr = C.sb([128, 64]); ra = C.sb([128, 512]); rb = C.sb([128, 512]); b_r = S.buf()
        Tn = [C.sb([128, 2048], BF16)] * 2; Tr = [C.sb([64, 2048], BF16)] * 2
        b_T = [S.buf()] * 2
        b_QT, b_KT, b_V = S.bufs(3)
        ps, psb = C.ps, C.psb

        def ps_bf(i):
            return ps[i][:].bitcast(BF16)

        for i in range(ntile):
            p = i % 2
            s_ = st[p]
            C.load(xt[p][:], x[i * 128:(i + 1) * 128, :], [b_x[p]])
            if dbg == 1:
                continue
            C.memset(s_[:], 0.0, [b_st[p]])
            C.act(junk[:, 0:D], xt[p][:], AF.Square, [b_x[p]], [b_junk, b_st[p]], accum_out=s_[:, 0:1])
            C.rstd(s_[:, 1:2], s_[:, 0:1], 1.0 / D, [b_st[p]], [b_st[p]])
            C.stt(tmpf[:, 0:D], xt[p][:], s_[:, 1:2], gmod[:], ALU.mult, ALU.mult, [b_x[p], b_st[p], b_g], [b_tmpf])
            C.tt(hb[p][:], tmpf[:, 0:D], shiftb[:], ALU.add, [b_tmpf, b_s], [b_hb[p]])
            for k in range(16):
                bk = k // 8
                C.tr(ps_bf(bk)[:, (k % 8) * 128:(k % 8 + 1) * 128], hb[p][:, k * 128:(k + 1) * 128], C.idb[:],
                     [b_hb[p], C.b_id], [psb[bk]], sig=(k % 8 == 7))
            for bk in range(2):
                C.act(hT[p][:, bk * 1024:(bk + 1) * 1024], ps_bf(bk), AF.Copy, [psb[bk]], [b_hT[p]])
            if dbg == 2:
                continue
            for (bk, n0, n1) in ((2, 0, 512), (3, 512, 1024), (4, 1024, 1088)):
                for k in range(16):
                    C.mm(ps[bk][:, 0:n1 - n0], hT[p][:, k * 128:(k + 1) * 128],
                         w_in_sb[:, k * 1088 + n0:k * 1088 + n1], k == 0, k == 15, [b_hT[p], b_w], [psb[bk]])
            for (bk, col, gt) in ((2, 2, gq), (3, 4, gkv)):
                C.act(junk[:, 0:512], ps[bk][:], AF.Square, [psb[bk]], [b_junk, b_st[p]], accum_out=s_[:, col:col + 1])
                C.rstd(s_[:, col + 1:col + 2], s_[:, col:col + 1], 1.0 / 512, [b_st[p]], [b_st[p]])
                o = (bk - 2) * 512
                C.stt(cn[p][:, o:o + 512], ps[bk][:], s_[:, col + 1:col + 2], gt[:], ALU.mult, ALU.mult,
                      [psb[bk], b_st[p], b_gs], [b_cn[p]])
            C.act(kr[p][:], ps[4][:, 0:64], AF.Copy, [psb[4]], [b_kr[p]])
            for k in range(8):
                C.tr(ps_bf(5)[:, k * 128:(k + 1) * 128], cn[p][:, k * 128:(k + 1) * 128], C.idb[:],
                     [b_cn[p], C.b_id], [psb[5]], sig=(k == 7))
            C.act(cT[p][:], ps_bf(5), AF.Copy, [psb[5]], [b_cT[p]])
            if dbg == 3:
                continue
            for c in range(8):
                bk = 6 + c % 2
                for k in range(4):
                    C.mm(ps[bk][:, 0:384], cT[p][:, k * 128:(k + 1) * 128],
                         w_uq_sb[:, k * 3072 + c * 384:k * 3072 + (c + 1) * 384], k == 0, k == 3,
                         [b_cT[p], b_w], [psb[bk]])
                C.act(qsb[:, c * 384:(c + 1) * 384], ps[bk][:, 0:384], AF.Copy, [psb[bk]], [b_q])
            C.act(tmpf[:, 0:3072], qsb[:], AF.Square, [b_q], [b_tmpf])
            C.red(s_[:, 16:32], tmpf[:, 0:3072].rearrange("p (h d) -> p h d", h=16), [b_tmpf], [b_st[p]])
            C.rstd(s_[:, 16:32], s_[:, 16:32], 1.0 / 192, [b_st[p]], [b_st[p]])
            q3 = qsb[:].rearrange("p (h d) -> p h d", h=16)
            C.tt(q3, q3, V(s_[:, 16:17], (1, 16), (0, 192)), ALU.mult, [b_q, b_st[p]], [b_q])
            C.tt(q3, q3, V(gqn[:], (0, 16), (1, 192)), ALU.mult, [b_q, b_gs], [b_q])
            qf3 = qf[:].rearrange("p (h d) -> p h d", h=16)
            C.cp(qf3[:, :, 0:128], q3[:, :, 0:128], [b_q], [b_qf], eng=S.pool)
            cosv = V(cos[:, i * 32:i * 32 + 1], (0, 16), (1, 32)); sinv = V(sin[:, i * 32:i * 32 + 1], (0, 16), (1, 32))
            ra3 = V(ra[:], (32, 16), (1, 32)); rb3 = V(rb[:], (32, 16), (1, 32))
            rope_apply(C, qf3[:, :, 128:160], qf3[:, :, 160:192], q3[:, :, 128:160], q3[:, :, 160:192],
                       cosv, sinv, ra3, rb3, [b_q, b_cs], [b_qf], b_r)
            if dbg == 4:
                continue
            tp = i % 2
            for h in range(16):
                C.tr(ps_bf(h // 8)[:, (h % 8) * 128:(h % 8 + 1) * 128], qf3[:, h, 0:128], C.idb[:],
                     [b_qf, C.b_id], [psb[h // 8]], sig=False)
                C.tr(ps_bf(2 + h // 8)[0:64, (h % 8) * 128:(h % 8 + 1) * 128], qf3[:, h, 128:192], C.idb[:],
                     [b_qf, C.b_id], [psb[2 + h // 8]], sig=(h % 8 == 7))
            for bk in range(2):
                C.act(Tn[tp][:, bk * 1024:(bk + 1) * 1024], ps_bf(bk), AF.Copy, [psb[bk]], [b_T[tp]])
                C.cp(Tr[tp][:, bk * 1024:(bk + 1) * 1024], ps_bf(2 + bk)[0:64, :], [psb[2 + bk]], [b_T[tp]])
            C.store(QT[:, 0:128, i * 128:(i + 1) * 128].rearrange("h d t -> d h t"),
                    Tn[tp][:].rearrange("p (h t) -> p h t", h=16), [b_T[tp]], [b_QT])
            C.store(QT[:, 128:192, i * 128:(i + 1) * 128].rearrange("h d t -> d h t"),
                    Tr[tp][:].rearrange("p (h t) -> p h t", h=16), [b_T[tp]], [b_QT])
            if dbg == 5:
                continue
            kn3 = knsb[:, 0:2048].rearrange("p (h d) -> p h d", h=16)
            v3 = vsb[p][:].rearrange("p (h d) -> p h d", h=16)
            for c in range(8):
                bk = 6 + c % 2
                for k in range(4):
                    C.mm(ps[bk][:], cT[p][:, 512 + k * 128:512 + (k + 1) * 128],
                         w_ukv_sb[:, k * 4096 + c * 512:k * 4096 + (c + 1) * 512], k == 0, k == 3,
                         [b_cT[p], b_w], [psb[bk]])
                pv = ps[bk][:].rearrange("p (h d) -> p h d", h=2)
                C.act(kn3[:, 2 * c:2 * c + 2, :], pv[:, :, 0:128], AF.Copy, [psb[bk]], [b_kn])
                C.cp(v3[:, 2 * c:2 * c + 2, :], pv[:, :, 128:256], [psb[bk]], [b_v[p]])
            C.store(Vo[i * 128:(i + 1) * 128, :], vsb[p][:], [b_v[p]], [b_V])
            if dbg == 6:
                continue
            C.act(tmpf[:, 0:2048], knsb[:, 0:2048], AF.Square, [b_kn], [b_tmpf])
            C.red(s_[:, 32:48], tmpf[:, 0:2048].rearrange("p (h d) -> p h d", h=16), [b_tmpf], [b_st[p]])
            C.act(junk[:, 0:64], kr[p][:], AF.Square, [b_kr[p]], [b_junk, b_st[p]], accum_out=s_[:, 6:7])
            C.ts(s_[:, 32:48], s_[:, 32:48], s_[:, 6:7], ALU.add, [b_st[p]], [b_st[p]])
            C.rstd(s_[:, 32:48], s_[:, 32:48], 1.0 / 192, [b_st[p]], [b_st[p]])
            kfn3 = kfn[:, 0:2048].rearrange("p (h d) -> p h d", h=16)
            C.tt(kn3, kn3, V(s_[:, 32:33], (1, 16), (0, 128)), ALU.mult, [b_kn, b_st[p]], [b_kn])
            C.tt(kfn3, kn3, V(gkn[:], (0, 16), (1, 128)), ALU.mult, [b_kn, b_gs], [b_kf])
            C.tt(kr[p][:], kr[p][:], gkn[:, 128:192], ALU.mult, [b_kr[p], b_gs], [b_kr[p]])
            rope_apply(C, krr[:, 0:32], krr[:, 32:64], kr[p][:, 0:32], kr[p][:, 32:64],
                       cos[:, i * 32:(i + 1) * 32], sin[:, i * 32:(i + 1) * 32], ra[:, 0:32], rb[:, 0:32],
                       [b_kr[p], b_cs], [b_r], b_r)
            C.tt(V(kfr[:, 2048:2049], (64, 16), (1, 64)), V(krr[:], (0, 16), (1, 64)), V(s_[:, 32:33], (1, 16), (0, 64)),
                 ALU.mult, [b_r, b_st[p]], [b_kf])
            if dbg == 7:
                continue
            tp = (i + 1) % 2
            kfr3 = V(kfr[:, 2048:2049], (64, 16), (1, 64))
            for h in range(16):
                C.tr(ps_bf(h // 8)[:, (h % 8) * 128:(h % 8 + 1) * 128], kfn3[:, h, :], C.idb[:],
                     [b_kf, C.b_id], [psb[h // 8]], sig=False)
                C.tr(ps_bf(2 + h // 8)[0:64, (h % 8) * 128:(h % 8 + 1) * 128], kfr3[:, h, :], C.idb[:],
                     [b_kf, C.b_id], [psb[2 + h // 8]], sig=(h % 8 == 7))
            for bk in range(2):
                C.act(Tn[tp][:, bk * 1024:(bk + 1) * 1024], ps_bf(bk), AF.Copy, [psb[bk]], [b_T[tp]])
                C.cp(Tr[tp][:, bk * 1024:(bk + 1) * 1024], ps_bf(2 + bk)[0:64, :], [psb[2 + bk]], [b_T[tp]])
            C.store(KT[:, 0:128, i * 128:(i + 1) * 128].rearrange("h d t -> d h t"),
                    Tn[tp][:].rearrange("p (h t) -> p h t", h=16), [b_T[tp]], [b_KT])
            C.store(KT[:, 128:192, i * 128:(i + 1) * 128].rearrange("h d t -> d h t"),
                    Tr[tp][:].rearrange("p (h t) -> p h t", h=16), [b_T[tp]], [b_KT])
        S.finish([b_QT, b_KT, b_V])
    return C


def core_tokens(core):
    b, j = core // 4, core % 4
    return b, slice(NT * j, NT * (j + 1))


def pos_layout(positions, core):
    b, sl = core_tokens(core)
    return np.ascontiguousarray(np.asarray(positions[b, sl], np.int32).reshape(16, 128).T)


def inv_freq(half):
    return (10000.0 ** (-np.arange(half, dtype=np.float32) / np.float32(half))).astype(np.float32).reshape(1, half)


def row(v):
    return np.ascontiguousarray(np.asarray(v, np.float32).reshape(1, -1))


def build_mla_attn(nqb=4, nheads=16):
    C = Ctx()
    nc = C.nc
    QT = C.din("QT", [16, 192, NT], BF16); KT = C.din("KT", [16, 192, SEQ], BF16)
    Vp = C.din("Vp", [16, 128, 64 * 128], BF16)
    maskb_d = C.din("maskb", [128, 64]); tri_d = C.din("tri", [128, 128])
    x = C.din("x", [NT, D]); gate_d = C.din("gate", [1, D]); w_o = C.din("w_o", [D, D])
    ident = C.din("ident", [128, 128])
    x1 = C.dout("x1", [NT, D])
    SCALE = float(192 ** -0.5)
    with C.es:
        C.start()
        S = C.S
        ps, psb = C.ps, C.psb
        b_w = S.buf("w")
        w_o_sb = load_w_bf16(C, w_o, D, D, b_w)
        maskb = C.sb([128, 64]); trif = C.sb([128, 128]); trib = C.sb([128, 128], BF16)
        ones = C.sb([128, 128], BF16); gate = C.sb([128, D])
        b_c = S.buf("consts")
        C.load(maskb[:], maskb_d[:, :], [b_c]); C.load(trif[:], tri_d[:, :], [b_c])
        C.load(gate[:], pbc(gate_d, D), [b_c])
        C.cp(trib[:], trif[:], [b_c], [b_c])
        C.memset(ones[:], 1.0, [b_c])
        NS = 3
        ktn = [C.sb([128, 2048], BF16) for _ in range(NS)]; ktr = [C.sb([64, 2048], BF16) for _ in range(NS)]
        vs = [C.sb([128, 2048], BF16) for _ in range(NS)]; b_kv = S.bufs(NS)
        qn = [C.sb([128, 512], BF16) for _ in range(2)]; qr = [C.sb([64, 512], BF16) for _ in range(2)]; b_qq = S.bufs(2)
        pt = [C.sb([128, 512], BF16) for _ in range(3)]; b_pt = S.bufs(3)
        ot = C.sb([128, 16 * 512], BF16); b_ot = S.buf()
        rden = C.sb([128, 512]); b_rd = S.buf()
        xt = [C.sb([128, D]) for _ in range(2)]; b_x = S.bufs(2)
        yt = C.sb([128, D]); b_y = S.buf()
        b_out = S.buf("out")
        heads = [(qb, h) for qb in range(nqb) for h in range(nheads)]
        segs = []
        for hi, (qb, h) in enumerate(heads):
            nch = 48 + 4 * (qb + 1)
            for seg in range(4):
                c0 = seg * 16
                segs.append(dict(hi=hi, qb=qb, h=h, c0=c0, c1=min(nch, c0 + 16), nch=nch, idx=len(segs)))

        def load_q(hi):
            if hi >= len(heads):
                return
            qb, h = heads[hi]
            qp = hi % 2
            C.load(qn[qp][:], QT[h, 0:128, qb * 512:(qb + 1) * 512], [b_qq[qp]])
            C.load(qr[qp][:], QT[h, 128:192, qb * 512:(qb + 1) * 512], [b_qq[qp]])

        def load_seg(si):
            if si >= len(segs):
                return
            sg = segs[si]
            sl = si % NS
            nk = (sg["c1"] - sg["c0"]) * 128
            k0 = sg["c0"] * 128
            C.load(ktn[sl][:, 0:nk], KT[sg["h"], 0:128, k0:k0 + nk], [b_kv[sl]])
            C.load(ktr[sl][:, 0:nk], KT[sg["h"], 128:192, k0:k0 + nk], [b_kv[sl]])
            C.load(vs[sl][:, 0:nk], Vp[sg["h"], :, k0:k0 + nk], [b_kv[sl]])

        chunks = []
        for sg in segs:
            for c in range(sg["c0"], sg["c1"]):
                chunks.append(dict(sg=sg, c=c, n=len(chunks)))

        def emit_s(ch):
            sg = ch["sg"]; c = ch["c"]
            hi = sg["hi"]; qp = hi % 2; sl = sg["idx"] % NS
            lc = c - sg["c0"]
            dc = c - (sg["nch"] - 4)
            q0 = 128 * dc if dc > 0 else 0
            bs = ch["n"] % 2
            C.mm(ps[bs][:, q0:512], ktn[sl][:, lc * 128:(lc + 1) * 128], qn[qp][:, q0:512], True, False,
                 [b_kv[sl], b_qq[qp]], [psb[bs]])
            C.mm(ps[bs][:, q0:512], ktr[sl][:, lc * 128:(lc + 1) * 128], qr[qp][:, q0:512], False, True,
                 [b_kv[sl], b_qq[qp]], [psb[bs]])

        def emit_rest(ch):
            sg = ch["sg"]; c = ch["c"]
            hi = sg["hi"]; qp = hi % 2; sl = sg["idx"] % NS
            nch = sg["nch"]; h = sg["h"]; qb = sg["qb"]
            lc = c - sg["c0"]
            dc = c - (nch - 4)
            q0 = 128 * dc if dc > 0 else 0
            bs = ch["n"] % 2
            pi = ch["n"] % 3
            bo = 2 + qp
            bd = 4 + qp
            C.act(pt[pi][:, q0:512], ps[bs][:, q0:512], AF.Exp, [psb[bs], b_c], [b_pt[pi]],
                  bias=maskb[:, c:c + 1], scale=SCALE)
            if dc >= 0:
                C.tt(pt[pi][:, q0:q0 + 128], pt[pi][:, q0:q0 + 128], trib[:], ALU.mult,
                     [b_pt[pi], b_c], [b_pt[pi]])
            C.mm(ps[bo][:, q0:512], vs[sl][:, lc * 128:(lc + 1) * 128], pt[pi][:, q0:512], c == 0, c == nch - 1,
                 [b_kv[sl], b_pt[pi]], [psb[bo]], sig=False)
            C.mm(ps[bd][:, q0:512], ones[:], pt[pi][:, q0:512], c == 0, c == nch - 1,
                 [b_c, b_pt[pi]], [psb[bd]], sig=True)
            if c == nch - 1:
                C.recip(rden[:], ps[bd][:], [psb[bd]], [b_rd])
                C.tt(ot[:, h * 512:(h + 1) * 512], ps[bo][:], rden[:], ALU.mult, [psb[bo], b_rd], [b_ot])
                if h == nheads - 1:
                    emit_wo(qb)

        def emit_wo(qb):
            for t4 in range(4):
                ti = qb * 4 + t4
                xp = ti % 2
                C.load(xt[xp][:], x[ti * 128:(ti + 1) * 128, :], [b_x[xp]])
                for half in range(2):
                    for nn in range(2):
                        bk = 6 + nn
                        n0 = half * 1024 + nn * 512
                        for hh in range(nheads):
                            C.mm(ps[bk][:], ot[:, hh * 512 + t4 * 128:hh * 512 + (t4 + 1) * 128],
                                 w_o_sb[:, hh * 2048 + n0:hh * 2048 + n0 + 512], hh == 0, hh == nheads - 1,
                                 [b_ot, b_w], [psb[bk]])
                        C.tt(yt[:, n0:n0 + 512], ps[bk][:], gate[:, n0:n0 + 512], ALU.mult, [psb[bk], b_c], [b_y])
                C.tt(yt[:], yt[:], xt[xp][:], ALU.add, [b_y, b_x[xp]], [b_y])
                C.store(x1[ti * 128:(ti + 1) * 128, :], yt[:], [b_y], [b_out])
        load_q(0)
        load_seg(0)
        load_seg(1)
        for i in range(len(chunks) + 1):
            if i < len(chunks):
                emit_s(chunks[i])
            if i >= 1:
                emit_rest(chunks[i - 1])
            if i < len(chunks) and chunks[i]["c"] == chunks[i]["sg"]["c0"]:
                load_seg(chunks[i]["sg"]["idx"] + 2)
                if chunks[i]["c"] == 0:
                    load_q(chunks[i]["sg"]["hi"] + 1)
        S.finish([b_out])
    return C


def tri_mask():
    p = np.arange(128)[:, None]
    i = np.arange(128)[None, :]
    return (p <= i).astype(np.float32)


def mla_exchange(p1res):
    maps = []
    for core in range(8):
        b, j = core // 4, core % 4
        KT_all = np.concatenate([np.asarray(p1res[b * 4 + jj]["KT"]) for jj in range(4)], axis=2)
        V_all = np.concatenate([np.asarray(p1res[b * 4 + jj]["V"]) for jj in range(4)], axis=0)
        nvalid = NT * (j + 1)
        KTp = np.zeros((16, 192, SEQ), dtype=KT_all.dtype)
        KTp[:, :, SEQ - nvalid:] = KT_all[:, :, :nvalid]
        Vs = np.zeros((SEQ, D), dtype=V_all.dtype)
        Vs[SEQ - nvalid:] = V_all[:nvalid]
        Vp = np.ascontiguousarray(Vs.reshape(64, 128, 16, 128).transpose(2, 1, 0, 3)).reshape(16, 128, 64 * 128)
        maskb = np.zeros((128, 64), np.float32)
        maskb[:, :(SEQ - nvalid) // 128] = -30000.0
        maps.append(dict(KT=KTp, Vp=Vp, maskb=maskb))
    return maps


def fence(old, new):
    evs = []
    for b in old:
        if b.w is not None:
            evs.append(b.w)
        evs.extend(b.r)
    evs = Sched._compact(evs) if evs else []
    for b in new:
        b.w = None
        b.r = list(evs)


def build_peer(ntb=4, ngroups=32):
    C = Ctx()
    nc = C.nc
    x = C.din("x", [NT, D])
    shift_d = C.din("shift", [1, D]); scale_d = C.din("scale", [1, D]); gate_d = C.din("gate", [1, D]); g_d = C.din("g", [1, D])
    w_q = C.din("w_q", [D, D]); skT_d = C.din("skT", [128, 16 * 128])
    UT = C.din("UT", [D, 16384]); Vt = C.din("Vt", [16384, D])
    ident = C.din("ident", [128, 128])
    out = C.dout("x2", [NT, D])
    MASK_T = 1.0 - 2e-4
    with C.es:
        S = Sched(nc, C.es)
        C.S = S
        psY = C.es.enter_context(nc.psum_tensor("psY", [128, 2048], F32)); b_psY = S.buf("psY"); b_psY.excl = True
        ps = [C.es.enter_context(nc.psum_tensor("psq%d" % i, [128, 512], F32)) for i in range(4)]
        psb = S.bufs(4)
        for b in psb:
            b.excl = True
        psYb = [psY[:, i * 512:(i + 1) * 512] for i in range(4)]
        C.consts(ident)
        yacc = [C.sb([128, D]) for _ in range(4)]; b_ya = S.bufs(4)
        hTb = C.sb([128, 16 * 512], BF16); b_hTb = S.buf()
        a1 = [C.sb([128, 1024]) for _ in range(4)]; a2 = [C.sb([128, 1024]) for _ in range(4)]; b_a = S.bufs(4)
        diag = C.sb([128, 32 * 128], BF16); b_dg = S.buf()
        st = C.sb([128, 64]); b_st = S.buf()
        AW = 29696
        arena = C.sb([128, AW])
        o = 0
        def carve(n, dt=F32):
            nonlocal o
            v = arena[:, o:o + n]
            o += n
            return v.bitcast(dt) if dt != F32 else v
        gmod = carve(2048); shiftb = carve(2048); xt = carve(2048); h2 = carve(2048)
        hTf = carve(8192); wq = [carve(2048), carve(2048)]; qTc = [carve(512), carve(512)]
        s_sb = [carve(512), carve(512)]
        skT = carve(2048); tk = carve(256); cand = carve(512); junk = carve(1024, BF16)
        assert o <= AW, o
        pb_g, pb_x, pb_h2, pb_hTf, pb_s = S.bufs(5)
        pb_wq = S.bufs(2); pb_qT = S.bufs(2)
        b_sk, b_tk, b_cd, b_junk = S.bufs(4)
        pro_bufs = [pb_g, pb_x, pb_h2, pb_hTf, pb_s, b_sk, b_tk, b_cd, b_junk] + pb_wq + pb_qT
        o = 0
        utg = [carve(4096, BF16), carve(4096, BF16)]
        vg = [carve(4096, BF16), carve(4096, BF16)]
        Pp = carve(2048)
        Mp = [carve(1024, BF16) for _ in range(4)]
        gel = [carve(512), carve(512)]
        actT = [carve(1024, BF16), carve(1024, BF16)]
        ytmp = [carve(2048), carve(2048)]
        assert o <= AW, o
        mb_ut = S.bufs(2); mb_v = S.bufs(2); mb_P = S.buf(); mb_M = S.bufs(4); mb_gel = S.bufs(2)
        mb_act = S.bufs(2); mb_yt = S.bufs(2)
        main_bufs = mb_ut + mb_v + [mb_P] + mb_M + mb_gel + mb_act + mb_yt
        o = 0
        gateb = carve(2048); ext = [carve(2048), carve(2048)]; eo = [carve(2048), carve(2048)]
        eb_g = S.buf(); eb_x = S.bufs(2); eb_o = S.bufs(2)
        epi_bufs = [eb_g] + eb_x + eb_o
        b_out = S.buf("out")
        UTv = UT.rearrange("(k p) e -> p k e", p=128)
        wqv = w_q.rearrange("(k p) n -> p k n", p=128)
        a1v = [t[:] for t in a1]; a2v = [t[:] for t in a2]

        for tb in range(ntb):
            fence(epi_bufs + main_bufs, pro_bufs)
            C.load(gmod, pbc(g_d, D), [pb_g]); C.load(h2, pbc(scale_d, D), [pb_h2]); C.load(shiftb, pbc(shift_d, D), [pb_g])
            C.load(skT, skT_d[:, :], [b_sk])
            C.stt(gmod, h2, 1.0, gmod, ALU.add, ALU.mult, [pb_h2, pb_g], [pb_g])
            for tt in range(4):
                ti = tb * 4 + tt
                C.load(xt, x[ti * 128:(ti + 1) * 128, :], [pb_x])
                C.memset(st[:, 0:1], 0.0, [b_st])
                C.act(junk, xt, AF.Square, [pb_x], [b_junk, b_st], accum_out=st[:, 0:1])
                C.rstd(st[:, 1:2], st[:, 0:1], 1.0 / D, [b_st], [b_st])
                C.stt(h2, xt, st[:, 1:2], gmod, ALU.mult, ALU.mult, [pb_x, b_st, pb_g], [pb_h2])
                C.tt(h2, h2, shiftb, ALU.add, [pb_h2, pb_g], [pb_h2])
                for bk in range(4):
                    for kk in range(4):
                        k = bk * 4 + kk
                        C.tr(ps[bk][:, kk * 128:(kk + 1) * 128], h2[:, k * 128:(k + 1) * 128], C.idf[:],
                             [pb_h2, C.b_id], [psb[bk]], sig=(kk == 3))
                    src = ps[bk][:].rearrange("p (k t) -> p k t", k=4)
                    C.act(V(hTf[:, bk * 4 * 512 + tt * 128:bk * 4 * 512 + tt * 128 + 1], (512, 4), (1, 128)), src, AF.Copy,
                          [psb[bk]], [pb_hTf])
                    C.cp(V(hTb[:, bk * 4 * 512 + tt * 128:bk * 4 * 512 + tt * 128 + 1], (512, 4), (1, 128)), src,
                         [psb[bk]], [b_hTb])
            for cc in range(16):
                hd, pp = cc // 2, cc % 2
                w = wq[cc % 2]
                C.load(w.rearrange("p (k n) -> p k n", k=16), wqv[:, :, cc * 128:(cc + 1) * 128], [pb_wq[cc % 2]])
                bq = cc % 2
                for k in range(16):
                    C.mm(ps[bq][:], w[:, k * 128:(k + 1) * 128], hTf[:, k * 512:(k + 1) * 512], k == 0, k == 15,
                         [pb_wq[cc % 2], pb_hTf], [psb[bq]])
                C.act(qTc[cc % 2], ps[bq][:], AF.Copy, [psb[bq]], [pb_qT[cc % 2]])
                bs = 2 + pp
                for tt in range(4):
                    C.mm(ps[bs][:, tt * 128:(tt + 1) * 128], qTc[cc % 2][:, tt * 128:(tt + 1) * 128],
                         skT[:, cc * 128:(cc + 1) * 128], True, True, [pb_qT[cc % 2], b_sk], [psb[bs]], sig=(tt == 3))
                C.act(s_sb[pp], ps[bs][:], AF.Copy, [psb[bs]], [pb_s])
                if pp == 0:
                    continue
                for tt in range(4):
                    T = tk[:, tt * 64:(tt + 1) * 64]
                    for half in range(2):
                        sv = s_sb[half][:, tt * 128:(tt + 1) * 128]
                        C.S.op(S.dve, lambda: nc.vector.max(out=T[:, half * 16:half * 16 + 8], in_=sv), [pb_s], [b_tk])
                        C.S.op(S.dve, lambda: nc.vector.match_replace(out=cand[:, 0:128], in_to_replace=T[:, half * 16:half * 16 + 8],
                                                                      in_values=sv, imm_value=-1e30), [pb_s, b_tk], [b_cd])
                        C.S.op(S.dve, lambda: nc.vector.max(out=T[:, half * 16 + 8:half * 16 + 16], in_=cand[:, 0:128]), [b_cd], [b_tk])
                    C.tt(V(cand[:, 0:1], (16, 16), (1, 16)), V(T[:, 0:1], (1, 16), (0, 16)), V(T[:, 16:17], (0, 16), (1, 16)),
                         ALU.add, [b_tk], [b_cd])
                    C.S.op(S.dve, lambda: nc.vector.max(out=T[:, 32:40], in_=cand[:, 0:256]), [b_cd], [b_tk])
                    C.S.op(S.dve, lambda: nc.vector.match_replace(out=cand[:, 256:512], in_to_replace=T[:, 32:40],
                                                                  in_values=cand[:, 0:256], imm_value=-1e30), [b_cd, b_tk], [b_cd])
                    C.S.op(S.dve, lambda: nc.vector.max(out=T[:, 40:48], in_=cand[:, 256:512]), [b_cd], [b_tk])
                    C.ts(T[:, 48:49], T[:, 47:48], -1.0, ALU.mult, [b_tk], [b_tk])
                    C.ts(T[:, 49:50], T[:, 47:48], -0.5, ALU.mult, [b_tk], [b_tk])
                    C.memset(T[:, 50:51], 0.0, [b_tk])
                    C.act(T[:, 52:64][:, 0:12], T[:, 32:44], AF.Exp, [b_tk], [b_tk], bias=T[:, 48:49], scale=1.0)
                    C.red(T[:, 50:51], T[:, 52:64], [b_tk], [b_tk])
                    C.act(T[:, 52:56], T[:, 44:48], AF.Exp, [b_tk], [b_tk], bias=T[:, 48:49], scale=1.0)
                    C.red(T[:, 51:52], T[:, 52:56], [b_tk], [b_tk])
                    C.tt(T[:, 50:51], T[:, 50:51], T[:, 51:52], ALU.add, [b_tk], [b_tk])
                    C.recip(T[:, 51:52], T[:, 50:51], [b_tk], [b_tk])
                    di = tt * 8 + hd
                    C.ts(diag[:, di * 128:(di + 1) * 128], C.idf[:], T[:, 51:52], ALU.mult, [C.b_id, b_tk], [b_dg])
                    C.act(a1[tt][:, hd * 128:(hd + 1) * 128], s_sb[0][:, tt * 128:(tt + 1) * 128], AF.Exp, [pb_s, b_tk], [b_a[tt]],
                          bias=T[:, 49:50], scale=1.0)
                    C.act(a2[tt][:, hd * 128:(hd + 1) * 128], s_sb[1][:, tt * 128:(tt + 1) * 128], AF.Exp, [pb_s, b_tk], [b_a[tt]],
                          bias=T[:, 49:50], scale=1.0)
            fence(pro_bufs, main_bufs)
            for tt in range(4):
                C.memset(yacc[tt][:], 0.0, [b_ya[tt]], eng=S.pool)
            def load_ut(g):
                if g < ngroups:
                    C.load(utg[g % 2].rearrange("p (k e) -> p k e", k=16), UTv[:, :, g * 512:(g + 1) * 512], [mb_ut[g % 2]], q=S.pool)

            def load_v(g):
                if g < ngroups:
                    C.load(vg[g % 2].rearrange("p (c d) -> p c d", c=4),
                           Vt[g * 512:(g + 1) * 512, :].rearrange("(c p) d -> p c d", p=128), [mb_v[g % 2]], q=S.pool)

            def emit_A(g, sg):
                sl = g % 2
                for c2 in range(2):
                    c = sg * 2 + c2
                    for k in range(16):
                        C.mm(ps[c2][:], utg[sl][:, k * 512 + c * 128:k * 512 + (c + 1) * 128], hTb[:, k * 512:(k + 1) * 512],
                             k == 0, k == 15, [mb_ut[sl], b_hTb], [psb[c2]])

            def emit_gelu(g, sg):
                for c2 in range(2):
                    C.act(gel[c2], ps[c2][:], AF.Gelu, [psb[c2]], [mb_gel[c2]])

            def emit_actT(g, sg):
                at = actT[g % 2]
                for c2 in range(2):
                    c = sg * 2 + c2
                    C.tt(at[:, c * 512:(c + 1) * 512], gel[c2], ps[2 + c2][:], ALU.mult, [mb_gel[c2], psb[2 + c2]], [mb_act[g % 2]])

            def emit_G(g, sg, prev, hook=None):
                i1 = g * 4 + sg * 2
                for tt in range(4):
                    mi = tt
                    P3 = V(Pp[:, 0:1], (256, 8), (128, 2), (1, 128))
                    C.tt(P3, V(a1v[tt][:, i1:i1 + 1], (128, 8), (1, 2), (0, 128)), V(a2v[tt][:, 0:1], (128, 8), (0, 2), (1, 128)),
                         ALU.mult, [b_a[tt]], [mb_P])
                    C.stt(Mp[mi], Pp, MASK_T, Pp, ALU.is_ge, ALU.mult, [mb_P], [mb_M[mi]])
                    if tt == 0:
                        continue
                    if tt == 1:
                        if prev is not None:
                            emit_actT(*prev)
                        emit_gelu(g, sg)
                        if hook is not None:
                            hook()
                    for t2 in ((0, 1) if tt == 1 else (tt,)):
                        for c2 in range(2):
                            for hd in range(8):
                                di = t2 * 8 + hd
                                C.mm(ps[2 + c2][:, t2 * 128:(t2 + 1) * 128], Mp[t2][:, hd * 256 + c2 * 128:hd * 256 + (c2 + 1) * 128],
                                     diag[:, di * 128:(di + 1) * 128], hd == 0, hd == 7, [mb_M[t2], b_dg], [psb[2 + c2]],
                                     sig=(c2 == 1 and hd == 7))

            ny = [0]

            def emit_S2(g, tt):
                if g < 0:
                    return
                sl = g % 2
                at = actT[g % 2]
                for dn in range(4):
                    for c in range(4):
                        C.mm(psYb[dn], at[:, c * 512 + tt * 128:c * 512 + (tt + 1) * 128],
                             vg[sl][:, c * 2048 + dn * 512:c * 2048 + (dn + 1) * 512], c == 0, c == 3,
                             [mb_act[g % 2], mb_v[sl]], [b_psY], sig=(dn == 3 and c == 3))
                yi = ny[0] % 2
                ny[0] += 1
                C.act(ytmp[yi], psY[:], AF.Copy, [b_psY], [mb_yt[yi]])
                C.tt(yacc[tt][:], yacc[tt][:], ytmp[yi], ALU.add, [mb_yt[yi], b_ya[tt]], [b_ya[tt]], eng=S.pool)

            load_ut(0); load_v(0); load_ut(1)
            prev = None
            for g in range(ngroups):
                emit_A(g, 0)
                emit_G(g, 0, prev, hook=lambda: emit_S2(g - 1, 0))
                prev = (g, 0)
                emit_S2(g - 1, 1)
                emit_A(g, 1)
                emit_S2(g - 1, 2)
                emit_G(g, 1, prev)
                prev = (g, 1)
                emit_S2(g - 1, 3)
                load_v(g + 1)
                load_ut(g + 2)
            emit_actT(*prev)
            for tt in range(4):
                emit_S2(ngroups - 1, tt)
            fence(main_bufs, epi_bufs)
            C.load(gateb, pbc(gate_d, D), [eb_g])
            for tt in range(4):
                ti = tb * 4 + tt
                C.load(ext[tt % 2], x[ti * 128:(ti + 1) * 128, :], [eb_x[tt % 2]])
                C.tt(eo[tt % 2], yacc[tt][:], gateb, ALU.mult, [b_ya[tt], eb_g], [eb_o[tt % 2]])
                C.tt(eo[tt % 2], eo[tt % 2], ext[tt % 2], ALU.add, [eb_o[tt % 2], eb_x[tt % 2]], [eb_o[tt % 2]])
                C.store(out[ti * 128:(ti + 1) * 128, :], eo[tt % 2], [eb_o[tt % 2]], [b_out])
        S.finish([b_out])
    return C


def peer_inputs(inputs, layer):
    sk = np.asarray(inputs["peer_sub_keys"][layer], np.float32)
    skT = np.ascontiguousarray(sk.reshape(16, 128, 128).transpose(2, 0, 1)).reshape(128, 16 * 128)
    UT = np.ascontiguousarray(np.asarray(inputs["peer_u"][layer], np.float32).T)
    Vt = np.ascontiguousarray(np.asarray(inputs["peer_v"][layer], np.float32))
    return dict(skT=skT, UT=UT, Vt=Vt, w_q=np.ascontiguousarray(np.asarray(inputs["peer_w_q"][layer], np.float32)))


def build_dil_pre(nchunks=36):
    C = Ctx()
    nc = C.nc
    x = C.din("x", [NT, D]); pos = C.din("pos", [128, 16], I32)
    shift_d = C.din("shift", [1, D]); scale_d = C.din("scale", [1, D]); g_d = C.din("g", [1, D])
    w_in = C.din("w_in", [D, 18432]); gqk_d = C.din("gqk", [1, 6 * 128])
    invf = C.din("invf", [1, 64]); ident = C.din("ident", [128, 128])
    Z = C.dout("Z", [NT, 18432], BF16)
    with C.es:
        C.start()
        S = C.S
        ps, psb = C.ps, C.psb
        C.consts(ident)
        tmpf = C.sb([128, D]); b_tmpf = S.buf()
        gmod, shiftb, b_g, b_s = norm_modulate_setup(C, g_d, scale_d, shift_d, tmpf[:], b_tmpf)
        gqk = C.sb([128, 768]); b_gs = S.buf()
        C.load(gqk[:], pbc(gqk_d, 768), [b_gs])
        b_cs = S.buf("cossin")
        cos, sin = rope_tables(C, pos, invf, 64, b_cs)
        hT = C.sb([128, 16 * NT], BF16); b_hT = S.buf()
        xt = [C.sb([128, D]) for _ in range(2)]; b_x = S.bufs(2)
        junk = C.sb([128, D], BF16); b_junk = S.buf()
        hb = C.sb([128, D], BF16); b_hb = S.buf()
        st = C.sb([128, 16]); b_st = S.buf()
        wch = [C.sb([128, 16 * 512], BF16) for _ in range(2)]; b_w = S.bufs(2)
        zn = C.sb([128, 512]); b_zn = S.buf()
        ra = C.sb([128, 256]); rb = C.sb([128, 256]); b_r = S.buf()
        ot = [C.sb([128, 512], BF16) for _ in range(3)]; b_o = S.bufs(3)
        b_Z = S.buf("Z")

        def ps_bf(i):
            return ps[i][:].bitcast(BF16)

        hT3 = hT[:].rearrange("p (k t) -> p k t", k=16)
        for i in range(NTILE):
            p = i % 2
            C.load(xt[p][:], x[i * 128:(i + 1) * 128, :], [b_x[p]])
            C.memset(st[:, 0:1], 0.0, [b_st])
            C.act(junk[:], xt[p][:], AF.Square, [b_x[p]], [b_junk, b_st], accum_out=st[:, 0:1])
            C.rstd(st[:, 1:2], st[:, 0:1], 1.0 / D, [b_st], [b_st])
            C.stt(tmpf[:], xt[p][:], st[:, 1:2], gmod[:], ALU.mult, ALU.mult, [b_x[p], b_st, b_g], [b_tmpf])
            C.tt(hb[:], tmpf[:], shiftb[:], ALU.add, [b_tmpf, b_s], [b_hb])
            for k in range(16):
                bk = k // 8
                C.tr(ps_bf(bk)[:, (k % 8) * 128:(k % 8 + 1) * 128], hb[:, k * 128:(k + 1) * 128], C.idb[:],
                     [b_hb, C.b_id], [psb[bk]], sig=(k % 8 == 7))
            for bk in range(2):
                C.act(hT3[:, bk * 8:(bk + 1) * 8, i * 128:(i + 1) * 128],
                      ps_bf(bk).rearrange("p (k t) -> p k t", k=8), AF.Copy, [psb[bk]], [b_hT])
        wv = w_in.rearrange("(k p) n -> p k n", p=128)
        no = 0
        for c in range(nchunks):
            blk = c // 4
            g, r = blk // 3, blk % 3
            w = wch[c % 2]
            C.load(w[:].rearrange("p (k n) -> p k n", k=16), wv[:, :, c * 512:(c + 1) * 512], [b_w[c % 2]], q=S.pool)
            for i in range(NTILE):
                bk = 2 + (c * NTILE + i) % 6
                for k in range(16):
                    C.mm(ps[bk][:], hT[:, k * NT + i * 128:k * NT + (i + 1) * 128], w[:, k * 512:(k + 1) * 512],
                         k == 0, k == 15, [b_hT, b_w[c % 2]], [psb[bk]])
                oi = no % 3
                no += 1
                o_ = ot[oi]
                if r == 2:
                    C.act(o_[:], ps[bk][:], AF.Copy, [psb[bk]], [b_o[oi]])
                else:
                    gain = gqk[:, (r * 3 + g) * 128:(r * 3 + g + 1) * 128]
                    C.act(junk[:, 0:512], ps[bk][:], AF.Square, [psb[bk]], [b_junk])
                    C.red(st[:, 4:8], junk[:, 0:512].rearrange("p (h d) -> p h d", h=4), [b_junk], [b_st])
                    C.rstd(st[:, 4:8], st[:, 4:8], 1.0 / 128, [b_st], [b_st])
                    z3 = zn[:].rearrange("p (h d) -> p h d", h=4)
                    C.tt(z3, ps[bk][:].rearrange("p (h d) -> p h d", h=4), V(st[:, 4:5], (1, 4), (0, 128)), ALU.mult,
                         [psb[bk], b_st], [b_zn])
                    C.tt(z3, z3, V(gain, (0, 4), (1, 128)), ALU.mult, [b_zn, b_gs], [b_zn])
                    o3 = o_[:].rearrange("p (h d) -> p h d", h=4)
                    cosv = V(cos[:, i * 64:i * 64 + 1], (0, 4), (1, 64)); sinv = V(sin[:, i * 64:i * 64 + 1], (0, 4), (1, 64))
                    rope_apply(C, o3[:, :, 0:64], o3[:, :, 64:128], z3[:, :, 0:64], z3[:, :, 64:128], cosv, sinv,
                               V(ra[:], (64, 4), (1, 64)), V(rb[:], (64, 4), (1, 64)), [b_zn, b_cs], [b_o[oi]], b_r)
                C.store(Z[i * 128:(i + 1) * 128, c * 512:(c + 1) * 512], o_[:], [b_o[oi]], [b_Z])
        S.finish([b_Z])
    return C


DIL = (1, 4, 16)


def build_dil_attn(ngh=48):
    C = Ctx()
    nc = C.nc
    QT = C.din("QT", [48, 128, NT], BF16); KT = C.din("KT", [48, 128, 4096], BF16); Vb = C.din("Vb", [48, 128, 4096], BF16)
    halo_d = C.din("halo", [128, 1]); tri_d = C.din("tri2", [128, 256])
    x = C.din("x", [NT, D]); gate_d = C.din("gate", [1, D]); w_o = C.din("w_o", [D, D])
    ident = C.din("ident", [128, 128])
    x1 = C.dout("x1", [NT, D])
    SCALE = float(128 ** -0.5)
    with C.es:
        C.start()
        S = C.S
        ps, psb = C.ps, C.psb
        b_w = S.buf("w")
        w_o_sb = load_w_bf16(C, w_o, D, D, b_w)
        trif = C.sb([128, 256]); tri = C.sb([128, 256], BF16); trih = C.sb([128, 256], BF16); halo = C.sb([128, 1])
        ones = C.sb([128, 128], BF16); gate = C.sb([128, D])
        b_c = S.buf("consts")
        C.load(trif[:], tri_d[:, :], [b_c]); C.load(halo[:], halo_d[:, :], [b_c]); C.load(gate[:], pbc(gate_d, D), [b_c])
        C.cp(tri[:], trif[:], [b_c], [b_c])
        C.cp(trih[:], trif[:], [b_c], [b_c])
        C.ts(trih[:, 0:128], trif[:, 0:128], halo[:, 0:1], ALU.mult, [b_c], [b_c])
        C.memset(ones[:], 1.0, [b_c])
        qt = [C.sb([128, NT], BF16) for _ in range(2)]; kt = [C.sb([128, 4096], BF16) for _ in range(2)]
        vb = [C.sb([128, 4096], BF16) for _ in range(2)]; b_in = S.bufs(2)
        pt = [C.sb([128, 256], BF16) for _ in range(3)]; b_pt = S.bufs(3)
        accO = C.sb([128, NT]); accD = C.sb([128, NT]); b_acc = S.buf()
        otall = C.sb([128, 16 * NT], BF16); b_ot = S.buf()
        xt = [accD] * 2; b_x = [b_acc] * 2
        yt = accO; b_y = b_acc
        b_out = S.buf("out")
        ghs = [(h, g) for h in range(16) for g in range(3) if g * 16 + h < ngh]

        def load_gh(i):
            if i >= len(ghs):
                return
            h, g = ghs[i]
            gh = g * 16 + h
            d = DIL[g]
            ip = i % 2
            C.load(qt[ip][:], QT[gh, :, :], [b_in[ip]])
            C.load(kt[ip][:, 0:NT + 128 * d], KT[gh, :, 0:NT + 128 * d], [b_in[ip]])
            C.load(vb[ip][:, 0:NT + 128 * d], Vb[gh, :, 0:NT + 128 * d], [b_in[ip]])

        tiles = []
        for i, (h, g) in enumerate(ghs):
            for tile in range(16):
                tiles.append(dict(i=i, h=h, g=g, tile=tile, n=len(tiles)))

        def emit_s(t):
            ip = t["i"] % 2
            d = DIL[t["g"]]; nb = 16 // d
            tile = t["tile"]
            r, n = tile // nb, tile % nb
            kb_prev = r * (nb + 1) + n
            kb_cur = kb_prev + 1
            bs = t["n"] % 2
            qv = qt[ip][:, tile * 128:(tile + 1) * 128]
            C.mm(ps[bs][:, 0:128], kt[ip][:, kb_prev * 128:(kb_prev + 1) * 128], qv, True, True,
                 [b_in[ip]], [psb[bs]], sig=False)
            C.mm(ps[bs][:, 128:256], kt[ip][:, kb_cur * 128:(kb_cur + 1) * 128], qv, True, True,
                 [b_in[ip]], [psb[bs]])

        def emit_rest(t):
            ip = t["i"] % 2
            h, g = t["h"], t["g"]
            d = DIL[g]; nb = 16 // d
            tile = t["tile"]
            r, n = tile // nb, tile % nb
            kb_prev = r * (nb + 1) + n
            kb_cur = kb_prev + 1
            bs = t["n"] % 2
            pi = t["n"] % 3
            t4, tq = tile // 4, tile % 4
            nbank = t["n"] // 4
            bo = 2 + (nbank % 2)
            bd = 4 + (nbank % 2)
            C.act(pt[pi][:], ps[bs][:, 0:256], AF.Exp, [psb[bs]], [b_pt[pi]], scale=SCALE)
            C.tt(pt[pi][:], pt[pi][:], (trih if n == 0 else tri)[:], ALU.mult, [b_pt[pi], b_c], [b_pt[pi]])
            oc = slice(tq * 128, (tq + 1) * 128)
            C.mm(ps[bo][:, oc], vb[ip][:, kb_prev * 128:(kb_prev + 1) * 128], pt[pi][:, 0:128], True, False,
                 [b_in[ip], b_pt[pi]], [psb[bo]])
            C.mm(ps[bo][:, oc], vb[ip][:, kb_cur * 128:(kb_cur + 1) * 128], pt[pi][:, 128:256], False, True,
                 [b_in[ip], b_pt[pi]], [psb[bo]], sig=False)
            C.mm(ps[bd][:, oc], ones[:], pt[pi][:, 0:128], True, False, [b_c, b_pt[pi]], [psb[bd]])
            C.mm(ps[bd][:, oc], ones[:], pt[pi][:, 128:256], False, True, [b_c, b_pt[pi]], [psb[bd]], sig=True)
            if tq != 3:
                return
            if d == 1:
                dO = accO[:, t4 * 512:(t4 + 1) * 512]; dD = accD[:, t4 * 512:(t4 + 1) * 512]
                sO = ps[bo][:]; sD = ps[bd][:]
            elif d == 4:
                r0 = t4
                dO = V(accO[:, r0:r0 + 1], (4, 512)); dD = V(accD[:, r0:r0 + 1], (4, 512))
                sO = ps[bo][:]; sD = ps[bd][:]
            else:
                r0 = t4 * 4
                dO = V(accO[:, r0:r0 + 1], (1, 4), (16, 128)); dD = V(accD[:, r0:r0 + 1], (1, 4), (16, 128))
                sO = ps[bo][:].rearrange("p (r i) -> p r i", r=4); sD = ps[bd][:].rearrange("p (r i) -> p r i", r=4)
            if g == 0:
                C.cp(dO, sO, [psb[bo]], [b_acc])
                C.act(dD, sD, AF.Copy, [psb[bd]], [b_acc])
            else:
                C.tt(dO, dO, sO, ALU.add, [psb[bo], b_acc], [b_acc])
                C.tt(dD, dD, sD, ALU.add, [psb[bd], b_acc], [b_acc])
            if tile == 15 and (g == 2 or t["i"] == len(ghs) - 1):
                C.recip(accD[:], accD[:], [b_acc], [b_acc])
                C.tt(otall[:, h * NT:(h + 1) * NT], accO[:], accD[:], ALU.mult, [b_acc], [b_ot])

        load_gh(0)
        for i in range(len(tiles) + 1):
            if i < len(tiles):
                emit_s(tiles[i])
            if i >= 1:
                emit_rest(tiles[i - 1])
            if i < len(tiles) and tiles[i]["tile"] == 0:
                load_gh(tiles[i]["i"] + 1)
        for ti in range(NTILE):
            xp = ti % 2
            C.load(xt[xp][:], x[ti * 128:(ti + 1) * 128, :], [b_x[xp]])
            for nn in range(4):
                bk = 6 + nn % 2
                n0 = nn * 512
                for hh in range(16):
                    C.mm(ps[bk][:], otall[:, hh * NT + ti * 128:hh * NT + (ti + 1) * 128],
                         w_o_sb[:, hh * 2048 + n0:hh * 2048 + n0 + 512], hh == 0, hh == 15, [b_ot, b_w], [psb[bk]])
                C.tt(yt[:, n0:n0 + 512], ps[bk][:], gate[:, n0:n0 + 512], ALU.mult, [psb[bk], b_c], [b_y])
            C.tt(yt[:], yt[:], xt[xp][:], ALU.add, [b_y, b_x[xp]], [b_y])
            C.store(x1[ti * 128:(ti + 1) * 128, :], yt[:], [b_y], [b_out])
        S.finish([b_out])
    return C


def tri2_mask():
    a = np.arange(128)[:, None]
    qi = np.arange(128)[None, :]
    return np.concatenate([(a >= qi), (a <= qi)], axis=1).astype(np.float32)


def dil_exchange(p4res):
    maps = []
    for core in range(8):
        b, j = core // 4, core % 4
        Zc = np.asarray(p4res[core]["Z"]).reshape(NT, 3, 3, 16, 128)
        if j > 0:
            Zp = np.asarray(p4res[core - 1]["Z"]).reshape(NT, 3, 3, 16, 128)
        else:
            Zp = np.zeros_like(Zc)
        dt = Zc.dtype
        QT = np.zeros((48, 128, NT), dt); KT = np.zeros((48, 128, 4096), dt); Vb = np.zeros((48, 128, 4096), dt)
        for g, d in enumerate(DIL):
            nb = 16 // d
            def sub(Zx, which):
                a = Zx[:, g, which]
                return a.reshape(NT // d, d, 16, 128).transpose(1, 0, 2, 3).reshape(d, nb, 128, 16, 128)
            q = sub(Zc, 0); k = sub(Zc, 1); v = sub(Zc, 2)
            kp = sub(Zp, 1)[:, nb - 1:nb]; vp = sub(Zp, 2)[:, nb - 1:nb]
            kk = np.concatenate([kp, k], axis=1)
            vv = np.concatenate([vp, v], axis=1)
            QT[g * 16:(g + 1) * 16] = q.transpose(3, 4, 0, 1, 2).reshape(16, 128, NT)
            KT[g * 16:(g + 1) * 16, :, :NT + 128 * d] = kk.transpose(3, 4, 0, 1, 2).reshape(16, 128, NT + 128 * d)
            Vb[g * 16:(g + 1) * 16, :, :NT + 128 * d] = vv.transpose(3, 2, 0, 1, 4).reshape(16, 128, NT + 128 * d)
        halo = np.full((128, 1), 1.0 if j > 0 else 0.0, np.float32)
        maps.append(dict(QT=QT, KT=KT, Vb=Vb, halo=halo))
    return maps


def _peer_layer(inputs, layer, x_cores, mod):
    C = build_peer()
    pin = peer_inputs(inputs, layer)
    maps = []
    for core in range(8):
        b = core // 4
        m = mod[layer, b]
        d = dict(pin)
        d.update(x=x_cores[core], shift=row(m[6144:8192]), scale=row(m[8192:10240]), gate=row(m[10240:12288]),
                 g=row(inputs["norm_g"][layer, 1]), ident=IDENT)
        maps.append(d)
    res = run(C, maps)
    return [np.asarray(res[c]["x2"]) for c in range(8)]


def kernel(**inputs):
    inputs = {k: np.asarray(v) for k, v in inputs.items()}
    x = inputs["x"].astype(np.float32, copy=False)
    positions = inputs["positions"]
    mod = run_mod(inputs)
    x_cores = []
    for core in range(8):
        b, sl = core_tokens(core)
        x_cores.append(np.ascontiguousarray(x[b, sl]))
    f32 = lambda a: np.ascontiguousarray(np.asarray(a, np.float32))

    C = build_mla_pre()
    maps = []
    for core in range(8):
        b = core // 4
        m = mod[0, b]
        maps.append(dict(x=x_cores[core], pos=pos_layout(positions, core),
                         shift=row(m[0:2048]), scale=row(m[2048:4096]), g=row(inputs["norm_g"][0, 0]),
                         w_in=f32(inputs["mla_w_in"][0]), g_q=row(inputs["mla_g_q"][0]), w_uq=f32(inputs["mla_w_uq"][0]),
                         g_kv=row(inputs["mla_g_kv"][0]), w_ukv=f32(inputs["mla_w_ukv"][0]),
                         g_qn=row(inputs["mla_g_qn"][0]), g_kn=row(inputs["mla_g_kn"][0]),
                         invf=inv_freq(32), ident=IDENT))
    p1 = run(C, maps)
    ex = mla_exchange(p1)
    C = build_mla_attn()
    maps = []
    for core in range(8):
        b = core // 4
        m = mod[0, b]
        d = dict(ex[core])
        d.update(QT=np.asarray(p1[core]["QT"]), tri=tri_mask(), x=x_cores[core], gate=row(m[4096:6144]),
                 w_o=f32(inputs["mla_w_o"][0]), ident=IDENT)
        maps.append(d)
    res = run(C, maps)
    del p1, ex, maps
    x_cores = [np.asarray(res[c]["x1"]) for c in range(8)]
    x_cores = _peer_layer(inputs, 0, x_cores, mod)

    gqk = np.concatenate([np.asarray(inputs["dil_g_qn"][0], np.float32).reshape(-1),
                          np.asarray(inputs["dil_g_kn"][0], np.float32).reshape(-1)]).reshape(1, 768)
    C = build_dil_pre()
    maps = []
    for core in range(8):
        b = core // 4
        m = mod[1, b]
        maps.append(dict(x=x_cores[core], pos=pos_layout(positions, core),
                         shift=row(m[0:2048]), scale=row(m[2048:4096]), g=row(inputs["norm_g"][1, 0]),
                         w_in=f32(inputs["dil_w_in"][0]), gqk=gqk, invf=inv_freq(64), ident=IDENT))
    p4 = run(C, maps)
    ex = dil_exchange(p4)
    del p4
    C = build_dil_attn()
    maps = []
    for core in range(8):
        b = core // 4
        m = mod[1, b]
        d = dict(ex[core])
        d.update(tri2=tri2_mask(), x=x_cores[core], gate=row(m[4096:6144]), w_o=f32(inputs["dil_w_o"][0]), ident=IDENT)
        maps.append(d)
    res = run(C, maps)
    del ex, maps
    x_cores = [np.asarray(res[c]["x1"]) for c in range(8)]
    x_cores = _peer_layer(inputs, 1, x_cores, mod)

    out = np.zeros((2, SEQ, D), np.float32)
    for core in range(8):
        b, sl = core_tokens(core)
        out[b, sl] = x_cores[core]
    return out
```

```python
import numpy as np
import ml_dtypes
from contextlib import ExitStack
import concourse.bass as bass
import concourse.mybir as mybir
from concourse.bass_utils import run_bass_kernel_spmd

F32 = mybir.dt.float32
BF16 = mybir.dt.bfloat16
I32 = mybir.dt.int32
ALU = mybir.AluOpType
AF = mybir.ActivationFunctionType
AX = mybir.AxisListType
NPBF = ml_dtypes.bfloat16

D = 2048
NT = 2048
NTILE = 16
SEQ = 8192
EPS = 1e-6
TWO_PI = float(2 * np.pi)


class Eng:
    def __init__(self, S, name, handle):
        self.S = S
        self.name = name
        self.h = handle
        self.sem = S.es.enter_context(S.nc.semaphore("sem_" + name))
        self.count = 0
        self.seen = {}
        self.seen_dma = {}


class Buf:
    def __init__(self, S, name):
        self.S = S
        self.name = name
        self.w = None
        self.r = []
        self.dsem = None
        self.dval = 0
        self.excl = False

    def dma_sem(self):
        if self.dsem is None:
            self.dsem = self.S.es.enter_context(self.S.nc.semaphore("d_" + self.name))
        return self.dsem


class Sched:
    def __init__(self, nc, es):
        self.nc = nc
        self.es = es
        self.pe = Eng(self, "pe", nc.tensor)
        self.dve = Eng(self, "dve", nc.vector)
        self.act = Eng(self, "act", nc.scalar)
        self.pool = Eng(self, "pool", nc.gpsimd)
        self.sp = Eng(self, "sp", nc.sync)
        self.nbuf = 0

    def buf(self, name=None):
        self.nbuf += 1
        return Buf(self, name or ("b%d" % self.nbuf))

    def bufs(self, n):
        return [self.buf() for _ in range(n)]

    def _wait(self, X, ev):
        if ev is None:
            return
        if ev[0] == 'e':
            _, E, n = ev
            if X.seen.get(E.name, 0) >= n:
                return
            if E is X and n > E.count:
                return
            assert n <= E.count, "waiting on an un-signalled instruction of %s" % E.name
            X.h.wait_ge(E.sem, n)
            X.seen[E.name] = n
        else:
            _, sem, val, key = ev
            if X.seen_dma.get(key, 0) >= val:
                return
            X.h.wait_ge(sem, val)
            X.seen_dma[key] = val

    def _deps(self, X, reads, writes):
        for b in reads:
            self._wait(X, b.w)
            if b.excl:
                for ev in b.r:
                    if not (ev[0] == 'e' and ev[1] is X):
                        self._wait(X, ev)
        for b in writes:
            self._wait(X, b.w)
            for ev in b.r:
                self._wait(X, ev)

    def op(self, X, fn, reads=(), writes=(), signal=True):
        self._deps(X, reads, writes)
        ins = fn()
        if signal:
            X.count += 1
            ins.then_inc(X.sem, 1)
            ev = ('e', X, X.count)
        else:
            ev = ('e', X, X.count + 1)
        for b in reads:
            b.r.append(ev)
            if len(b.r) > 24:
                b.r = self._compact(b.r)
        for b in writes:
            b.w = ev
            b.r = []
        return ins

    @staticmethod
    def _compact(evs):
        last = {}
        for ev in evs:
            k = ('e', ev[1].name) if ev[0] == 'e' else ('d', ev[3])
            if k not in last or ev[2] > last[k][2]:
                last[k] = ev
        return list(last.values())

    def dma(self, Q, out, in_, reads=(), writes=(), **kw):
        self._deps(Q, reads, writes)
        owner = writes[0] if writes else reads[0]
        sem = owner.dma_sem()
        owner.dval += 16
        Q.h.dma_start(out=out, in_=in_, **kw).then_inc(sem, 16)
        ev = ('d', sem, owner.dval, owner.name)
        for b in reads:
            b.r.append(ev)
            if len(b.r) > 24:
                b.r = self._compact(b.r)
        for b in writes:
            b.w = ev
            b.r = []
        return ev

    def finish(self, bufs):
        for b in bufs:
            self._wait(self.sp, b.w)
            for ev in b.r:
                self._wait(self.sp, ev)


def V(ap, *dims):
    return bass.AP(ap.tensor, ap.offset, [list(ap.ap[0])] + [list(d) for d in dims])


def pbc(dram_ap_row, n, parts=128):
    return bass.AP(dram_ap_row.tensor, dram_ap_row.offset, [[0, parts], [1, n]])


class Ctx:
    def __init__(self):
        self.nc = bass.Bass("TRN2", target_bir_lowering=False)
        self.es = ExitStack()
        self.S = None
        self.n = 0

    def start(self):
        self.S = Sched(self.nc, self.es)
        self.ps = [self.es.enter_context(self.nc.psum_tensor("ps%d" % i, [128, 512], F32)) for i in range(8)]
        self.psb = [self.S.buf("psb%d" % i) for i in range(8)]
        for b in self.psb:
            b.excl = True

    def din(self, name, shape, dt=F32):
        return self.nc.dram_tensor(name, list(shape), dt, kind="ExternalInput").ap()

    def dout(self, name, shape, dt=F32):
        return self.nc.dram_tensor(name, list(shape), dt, kind="ExternalOutput").ap()

    def dint(self, name, shape, dt=F32):
        return self.nc.dram_tensor(name, list(shape), dt, kind="Internal").ap()

    def sb(self, shape, dt=F32, name=None):
        self.n += 1
        t = self.es.enter_context(self.nc.sbuf_tensor(name or ("t%d" % self.n), list(shape), dt))
        return t

    def act(self, out, in_, func, r, w, **kw):
        nc = self.nc
        return self.S.op(self.S.act, lambda: nc.scalar.activation(out=out, in_=in_, func=func, **kw), r, w)

    def tt(self, out, in0, in1, op, r, w, eng=None):
        e = eng or self.S.dve
        return self.S.op(e, lambda: e.h.tensor_tensor(out=out, in0=in0, in1=in1, op=op), r, w)

    def ts(self, out, in0, s1, op0, r, w, s2=None, op1=None, eng=None):
        e = eng or self.S.dve
        if op1 is None:
            return self.S.op(e, lambda: e.h.tensor_scalar(out=out, in0=in0, scalar1=s1, scalar2=None, op0=op0), r, w)
        return self.S.op(e, lambda: e.h.tensor_scalar(out=out, in0=in0, scalar1=s1, scalar2=s2, op0=op0, op1=op1), r, w)

    def stt(self, out, in0, scalar, in1, op0, op1, r, w, eng=None):
        e = eng or self.S.dve
        return self.S.op(e, lambda: e.h.scalar_tensor_tensor(out=out, in0=in0, scalar=scalar, in1=in1, op0=op0, op1=op1), r, w)

    def cp(self, out, in_, r, w, eng=None):
        e = eng or self.S.dve
        return self.S.op(e, lambda: e.h.tensor_copy(out=out, in_=in_), r, w)

    def red(self, out, in_, r, w, op=ALU.add):
        nc = self.nc
        return self.S.op(self.S.dve, lambda: nc.vector.tensor_reduce(out=out, in_=in_, axis=AX.X, op=op), r, w)

    def recip(self, out, in_, r, w):
        nc = self.nc
        return self.S.op(self.S.dve, lambda: nc.vector.reciprocal(out=out, in_=in_), r, w)

    def memset(self, ap, val, w, eng=None):
        e = eng or self.S.dve
        return self.S.op(e, lambda: e.h.memset(ap, val), (), w)

    def mm(self, out, lhsT, rhs, start, stop, r, w, sig=None):
        nc = self.nc
        if sig is None:
            sig = bool(stop)
        return self.S.op(self.S.pe, lambda: nc.tensor.matmul(out, lhsT=lhsT, rhs=rhs, start=start, stop=stop), r, w, signal=sig)

    def tr(self, out, in_, ident, r, w, sig=True):
        nc = self.nc
        return self.S.op(self.S.pe, lambda: nc.tensor.transpose(out, in_, ident), r, w, signal=sig)

    def load(self, out, in_, w, r=(), q=None, **kw):
        return self.S.dma(q or self.S.sp, out, in_, reads=r, writes=w, **kw)

    def store(self, out, in_, r, w, q=None, **kw):
        return self.S.dma(q or self.S.pool, out, in_, reads=r, writes=w, **kw)

    def rstd(self, out, ss, scale, r, w):
        self.act(out, ss, AF.Sqrt, list(r) + [self.b_eps], w, bias=self.eps_ap, scale=scale)
        self.recip(out, out, w, w)

    def consts(self, ident_dram):
        self.idf = self.sb([128, 128], F32, "idf")
        self.idb = self.sb([128, 128], BF16, "idb")
        self.b_id = self.S.buf("ident")
        self.load(self.idf[:], ident_dram[:, :], [self.b_id])
        self.cp(self.idb[:], self.idf[:], [self.b_id], [self.b_id])
        self.epst = self.sb([128, 1], F32, "epst")
        self.b_eps = self.S.buf("eps")
        self.memset(self.epst[:], EPS, [self.b_eps])
        self.eps_ap = self.epst[:, 0:1]


def run(ctx, in_maps):
    res = run_bass_kernel_spmd(ctx.nc, in_maps, core_ids=list(range(8)))
    if getattr(res, "exec_time_ns", None):
        print("[kernel] launch exec_time_ns", res.exec_time_ns)
    return res.results


IDENT = np.eye(128, dtype=np.float32)


def build_mod():
    C = Ctx()
    nc = C.nc
    NCOL = 6144
    ccol = C.din("ccol", [128, 16])
    W = C.din("W", [D, NCOL])
    bias = C.din("bias", [1, NCOL])
    out = C.dout("mod", [1, NCOL])
    with C.es:
        C.start()
        S = C.S
        ct = C.sb([128, 16]); sc = C.sb([128, 16])
        bt = C.sb([1, NCOL]); ot = C.sb([1, NCOL])
        slabs = [C.sb([128, 16 * 512]) for _ in range(2)]
        b_c, b_sc, b_b, b_o, b_out = S.bufs(5)
        b_sl = S.bufs(2)
        C.load(ct[:], ccol[:, :], [b_c])
        C.load(bt[:], bias[:, :], [b_b])
        C.act(sc[:], ct[:], AF.Silu, [b_c], [b_sc])
        Wv = W.rearrange("(k p) n -> p k n", p=128)
        for n in range(NCOL // 512):
            sl = slabs[n % 2]
            C.load(sl[:].rearrange("p (k n) -> p k n", k=16), Wv[:, :, n * 512:(n + 1) * 512], [b_sl[n % 2]])
            pb = n % 2
            for k in range(16):
                C.mm(C.ps[pb][0:1, :], sc[:, k:k + 1], sl[:, k * 512:(k + 1) * 512], k == 0, k == 15,
                     [b_sc, b_sl[n % 2]], [C.psb[pb]])
            C.tt(ot[:, n * 512:(n + 1) * 512], C.ps[pb][0:1, :], bt[:, n * 512:(n + 1) * 512], ALU.add,
                 [C.psb[pb], b_b], [b_o])
        C.store(out[:, :], ot[:], [b_o], [b_out])
        S.finish([b_out])
    return C


def run_mod(inputs):
    c = np.asarray(inputs["c"], np.float32)
    ada_w = np.asarray(inputs["ada_w"], np.float32)
    ada_b = np.asarray(inputs["ada_b"], np.float32)
    C = build_mod()
    maps = []
    for core in range(8):
        b, j = core // 4, core % 4
        cols = slice(6144 * j, 6144 * (j + 1))
        if j < 2:
            Wc = ada_w[0][:, 6144 * j:6144 * (j + 1)]
            bc = ada_b[0][6144 * j:6144 * (j + 1)]
        else:
            Wc = ada_w[1][:, 6144 * (j - 2):6144 * (j - 1)]
            bc = ada_b[1][6144 * (j - 2):6144 * (j - 1)]
        maps.append({"ccol": np.ascontiguousarray(c[b].reshape(16, 128).T),
                     "W": np.ascontiguousarray(Wc), "bias": np.ascontiguousarray(bc.reshape(1, -1))})
    res = run(C, maps)
    mod = np.zeros((2, 2, 12288), np.float32)
    for core in range(8):
        b, j = core // 4, core % 4
        l, jj = j // 2, j % 2
        mod[l, b, 6144 * jj:6144 * (jj + 1)] = res[core]["mod"][0]
    return mod


def rope_tables(C, pos_d, invf_d, half, b_out):
    S = C.S
    n = 16 * half
    posi = C.sb([128, 16], I32); posf = C.sb([128, 16])
    invf = C.sb([128, half])
    ang = C.sb([128, n]); kf = C.sb([128, n]); ki = C.sb([128, n], I32)
    cos = C.sb([128, n]); sin = C.sb([128, n])
    b_p, b_i, b_a, b_k = S.bufs(4)
    C.load(posi[:], pos_d[:, :], [b_p])
    C.load(invf[:], pbc(invf_d, half), [b_i])
    C.cp(posf[:], posi[:], [b_p], [b_p])
    C.tt(V(ang[:], (half, 16), (1, half)), V(posf[:], (1, 16), (0, half)), V(invf[:], (0, 16), (1, half)),
         ALU.mult, [b_p, b_i], [b_a])
    for (dst, shift) in ((sin, 0.0), (cos, float(np.pi / 2))):
        if shift != 0.0:
            C.ts(dst[:], ang[:], shift, ALU.add, [b_a], [b_out])
            src = dst
        else:
            src = ang
        C.ts(kf[:], src[:], float(1.0 / TWO_PI), ALU.mult, [b_a, b_out], [b_k])
        C.cp(ki[:], kf[:], [b_k], [b_k])
        C.cp(kf[:], ki[:], [b_k], [b_k])
        C.stt(dst[:], kf[:], -TWO_PI, src[:], ALU.mult, ALU.add, [b_k, b_a, b_out], [b_out])
        C.ts(dst[:], dst[:], float(np.pi), ALU.min, [b_out], [b_out], s2=float(-np.pi), op1=ALU.max)
        C.act(dst[:], dst[:], AF.Sin, [b_out], [b_out])
    return cos, sin


def rope_apply(C, out1, out2, x1, x2, cosv, sinv, ta, tb, r, w, b_t):
    C.tt(ta, x1, cosv, ALU.mult, r, [b_t])
    C.tt(tb, x2, sinv, ALU.mult, r, [b_t])
    C.tt(out1, ta, tb, ALU.subtract, [b_t], w)
    C.tt(ta, x2, cosv, ALU.mult, r, [b_t])
    C.tt(tb, x1, sinv, ALU.mult, r, [b_t])
    C.tt(out2, ta, tb, ALU.add, [b_t], w)


def load_w_bf16(C, dram_w, K, N, b):
    kc = K // 128
    t = C.sb([128, kc * N], BF16)
    src = dram_w.rearrange("(k p) n -> p k n", p=128)
    for k in range(kc):
        C.load(t[:, k * N:(k + 1) * N], src[:, k, :], [b], q=C.S.pool)
    return t


def norm_modulate_setup(C, g_d, scale_d, shift_d, tmp, b_t):
    S = C.S
    gmod = C.sb([128, D]); shiftb = C.sb([128, D])
    b_g, b_s = S.bufs(2)
    C.load(gmod[:], pbc(g_d, D), [b_g])
    C.load(tmp, pbc(scale_d, D), [b_t])
    C.load(shiftb[:], pbc(shift_d, D), [b_s])
    C.stt(gmod[:], tmp, 1.0, gmod[:], ALU.add, ALU.mult, [b_t, b_g], [b_g])
    return gmod, shiftb, b_g, b_s


def build_mla_pre(ntile=NTILE, dbg=0):
    C = Ctx()
    nc = C.nc
    x = C.din("x", [NT, D]); pos = C.din("pos", [128, 16], I32)
    shift_d = C.din("shift", [1, D]); scale_d = C.din("scale", [1, D]); g_d = C.din("g", [1, D])
    w_in = C.din("w_in", [D, 1088]); g_q = C.din("g_q", [1, 512]); w_uq = C.din("w_uq", [512, 3072])
    g_kv = C.din("g_kv", [1, 512]); w_ukv = C.din("w_ukv", [512, 4096])
    g_qn = C.din("g_qn", [1, 192]); g_kn = C.din("g_kn", [1, 192])
    invf = C.din("invf", [1, 32]); ident = C.din("ident", [128, 128])
    QT = C.dout("QT", [16, 192, NT], BF16); KT = C.dout("KT", [16, 192, NT], BF16)
    Vo = C.dout("V", [NT, D], BF16)
    with C.es:
        C.start()
        S = C.S
        C.consts(ident)
        b_w = S.buf("w")
        w_in_sb = load_w_bf16(C, w_in, D, 1088, b_w)
        w_uq_sb = load_w_bf16(C, w_uq, 512, 3072, b_w)
        w_ukv_sb = load_w_bf16(C, w_ukv, 512, 4096, b_w)
        junk = C.sb([128, 2048], BF16); b_junk = S.buf()
        tmpf = C.sb([128, 3072]); b_tmpf = S.buf()
        gmod, shiftb, b_g, b_s = norm_modulate_setup(C, g_d, scale_d, shift_d, tmpf[:, 0:D], b_tmpf)
        gq = C.sb([128, 512]); gkv = C.sb([128, 512]); gqn = C.sb([128, 192]); gkn = C.sb([128, 192])
        b_gs = S.buf("gains")
        for t_, d_, n_ in ((gq, g_q, 512), (gkv, g_kv, 512), (gqn, g_qn, 192), (gkn, g_kn, 192)):
            C.load(t_[:], pbc(d_, n_), [b_gs])
        b_cs = S.buf("cossin")
        cos, sin = rope_tables(C, pos, invf, 32, b_cs)

        xt = [C.sb([128, D]) for _ in range(2)]
        b_x = S.bufs(2)
        st = [C.sb([128, 64]) for _ in range(2)]; b_st = S.bufs(2)
        hb = [C.sb([128, D], BF16)] * 2; b_hb = [S.buf()] * 2
        hT = [C.sb([128, D], BF16)] * 2; b_hT = [S.buf()] * 2
        cn = [C.sb([128, 1024], BF16)] * 2; b_cn = [S.buf()] * 2
        cT = [C.sb([128, 1024], BF16)] * 2; b_cT = [S.buf()] * 2
        kr = [C.sb([128, 64]) for _ in range(2)]; b_kr = S.bufs(2)
        qsb = C.sb([128, 3072]); b_q = S.buf()
        qf = C.sb([128, 3072], BF16); b_qf = S.buf()
        knsb = qsb; b_kn = b_q
        kfn = qf; kfr = qf; b_kf = b_qf
        vsb = [C.sb([128, 2048], BF16)] * 2; b_v = [S.buf()] * 2
        krr = C.sb([128, 64]); ra = C.sb([128, 512]); rb = C.sb([128, 512]); b_r = S.buf()
        Tn = [C.sb([128, 2048], BF16)] * 2; Tr = [C.sb([64, 2048], BF16)] * 2
        b_T = [S.buf()] * 2
        b_QT, b_KT, b_V = S.bufs(3)
        ps, psb = C.ps, C.psb

        def ps_bf(i):
            return ps[i][:].bitcast(BF16)

        for i in range(ntile):
            p = i % 2
            s_ = st[p]
            C.load(xt[p][:], x[i * 128:(i + 1) * 128, :], [b_x[p]])
            if dbg == 1:
                continue
            C.memset(s_[:], 0.0, [b_st[p]])
            C.act(junk[:, 0:D], xt[p][:], AF.Square, [b_x[p]], [b_junk, b_st[p]], accum_out=s_[:, 0:1])
            C.rstd(s_[:, 1:2], s_[:, 0:1], 1.0 / D, [b_st[p]], [b_st[p]])
            C.stt(tmpf[:, 0:D], xt[p][:], s_[:, 1:2], gmod[:], ALU.mult, ALU.mult, [b_x[p], b_st[p], b_g], [b_tmpf])
            C.tt(hb[p][:], tmpf[:, 0:D], shiftb[:], ALU.add, [b_tmpf, b_s], [b_hb[p]])
            for k in range(16):
                bk = k // 8
                C.tr(ps_bf(bk)[:, (k % 8) * 128:(k % 8 + 1) * 128], hb[p][:, k * 128:(k + 1) * 128], C.idb[:],
                     [b_hb[p], C.b_id], [psb[bk]], sig=(k % 8 == 7))
            for bk in range(2):
                C.act(hT[p][:, bk * 1024:(bk + 1) * 1024], ps_bf(bk), AF.Copy, [psb[bk]], [b_hT[p]])
            if dbg == 2:
                continue
            for (bk, n0, n1) in ((2, 0, 512), (3, 512, 1024), (4, 1024, 1088)):
                for k in range(16):
                    C.mm(ps[bk][:, 0:n1 - n0], hT[p][:, k * 128:(k + 1) * 128],
                         w_in_sb[:, k * 1088 + n0:k * 1088 + n1], k == 0, k == 15, [b_hT[p], b_w], [psb[bk]])
            for (bk, col, gt) in ((2, 2, gq), (3, 4, gkv)):
                C.act(junk[:, 0:512], ps[bk][:], AF.Square, [psb[bk]], [b_junk, b_st[p]], accum_out=s_[:, col:col + 1])
                C.rstd(s_[:, col + 1:col + 2], s_[:, col:col + 1], 1.0 / 512, [b_st[p]], [b_st[p]])
                o = (bk - 2) * 512
                C.stt(cn[p][:, o:o + 512], ps[bk][:], s_[:, col + 1:col + 2], gt[:], ALU.mult, ALU.mult,
                      [psb[bk], b_st[p], b_gs], [b_cn[p]])
            C.act(kr[p][:], ps[4][:, 0:64], AF.Copy, [psb[4]], [b_kr[p]])
            for k in range(8):
                C.tr(ps_bf(5)[:, k * 128:(k + 1) * 128], cn[p][:, k * 128:(k + 1) * 128], C.idb[:],
                     [b_cn[p], C.b_id], [psb[5]], sig=(k == 7))
            C.act(cT[p][:], ps_bf(5), AF.Copy, [psb[5]], [b_cT[p]])
            if dbg == 3:
                continue
            for c in range(8):
                bk = 6 + c % 2
                for k in range(4):
                    C.mm(ps[bk][:, 0:384], cT[p][:, k * 128:(k + 1) * 128],
                         w_uq_sb[:, k * 3072 + c * 384:k * 3072 + (c + 1) * 384], k == 0, k == 3,
                         [b_cT[p], b_w], [psb[bk]])
                C.act(qsb[:, c * 384:(c + 1) * 384], ps[bk][:, 0:384], AF.Copy, [psb[bk]], [b_q])
            C.act(tmpf[:, 0:3072], qsb[:], AF.Square, [b_q], [b_tmpf])
            C.red(s_[:, 16:32], tmpf[:, 0:3072].rearrange("p (h d) -> p h d", h=16), [b_tmpf], [b_st[p]])
            C.rstd(s_[:, 16:32], s_[:, 16:32], 1.0 / 192, [b_st[p]], [b_st[p]])
            q3 = qsb[:].rearrange("p (h d) -> p h d", h=16)
            C.tt(q3, q3, V(s_[:, 16:17], (1, 16), (0, 192)), ALU.mult, [b_q, b_st[p]], [b_q])
            C.tt(q3, q3, V(gqn[:], (0, 16), (1, 192)), ALU.mult, [b_q, b_gs], [b_q])
            qf3 = qf[:].rearrange("p (h d) -> p h d", h=16)
            C.cp(qf3[:, :, 0:128], q3[:, :, 0:128], [b_q], [b_qf], eng=S.pool)
            cosv = V(cos[:, i * 32:i * 32 + 1], (0, 16), (1, 32)); sinv = V(sin[:, i * 32:i * 32 + 1], (0, 16), (1, 32))
            ra3 = V(ra[:], (32, 16), (1, 32)); rb3 = V(rb[:], (32, 16), (1, 32))
            rope_apply(C, qf3[:, :, 128:160], qf3[:, :, 160:192], q3[:, :, 128:160], q3[:, :, 160:192],
                       cosv, sinv, ra3, rb3, [b_q, b_cs], [b_qf], b_r)
            if dbg == 4:
                continue
            tp = i % 2
            for h in range(16):
                C.tr(ps_bf(h // 8)[:, (h % 8) * 128:(h % 8 + 1) * 128], qf3[:, h, 0:128], C.idb[:],
                     [b_qf, C.b_id], [psb[h // 8]], sig=False)
                C.tr(ps_bf(2 + h // 8)[0:64, (h % 8) * 128:(h % 8 + 1) * 128], qf3[:, h, 128:192], C.idb[:],
                     [b_qf, C.b_id], [psb[2 + h // 8]], sig=(h % 8 == 7))
            for bk in range(2):
                C.act(Tn[tp][:, bk * 1024:(bk + 1) * 1024], ps_bf(bk), AF.Copy, [psb[bk]], [b_T[tp]])
                C.cp(Tr[tp][:, bk * 1024:(bk + 1) * 1024], ps_bf(2 + bk)[0:64, :], [psb[2 + bk]], [b_T[tp]])
            C.store(QT[:, 0:128, i * 128:(i + 1) * 128].rearrange("h d t -> d h t"),
                    Tn[tp][:].rearrange("p (h t) -> p h t", h=16), [b_T[tp]], [b_QT])
            C.store(QT[:, 128:192, i * 128:(i + 1) * 128].rearrange("h d t -> d h t"),
                    Tr[tp][:].rearrange("p (h t) -> p h t", h=16), [b_T[tp]], [b_QT])
            if dbg == 5:
                continue
            kn3 = knsb[:, 0:2048].rearrange("p (h d) -> p h d", h=16)
            v3 = vsb[p][:].rearrange("p (h d) -> p h d", h=16)
            for c in range(8):
                bk = 6 + c % 2
                for k in range(4):
                    C.mm(ps[bk][:], cT[p][:, 512 + k * 128:512 + (k + 1) * 128],
                         w_ukv_sb[:, k * 4096 + c * 512:k * 4096 + (c + 1) * 512], k == 0, k == 3,
                         [b_cT[p], b_w], [psb[bk]])
                pv = ps[bk][:].rearrange("p (h d) -> p h d", h=2)
                C.act(kn3[:, 2 * c:2 * c + 2, :], pv[:, :, 0:128], AF.Copy, [psb[bk]], [b_kn])
                C.cp(v3[:, 2 * c:2 * c + 2, :], pv[:, :, 128:256], [psb[bk]], [b_v[p]])
            C.store(Vo[i * 128:(i + 1) * 128, :], vsb[p][:], [b_v[p]], [b_V])
            if dbg == 6:
                continue
            C.act(tmpf[:, 0:2048], knsb[:, 0:2048], AF.Square, [b_kn], [b_tmpf])
            C.red(s_[:, 32:48], tmpf[:, 0:2048].rearrange("p (h d) -> p h d", h=16), [b_tmpf], [b_st[p]])
            C.act(junk[:, 0:64], kr[p][:], AF.Square, [b_kr[p]], [b_junk, b_st[p]], accum_out=s_[:, 6:7])
            C.ts(s_[:, 32:48], s_[:, 32:48], s_[:, 6:7], ALU.add, [b_st[p]], [b_st[p]])
            C.rstd(s_[:, 32:48], s_[:, 32:48], 1.0 / 192, [b_st[p]], [b_st[p]])
            kfn3 = kfn[:, 0:2048].rearrange("p (h d) -> p h d", h=16)
            C.tt(kn3, kn3, V(s_[:, 32:33], (1, 16), (0, 128)), ALU.mult, [b_kn, b_st[p]], [b_kn])
            C.tt(kfn3, kn3, V(gkn[:], (0, 16), (1, 128)), ALU.mult, [b_kn, b_gs], [b_kf])
            C.tt(kr[p][:], kr[p][:], gkn[:, 128:192], ALU.mult, [b_kr[p], b_gs], [b_kr[p]])
            rope_apply(C, krr[:, 0:32], krr[:, 32:64], kr[p][:, 0:32], kr[p][:, 32:64],
                       cos[:, i * 32:(i + 1) * 32], sin[:, i * 32:(i + 1) * 32], ra[:, 0:32], rb[:, 0:32],
                       [b_kr[p], b_cs], [b_r], b_r)
            C.tt(V(kfr[:, 2048:2049], (64, 16), (1, 64)), V(krr[:], (0, 16), (1, 64)), V(s_[:, 32:33], (1, 16), (0, 64)),
                 ALU.mult, [b_r, b_st[p]], [b_kf])
            if dbg == 7:
                continue
            tp = (i + 1) % 2
            kfr3 = V(kfr[:, 2048:2049], (64, 16), (1, 64))
            for h in range(16):
                C.tr(ps_bf(h // 8)[:, (h % 8) * 128:(h % 8 + 1) * 128], kfn3[:, h, :], C.idb[:],
                     [b_kf, C.b_id], [psb[h // 8]], sig=False)
                C.tr(ps_bf(2 + h // 8)[0:64, (h % 8) * 128:(h % 8 + 1) * 128], kfr3[:, h, :], C.idb[:],
                     [b_kf, C.b_id], [psb[2 + h // 8]], sig=(h % 8 == 7))
            for bk in range(2):
                C.act(Tn[tp][:, bk * 1024:(bk + 1) * 1024], ps_bf(bk), AF.Copy, [psb[bk]], [b_T[tp]])
                C.cp(Tr[tp][:, bk * 1024:(bk + 1) * 1024], ps_bf(2 + bk)[0:64, :], [psb[2 + bk]], [b_T[tp]])
            C.store(KT[:, 0:128, i * 128:(i + 1) * 128].rearrange("h d t -> d h t"),
                    Tn[tp][:].rearrange("p (h t) -> p h t", h=16), [b_T[tp]], [b_KT])
            C.store(KT[:, 128:192, i * 128:(i + 1) * 128].rearrange("h d t -> d h t"),
                    Tr[tp][:].rearrange("p (h t) -> p h t", h=16), [b_T[tp]], [b_KT])
        S.finish([b_QT, b_KT, b_V])
    return C


def core_tokens(core):
    b, j = core // 4, core % 4
    return b, slice(NT * j, NT * (j + 1))


def pos_layout(positions, core):
    b, sl = core_tokens(core)
    return np.ascontiguousarray(np.asarray(positions[b, sl], np.int32).reshape(16, 128).T)


def inv_freq(half):
    return (10000.0 ** (-np.arange(half, dtype=np.float32) / np.float32(half))).astype(np.float32).reshape(1, half)


def row(v):
    return np.ascontiguousarray(np.asarray(v, np.float32).reshape(1, -1))


def build_mla_attn(nqb=4, nheads=16):
    C = Ctx()
    nc = C.nc
    QT = C.din("QT", [16, 192, NT], BF16); KT = C.din("KT", [16, 192, SEQ], BF16)
    Vp = C.din("Vp", [16, 128, 64 * 128], BF16)
    maskb_d = C.din("maskb", [128, 64]); tri_d = C.din("tri", [128, 128])
    x = C.din("x", [NT, D]); gate_d = C.din("gate", [1, D]); w_o = C.din("w_o", [D, D])
    ident = C.din("ident", [128, 128])
    x1 = C.dout("x1", [NT, D])
    SCALE = float(192 ** -0.5)
    with C.es:
        C.start()
        S = C.S
        ps, psb = C.ps, C.psb
        b_w = S.buf("w")
        w_o_sb = load_w_bf16(C, w_o, D, D, b_w)
        maskb = C.sb([128, 64]); trif = C.sb([128, 128]); trib = C.sb([128, 128], BF16)
        ones = C.sb([128, 128], BF16); gate = C.sb([128, D])
        b_c = S.buf("consts")
        C.load(maskb[:], maskb_d[:, :], [b_c]); C.load(trif[:], tri_d[:, :], [b_c])
        C.load(gate[:], pbc(gate_d, D), [b_c])
        C.cp(trib[:], trif[:], [b_c], [b_c])
        C.memset(ones[:], 1.0, [b_c])
        NS = 3
        ktn = [C.sb([128, 2048], BF16) for _ in range(NS)]; ktr = [C.sb([64, 2048], BF16) for _ in range(NS)]
        vs = [C.sb([128, 2048], BF16) for _ in range(NS)]; b_kv = S.bufs(NS)
        qn = [C.sb([128, 512], BF16) for _ in range(2)]; qr = [C.sb([64, 512], BF16) for _ in range(2)]; b_qq = S.bufs(2)
        pt = [C.sb([128, 512], BF16) for _ in range(3)]; b_pt = S.bufs(3)
        ot = C.sb([128, 16 * 512], BF16); b_ot = S.buf()
        rden = C.sb([128, 512]); b_rd = S.buf()
        xt = [C.sb([128, D]) for _ in range(2)]; b_x = S.bufs(2)
        yt = C.sb([128, D]); b_y = S.buf()
        b_out = S.buf("out")
        heads = [(qb, h) for qb in range(nqb) for h in range(nheads)]
        segs = []
        for hi, (qb, h) in enumerate(heads):
            nch = 48 + 4 * (qb + 1)
            for seg in range(4):
                c0 = seg * 16
                segs.append(dict(hi=hi, qb=qb, h=h, c0=c0, c1=min(nch, c0 + 16), nch=nch, idx=len(segs)))

        def load_q(hi):
            if hi >= len(heads):
                return
            qb, h = heads[hi]
            qp = hi % 2
            C.load(qn[qp][:], QT[h, 0:128, qb * 512:(qb + 1) * 512], [b_qq[qp]])
            C.load(qr[qp][:], QT[h, 128:192, qb * 512:(qb + 1) * 512], [b_qq[qp]])

        def load_seg(si):
            if si >= len(segs):
                return
            sg = segs[si]
            sl = si % NS
            nk = (sg["c1"] - sg["c0"]) * 128
            k0 = sg["c0"] * 128
            C.load(ktn[sl][:, 0:nk], KT[sg["h"], 0:128, k0:k0 + nk], [b_kv[sl]])
            C.load(ktr[sl][:, 0:nk], KT[sg["h"], 128:192, k0:k0 + nk], [b_kv[sl]])
            C.load(vs[sl][:, 0:nk], Vp[sg["h"], :, k0:k0 + nk], [b_kv[sl]])

        chunks = []
        for sg in segs:
            for c in range(sg["c0"], sg["c1"]):
                chunks.append(dict(sg=sg, c=c, n=len(chunks)))

        def emit_s(ch):
            sg = ch["sg"]; c = ch["c"]
            hi = sg["hi"]; qp = hi % 2; sl = sg["idx"] % NS
            lc = c - sg["c0"]
            dc = c - (sg["nch"] - 4)
            q0 = 128 * dc if dc > 0 else 0
            bs = ch["n"] % 2
            C.mm(ps[bs][:, q0:512], ktn[sl][:, lc * 128:(lc + 1) * 128], qn[qp][:, q0:512], True, False,
                 [b_kv[sl], b_qq[qp]], [psb[bs]])
            C.mm(ps[bs][:, q0:512], ktr[sl][:, lc * 128:(lc + 1) * 128], qr[qp][:, q0:512], False, True,
                 [b_kv[sl], b_qq[qp]], [psb[bs]])

        def emit_rest(ch):
            sg = ch["sg"]; c = ch["c"]
            hi = sg["hi"]; qp = hi % 2; sl = sg["idx"] % NS
            nch = sg["nch"]; h = sg["h"]; qb = sg["qb"]
            lc = c - sg["c0"]
            dc = c - (nch - 4)
            q0 = 128 * dc if dc > 0 else 0
            bs = ch["n"] % 2
            pi = ch["n"] % 3
            bo = 2 + qp
            bd = 4 + qp
            C.act(pt[pi][:, q0:512], ps[bs][:, q0:512], AF.Exp, [psb[bs], b_c], [b_pt[pi]],
                  bias=maskb[:, c:c + 1], scale=SCALE)
            if dc >= 0:
                C.tt(pt[pi][:, q0:q0 + 128], pt[pi][:, q0:q0 + 128], trib[:], ALU.mult,
                     [b_pt[pi], b_c], [b_pt[pi]])
            C.mm(ps[bo][:, q0:512], vs[sl][:, lc * 128:(lc + 1) * 128], pt[pi][:, q0:512], c == 0, c == nch - 1,
                 [b_kv[sl], b_pt[pi]], [psb[bo]], sig=False)
            C.mm(ps[bd][:, q0:512], ones[:], pt[pi][:, q0:512], c == 0, c == nch - 1,
                 [b_c, b_pt[pi]], [psb[bd]], sig=True)
            if c == nch - 1:
                C.recip(rden[:], ps[bd][:], [psb[bd]], [b_rd])
                C.tt(ot[:, h * 512:(h + 1) * 512], ps[bo][:], rden[:], ALU.mult, [psb[bo], b_rd], [b_ot])
                if h == nheads - 1:
                    emit_wo(qb)

        def emit_wo(qb):
            for t4 in range(4):
                ti = qb * 4 + t4
                xp = ti % 2
                C.load(xt[xp][:], x[ti * 128:(ti + 1) * 128, :], [b_x[xp]])
                for half in range(2):
                    for nn in range(2):
                        bk = 6 + nn
                        n0 = half * 1024 + nn * 512
                        for hh in range(nheads):
                            C.mm(ps[bk][:], ot[:, hh * 512 + t4 * 128:hh * 512 + (t4 + 1) * 128],
                                 w_o_sb[:, hh * 2048 + n0:hh * 2048 + n0 + 512], hh == 0, hh == nheads - 1,
                                 [b_ot, b_w], [psb[bk]])
                        C.tt(yt[:, n0:n0 + 512], ps[bk][:], gate[:, n0:n0 + 512], ALU.mult, [psb[bk], b_c], [b_y])
                C.tt(yt[:], yt[:], xt[xp][:], ALU.add, [b_y, b_x[xp]], [b_y])
                C.store(x1[ti * 128:(ti + 1) * 128, :], yt[:], [b_y], [b_out])
        load_q(0)
        load_seg(0)
        load_seg(1)
        for i in range(len(chunks) + 1):
            if i < len(chunks):
                emit_s(chunks[i])
            if i >= 1:
                emit_rest(chunks[i - 1])
            if i < len(chunks) and chunks[i]["c"] == chunks[i]["sg"]["c0"]:
                load_seg(chunks[i]["sg"]["idx"] + 2)
                if chunks[i]["c"] == 0:
                    load_q(chunks[i]["sg"]["hi"] + 1)
        S.finish([b_out])
    return C


def tri_mask():
    p = np.arange(128)[:, None]
    i = np.arange(128)[None, :]
    return (p <= i).astype(np.float32)


def mla_exchange(p1res):
    maps = []
    for core in range(8):
        b, j = core // 4, core % 4
        KT_all = np.concatenate([np.asarray(p1res[b * 4 + jj]["KT"]) for jj in range(4)], axis=2)
        V_all = np.concatenate([np.asarray(p1res[b * 4 + jj]["V"]) for jj in range(4)], axis=0)
        nvalid = NT * (j + 1)
        KTp = np.zeros((16, 192, SEQ), dtype=KT_all.dtype)
        KTp[:, :, SEQ - nvalid:] = KT_all[:, :, :nvalid]
        Vs = np.zeros((SEQ, D), dtype=V_all.dtype)
        Vs[SEQ - nvalid:] = V_all[:nvalid]
        Vp = np.ascontiguousarray(Vs.reshape(64, 128, 16, 128).transpose(2, 1, 0, 3)).reshape(16, 128, 64 * 128)
        maskb = np.zeros((128, 64), np.float32)
        maskb[:, :(SEQ - nvalid) // 128] = -30000.0
        maps.append(dict(KT=KTp, Vp=Vp, maskb=maskb))
    return maps


def fence(old, new):
    evs = []
    for b in old:
        if b.w is not None:
            evs.append(b.w)
        evs.extend(b.r)
    evs = Sched._compact(evs) if evs else []
    for b in new:
        b.w = None
        b.r = list(evs)


def build_peer(ntb=4, ngroups=32):
    C = Ctx()
    nc = C.nc
    x = C.din("x", [NT, D])
    shift_d = C.din("shift", [1, D]); scale_d = C.din("scale", [1, D]); gate_d = C.din("gate", [1, D]); g_d = C.din("g", [1, D])
    w_q = C.din("w_q", [D, D]); skT_d = C.din("skT", [128, 16 * 128])
    UT = C.din("UT", [D, 16384]); Vt = C.din("Vt", [16384, D])
    ident = C.din("ident", [128, 128])
    out = C.dout("x2", [NT, D])
    MASK_T = 1.0 - 2e-4
    with C.es:
        S = Sched(nc, C.es)
        C.S = S
        psY = C.es.enter_context(nc.psum_tensor("psY", [128, 2048], F32)); b_psY = S.buf("psY"); b_psY.excl = True
        ps = [C.es.enter_context(nc.psum_tensor("psq%d" % i, [128, 512], F32)) for i in range(4)]
        psb = S.bufs(4)
        for b in psb:
            b.excl = True
        psYb = [psY[:, i * 512:(i + 1) * 512] for i in range(4)]
        C.consts(ident)
        yacc = [C.sb([128, D]) for _ in range(4)]; b_ya = S.bufs(4)
        hTb = C.sb([128, 16 * 512], BF16); b_hTb = S.buf()
        a1 = [C.sb([128, 1024]) for _ in range(4)]; a2 = [C.sb([128, 1024]) for _ in range(4)]; b_a = S.bufs(4)
        diag = C.sb([128, 32 * 128], BF16); b_dg = S.buf()
        st = C.sb([128, 64]); b_st = S.buf()
        AW = 29696
        arena = C.sb([128, AW])
        o = 0
        def carve(n, dt=F32):
            nonlocal o
            v = arena[:, o:o + n]
            o += n
            return v.bitcast(dt) if dt != F32 else v
        gmod = carve(2048); shiftb = carve(2048); xt = carve(2048); h2 = carve(2048)
        hTf = carve(8192); wq = [carve(2048), carve(2048)]; qTc = [carve(512), carve(512)]
        s_sb = [carve(512), carve(512)]
        skT = carve(2048); tk = carve(256); cand = carve(512); junk = carve(1024, BF16)
        assert o <= AW, o
        pb_g, pb_x, pb_h2, pb_hTf, pb_s = S.bufs(5)
        pb_wq = S.bufs(2); pb_qT = S.bufs(2)
        b_sk, b_tk, b_cd, b_junk = S.bufs(4)
        pro_bufs = [pb_g, pb_x, pb_h2, pb_hTf, pb_s, b_sk, b_tk, b_cd, b_junk] + pb_wq + pb_qT
        o = 0
        utg = [carve(4096, BF16), carve(4096, BF16)]
        vg = [carve(4096, BF16), carve(4096, BF16)]
        Pp = carve(2048)
        Mp = [carve(1024, BF16) for _ in range(4)]
        gel = [carve(512), carve(512)]
        actT = [carve(1024, BF16), carve(1024, BF16)]
        ytmp = [carve(2048), carve(2048)]
        assert o <= AW, o
        mb_ut = S.bufs(2); mb_v = S.bufs(2); mb_P = S.buf(); mb_M = S.bufs(4); mb_gel = S.bufs(2)
        mb_act = S.bufs(2); mb_yt = S.bufs(2)
        main_bufs = mb_ut + mb_v + [mb_P] + mb_M + mb_gel + mb_act + mb_yt
        o = 0
        gateb = carve(2048); ext = [carve(2048), carve(2048)]; eo = [carve(2048), carve(2048)]
        eb_g = S.buf(); eb_x = S.bufs(2); eb_o = S.bufs(2)
        epi_bufs = [eb_g] + eb_x + eb_o
        b_out = S.buf("out")
        UTv = UT.rearrange("(k p) e -> p k e", p=128)
        wqv = w_q.rearrange("(k p) n -> p k n", p=128)
        a1v = [t[:] for t in a1]; a2v = [t[:] for t in a2]

        for tb in range(ntb):
            fence(epi_bufs + main_bufs, pro_bufs)
            C.load(gmod, pbc(g_d, D), [pb_g]); C.load(h2, pbc(scale_d, D), [pb_h2]); C.load(shiftb, pbc(shift_d, D), [pb_g])
            C.load(skT, skT_d[:, :], [b_sk])
            C.stt(gmod, h2, 1.0, gmod, ALU.add, ALU.mult, [pb_h2, pb_g], [pb_g])
            for tt in range(4):
                ti = tb * 4 + tt
                C.load(xt, x[ti * 128:(ti + 1) * 128, :], [pb_x])
                C.memset(st[:, 0:1], 0.0, [b_st])
                C.act(junk, xt, AF.Square, [pb_x], [b_junk, b_st], accum_out=st[:, 0:1])
                C.rstd(st[:, 1:2], st[:, 0:1], 1.0 / D, [b_st], [b_st])
                C.stt(h2, xt, st[:, 1:2], gmod, ALU.mult, ALU.mult, [pb_x, b_st, pb_g], [pb_h2])
                C.tt(h2, h2, shiftb, ALU.add, [pb_h2, pb_g], [pb_h2])
                for bk in range(4):
                    for kk in range(4):
                        k = bk * 4 + kk
                        C.tr(ps[bk][:, kk * 128:(kk + 1) * 128], h2[:, k * 128:(k + 1) * 128], C.idf[:],
                             [pb_h2, C.b_id], [psb[bk]], sig=(kk == 3))
                    src = ps[bk][:].rearrange("p (k t) -> p k t", k=4)
                    C.act(V(hTf[:, bk * 4 * 512 + tt * 128:bk * 4 * 512 + tt * 128 + 1], (512, 4), (1, 128)), src, AF.Copy,
                          [psb[bk]], [pb_hTf])
                    C.cp(V(hTb[:, bk * 4 * 512 + tt * 128:bk * 4 * 512 + tt * 128 + 1], (512, 4), (1, 128)), src,
                         [psb[bk]], [b_hTb])
            for cc in range(16):
                hd, pp = cc // 2, cc % 2
                w = wq[cc % 2]
                C.load(w.rearrange("p (k n) -> p k n", k=16), wqv[:, :, cc * 128:(cc + 1) * 128], [pb_wq[cc % 2]])
                bq = cc % 2
                for k in range(16):
                    C.mm(ps[bq][:], w[:, k * 128:(k + 1) * 128], hTf[:, k * 512:(k + 1) * 512], k == 0, k == 15,
                         [pb_wq[cc % 2], pb_hTf], [psb[bq]])
                C.act(qTc[cc % 2], ps[bq][:], AF.Copy, [psb[bq]], [pb_qT[cc % 2]])
                bs = 2 + pp
                for tt in range(4):
                    C.mm(ps[bs][:, tt * 128:(tt + 1) * 128], qTc[cc % 2][:, tt * 128:(tt + 1) * 128],
                         skT[:, cc * 128:(cc + 1) * 128], True, True, [pb_qT[cc % 2], b_sk], [psb[bs]], sig=(tt == 3))
                C.act(s_sb[pp], ps[bs][:], AF.Copy, [psb[bs]], [pb_s])
                if pp == 0:
                    continue
                for tt in range(4):
                    T = tk[:, tt * 64:(tt + 1) * 64]
                    for half in range(2):
                        sv = s_sb[half][:, tt * 128:(tt + 1) * 128]
                        C.S.op(S.dve, lambda: nc.vector.max(out=T[:, half * 16:half * 16 + 8], in_=sv), [pb_s], [b_tk])
                        C.S.op(S.dve, lambda: nc.vector.match_replace(out=cand[:, 0:128], in_to_replace=T[:, half * 16:half * 16 + 8],
                                                                      in_values=sv, imm_value=-1e30), [pb_s, b_tk], [b_cd])
                        C.S.op(S.dve, lambda: nc.vector.max(out=T[:, half * 16 + 8:half * 16 + 16], in_=cand[:, 0:128]), [b_cd], [b_tk])
                    C.tt(V(cand[:, 0:1], (16, 16), (1, 16)), V(T[:, 0:1], (1, 16), (0, 16)), V(T[:, 16:17], (0, 16), (1, 16)),
                         ALU.add, [b_tk], [b_cd])
                    C.S.op(S.dve, lambda: nc.vector.max(out=T[:, 32:40], in_=cand[:, 0:256]), [b_cd], [b_tk])
                    C.S.op(S.dve, lambda: nc.vector.match_replace(out=cand[:, 256:512], in_to_replace=T[:, 32:40],
                                                                  in_values=cand[:, 0:256], imm_value=-1e30), [b_cd, b_tk], [b_cd])
                    C.S.op(S.dve, lambda: nc.vector.max(out=T[:, 40:48], in_=cand[:, 256:512]), [b_cd], [b_tk])
                    C.ts(T[:, 48:49], T[:, 47:48], -1.0, ALU.mult, [b_tk], [b_tk])
                    C.ts(T[:, 49:50], T[:, 47:48], -0.5, ALU.mult, [b_tk], [b_tk])
                    C.memset(T[:, 50:51], 0.0, [b_tk])
                    C.act(T[:, 52:64][:, 0:12], T[:, 32:44], AF.Exp, [b_tk], [b_tk], bias=T[:, 48:49], scale=1.0)
                    C.red(T[:, 50:51], T[:, 52:64], [b_tk], [b_tk])
                    C.act(T[:, 52:56], T[:, 44:48], AF.Exp, [b_tk], [b_tk], bias=T[:, 48:49], scale=1.0)
                    C.red(T[:, 51:52], T[:, 52:56], [b_tk], [b_tk])
                    C.tt(T[:, 50:51], T[:, 50:51], T[:, 51:52], ALU.add, [b_tk], [b_tk])
                    C.recip(T[:, 51:52], T[:, 50:51], [b_tk], [b_tk])
                    di = tt * 8 + hd
                    C.ts(diag[:, di * 128:(di + 1) * 128], C.idf[:], T[:, 51:52], ALU.mult, [C.b_id, b_tk], [b_dg])
                    C.act(a1[tt][:, hd * 128:(hd + 1) * 128], s_sb[0][:, tt * 128:(tt + 1) * 128], AF.Exp, [pb_s, b_tk], [b_a[tt]],
                          bias=T[:, 49:50], scale=1.0)
                    C.act(a2[tt][:, hd * 128:(hd + 1) * 128], s_sb[1][:, tt * 128:(tt + 1) * 128], AF.Exp, [pb_s, b_tk], [b_a[tt]],
                          bias=T[:, 49:50], scale=1.0)
            fence(pro_bufs, main_bufs)
            for tt in range(4):
                C.memset(yacc[tt][:], 0.0, [b_ya[tt]], eng=S.pool)
            def load_ut(g):
                if g < ngroups:
                    C.load(utg[g % 2].rearrange("p (k e) -> p k e", k=16), UTv[:, :, g * 512:(g + 1) * 512], [mb_ut[g % 2]], q=S.pool)

            def load_v(g):
                if g < ngroups:
                    C.load(vg[g % 2].rearrange("p (c d) -> p c d", c=4),
                           Vt[g * 512:(g + 1) * 512, :].rearrange("(c p) d -> p c d", p=128), [mb_v[g % 2]], q=S.pool)

            def emit_A(g, sg):
                sl = g % 2
                for c2 in range(2):
                    c = sg * 2 + c2
                    for k in range(16):
                        C.mm(ps[c2][:], utg[sl][:, k * 512 + c * 128:k * 512 + (c + 1) * 128], hTb[:, k * 512:(k + 1) * 512],
                             k == 0, k == 15, [mb_ut[sl], b_hTb], [psb[c2]])

            def emit_gelu(g, sg):
                for c2 in range(2):
                    C.act(gel[c2], ps[c2][:], AF.Gelu, [psb[c2]], [mb_gel[c2]])

            def emit_actT(g, sg):
                at = actT[g % 2]
                for c2 in range(2):
                    c = sg * 2 + c2
                    C.tt(at[:, c * 512:(c + 1) * 512], gel[c2], ps[2 + c2][:], ALU.mult, [mb_gel[c2], psb[2 + c2]], [mb_act[g % 2]])

            def emit_G(g, sg, prev, hook=None):
                i1 = g * 4 + sg * 2
                for tt in range(4):
                    mi = tt
                    P3 = V(Pp[:, 0:1], (256, 8), (128, 2), (1, 128))
                    C.tt(P3, V(a1v[tt][:, i1:i1 + 1], (128, 8), (1, 2), (0, 128)), V(a2v[tt][:, 0:1], (128, 8), (0, 2), (1, 128)),
                         ALU.mult, [b_a[tt]], [mb_P])
                    C.stt(Mp[mi], Pp, MASK_T, Pp, ALU.is_ge, ALU.mult, [mb_P], [mb_M[mi]])
                    if tt == 0:
                        continue
                    if tt == 1:
                        if prev is not None:
                            emit_actT(*prev)
                        emit_gelu(g, sg)
                        if hook is not None:
                            hook()
                    for t2 in ((0, 1) if tt == 1 else (tt,)):
                        for c2 in range(2):
                            for hd in range(8):
                                di = t2 * 8 + hd
                                C.mm(ps[2 + c2][:, t2 * 128:(t2 + 1) * 128], Mp[t2][:, hd * 256 + c2 * 128:hd * 256 + (c2 + 1) * 128],
                                     diag[:, di * 128:(di + 1) * 128], hd == 0, hd == 7, [mb_M[t2], b_dg], [psb[2 + c2]],
                                     sig=(c2 == 1 and hd == 7))

            ny = [0]

            def emit_S2(g, tt):
                if g < 0:
                    return
                sl = g % 2
                at = actT[g % 2]
                for dn in range(4):
                    for c in range(4):
                        C.mm(psYb[dn], at[:, c * 512 + tt * 128:c * 512 + (tt + 1) * 128],
                             vg[sl][:, c * 2048 + dn * 512:c * 2048 + (dn + 1) * 512], c == 0, c == 3,
                             [mb_act[g % 2], mb_v[sl]], [b_psY], sig=(dn == 3 and c == 3))
                yi = ny[0] % 2
                ny[0] += 1
                C.act(ytmp[yi], psY[:], AF.Copy, [b_psY], [mb_yt[yi]])
                C.tt(yacc[tt][:], yacc[tt][:], ytmp[yi], ALU.add, [mb_yt[yi], b_ya[tt]], [b_ya[tt]], eng=S.pool)

            load_ut(0); load_v(0); load_ut(1)
            prev = None
            for g in range(ngroups):
                emit_A(g, 0)
                emit_G(g, 0, prev, hook=lambda: emit_S2(g - 1, 0))
                prev = (g, 0)
                emit_S2(g - 1, 1)
                emit_A(g, 1)
                emit_S2(g - 1, 2)
                emit_G(g, 1, prev)
                prev = (g, 1)
                emit_S2(g - 1, 3)
                load_v(g + 1)
                load_ut(g + 2)
            emit_actT(*prev)
            for tt in range(4):
                emit_S2(ngroups - 1, tt)
            fence(main_bufs, epi_bufs)
            C.load(gateb, pbc(gate_d, D), [eb_g])
            for tt in range(4):
                ti = tb * 4 + tt
                C.load(ext[tt % 2], x[ti * 128:(ti + 1) * 128, :], [eb_x[tt % 2]])
                C.tt(eo[tt % 2], yacc[tt][:], gateb, ALU.mult, [b_ya[tt], eb_g], [eb_o[tt % 2]])
                C.tt(eo[tt % 2], eo[tt % 2], ext[tt % 2], ALU.add, [eb_o[tt % 2], eb_x[tt % 2]], [eb_o[tt % 2]])
                C.store(out[ti * 128:(ti + 1) * 128, :], eo[tt % 2], [eb_o[tt % 2]], [b_out])
        S.finish([b_out])
    return C


def peer_inputs(inputs, layer):
    sk = np.asarray(inputs["peer_sub_keys"][layer], np.float32)
    skT = np.ascontiguousarray(sk.reshape(16, 128, 128).transpose(2, 0, 1)).reshape(128, 16 * 128)
    UT = np.ascontiguousarray(np.asarray(inputs["peer_u"][layer], np.float32).T)
    Vt = np.ascontiguousarray(np.asarray(inputs["peer_v"][layer], np.float32))
    return dict(skT=skT, UT=UT, Vt=Vt, w_q=np.ascontiguousarray(np.asarray(inputs["peer_w_q"][layer], np.float32)))


def build_dil_pre(nchunks=36):
    C = Ctx()
    nc = C.nc
    x = C.din("x", [NT, D]); pos = C.din("pos", [128, 16], I32)
    shift_d = C.din("shift", [1, D]); scale_d = C.din("scale", [1, D]); g_d = C.din("g", [1, D])
    w_in = C.din("w_in", [D, 18432]); gqk_d = C.din("gqk", [1, 6 * 128])
    invf = C.din("invf", [1, 64]); ident = C.din("ident", [128, 128])
    Z = C.dout("Z", [NT, 18432], BF16)
    with C.es:
        C.start()
        S = C.S
        ps, psb = C.ps, C.psb
        C.consts(ident)
        tmpf = C.sb([128, D]); b_tmpf = S.buf()
        gmod, shiftb, b_g, b_s = norm_modulate_setup(C, g_d, scale_d, shift_d, tmpf[:], b_tmpf)
        gqk = C.sb([128, 768]); b_gs = S.buf()
        C.load(gqk[:], pbc(gqk_d, 768), [b_gs])
        b_cs = S.buf("cossin")
        cos, sin = rope_tables(C, pos, invf, 64, b_cs)
        hT = C.sb([128, 16 * NT], BF16); b_hT = S.buf()
        xt = [C.sb([128, D]) for _ in range(2)]; b_x = S.bufs(2)
        junk = C.sb([128, D], BF16); b_junk = S.buf()
        hb = C.sb([128, D], BF16); b_hb = S.buf()
        st = C.sb([128, 16]); b_st = S.buf()
        wch = [C.sb([128, 16 * 512], BF16) for _ in range(2)]; b_w = S.bufs(2)
        zn = C.sb([128, 512]); b_zn = S.buf()
        ra = C.sb([128, 256]); rb = C.sb([128, 256]); b_r = S.buf()
        ot = [C.sb([128, 512], BF16) for _ in range(3)]; b_o = S.bufs(3)
        b_Z = S.buf("Z")

        def ps_bf(i):
            return ps[i][:].bitcast(BF16)

        hT3 = hT[:].rearrange("p (k t) -> p k t", k=16)
        for i in range(NTILE):
            p = i % 2
            C.load(xt[p][:], x[i * 128:(i + 1) * 128, :], [b_x[p]])
            C.memset(st[:, 0:1], 0.0, [b_st])
            C.act(junk[:], xt[p][:], AF.Square, [b_x[p]], [b_junk, b_st], accum_out=st[:, 0:1])
            C.rstd(st[:, 1:2], st[:, 0:1], 1.0 / D, [b_st], [b_st])
            C.stt(tmpf[:], xt[p][:], st[:, 1:2], gmod[:], ALU.mult, ALU.mult, [b_x[p], b_st, b_g], [b_tmpf])
            C.tt(hb[:], tmpf[:], shiftb[:], ALU.add, [b_tmpf, b_s], [b_hb])
            for k in range(16):
                bk = k // 8
                C.tr(ps_bf(bk)[:, (k % 8) * 128:(k % 8 + 1) * 128], hb[:, k * 128:(k + 1) * 128], C.idb[:],
                     [b_hb, C.b_id], [psb[bk]], sig=(k % 8 == 7))
            for bk in range(2):
                C.act(hT3[:, bk * 8:(bk + 1) * 8, i * 128:(i + 1) * 128],
                      ps_bf(bk).rearrange("p (k t) -> p k t", k=8), AF.Copy, [psb[bk]], [b_hT])
        wv = w_in.rearrange("(k p) n -> p k n", p=128)
        no = 0
        for c in range(nchunks):
            blk = c // 4
            g, r = blk // 3, blk % 3
            w = wch[c % 2]
            if c == 0:
                C.load(w[:].rearrange("p (k n) -> p k n", k=16), wv[:, :, 0:512], [b_w[0]], q=S.pool)
            if c + 1 < nchunks:
                C.load(wch[(c + 1) % 2][:].rearrange("p (k n) -> p k n", k=16), wv[:, :, (c + 1) * 512:(c + 2) * 512],
                       [b_w[(c + 1) % 2]], q=S.pool)
            for i in range(NTILE):
                bk = 2 + (c * NTILE + i) % 6
                for k in range(16):
                    C.mm(ps[bk][:], hT[:, k * NT + i * 128:k * NT + (i + 1) * 128], w[:, k * 512:(k + 1) * 512],
                         k == 0, k == 15, [b_hT, b_w[c % 2]], [psb[bk]])
                oi = no % 3
                no += 1
                o_ = ot[oi]
                if r == 2:
                    C.act(o_[:], ps[bk][:], AF.Copy, [psb[bk]], [b_o[oi]])
                else:
                    gain = gqk[:, (r * 3 + g) * 128:(r * 3 + g + 1) * 128]
                    C.act(junk[:, 0:512], ps[bk][:], AF.Square, [psb[bk]], [b_junk])
                    C.red(st[:, 4:8], junk[:, 0:512].rearrange("p (h d) -> p h d", h=4), [b_junk], [b_st])
                    C.rstd(st[:, 4:8], st[:, 4:8], 1.0 / 128, [b_st], [b_st])
                    z3 = zn[:].rearrange("p (h d) -> p h d", h=4)
                    C.tt(z3, ps[bk][:].rearrange("p (h d) -> p h d", h=4), V(st[:, 4:5], (1, 4), (0, 128)), ALU.mult,
                         [psb[bk], b_st], [b_zn])
                    C.tt(z3, z3, V(gain, (0, 4), (1, 128)), ALU.mult, [b_zn, b_gs], [b_zn])
                    o3 = o_[:].rearrange("p (h d) -> p h d", h=4)
                    cosv = V(cos[:, i * 64:i * 64 + 1], (0, 4), (1, 64)); sinv = V(sin[:, i * 64:i * 64 + 1], (0, 4), (1, 64))
                    rope_apply(C, o3[:, :, 0:64], o3[:, :, 64:128], z3[:, :, 0:64], z3[:, :, 64:128], cosv, sinv,
                               V(ra[:], (64, 4), (1, 64)), V(rb[:], (64, 4), (1, 64)), [b_zn, b_cs], [b_o[oi]], b_r)
                C.store(Z[i * 128:(i + 1) * 128, c * 512:(c + 1) * 512], o_[:], [b_o[oi]], [b_Z], q=S.sp)
        S.finish([b_Z])
    return C


DIL = (1, 4, 16)


def build_dil_attn(ngh=48):
    C = Ctx()
    nc = C.nc
    QT = C.din("QT", [48, 128, NT], BF16); KT = C.din("KT", [48, 128, 4096], BF16); Vb = C.din("Vb", [48, 128, 4096], BF16)
    halo_d = C.din("halo", [128, 1]); tri_d = C.din("tri2", [128, 256])
    x = C.din("x", [NT, D]); gate_d = C.din("gate", [1, D]); w_o = C.din("w_o", [D, D])
    ident = C.din("ident", [128, 128])
    x1 = C.dout("x1", [NT, D])
    SCALE = float(128 ** -0.5)
    with C.es:
        C.start()
        S = C.S
        ps, psb = C.ps, C.psb
        b_w = S.buf("w")
        w_o_sb = load_w_bf16(C, w_o, D, D, b_w)
        trif = C.sb([128, 256]); tri = C.sb([128, 256], BF16); trih = C.sb([128, 256], BF16); halo = C.sb([128, 1])
        ones = C.sb([128, 128], BF16); gate = C.sb([128, D])
        b_c = S.buf("consts")
        C.load(trif[:], tri_d[:, :], [b_c]); C.load(halo[:], halo_d[:, :], [b_c]); C.load(gate[:], pbc(gate_d, D), [b_c])
        C.cp(tri[:], trif[:], [b_c], [b_c])
        C.cp(trih[:], trif[:], [b_c], [b_c])
        C.ts(trih[:, 0:128], trif[:, 0:128], halo[:, 0:1], ALU.mult, [b_c], [b_c])
        C.memset(ones[:], 1.0, [b_c])
        qt = [C.sb([128, NT], BF16) for _ in range(2)]; kt = [C.sb([128, 4096], BF16) for _ in range(2)]
        vb = [C.sb([128, 4096], BF16) for _ in range(2)]; b_in = S.bufs(2)
        pt = [C.sb([128, 256], BF16) for _ in range(3)]; b_pt = S.bufs(3)
        accO = C.sb([128, NT]); accD = C.sb([128, NT]); b_acc = S.buf()
        otall = C.sb([128, 16 * NT], BF16); b_ot = S.buf()
        xt = [accD] * 2; b_x = [b_acc] * 2
        yt = accO; b_y = b_acc
        b_out = S.buf("out")
        ghs = [(h, g) for h in range(16) for g in range(3) if g * 16 + h < ngh]

        def load_gh(i):
            if i >= len(ghs):
                return
            h, g = ghs[i]
            gh = g * 16 + h
            d = DIL[g]
            ip = i % 2
            C.load(qt[ip][:], QT[gh, :, :], [b_in[ip]])
            C.load(kt[ip][:, 0:NT + 128 * d], KT[gh, :, 0:NT + 128 * d], [b_in[ip]])
            C.load(vb[ip][:, 0:NT + 128 * d], Vb[gh, :, 0:NT + 128 * d], [b_in[ip]])

        tiles = []
        for i, (h, g) in enumerate(ghs):
            for tile in range(16):
                tiles.append(dict(i=i, h=h, g=g, tile=tile, n=len(tiles)))

        def emit_s(t):
            ip = t["i"] % 2
            d = DIL[t["g"]]; nb = 16 // d
            tile = t["tile"]
            r, n = tile // nb, tile % nb
            kb_prev = r * (nb + 1) + n
            kb_cur = kb_prev + 1
            bs = t["n"] % 2
            qv = qt[ip][:, tile * 128:(tile + 1) * 128]
            C.mm(ps[bs][:, 0:128], kt[ip][:, kb_prev * 128:(kb_prev + 1) * 128], qv, True, True,
                 [b_in[ip]], [psb[bs]], sig=False)
            C.mm(ps[bs][:, 128:256], kt[ip][:, kb_cur * 128:(kb_cur + 1) * 128], qv, True, True,
                 [b_in[ip]], [psb[bs]])

        def emit_rest(t):
            ip = t["i"] % 2
            h, g = t["h"], t["g"]
            d = DIL[g]; nb = 16 // d
            tile = t["tile"]
            r, n = tile // nb, tile % nb
            kb_prev = r * (nb + 1) + n
            kb_cur = kb_prev + 1
            bs = t["n"] % 2
            pi = t["n"] % 3
            t4, tq = tile // 4, tile % 4
            nbank = t["n"] // 4
            bo = 2 + (nbank % 2)
            bd = 4 + (nbank % 2)
            C.act(pt[pi][:], ps[bs][:, 0:256], AF.Exp, [psb[bs]], [b_pt[pi]], scale=SCALE)
            C.tt(pt[pi][:], pt[pi][:], (trih if n == 0 else tri)[:], ALU.mult, [b_pt[pi], b_c], [b_pt[pi]])
            oc = slice(tq * 128, (tq + 1) * 128)
            C.mm(ps[bo][:, oc], vb[ip][:, kb_prev * 128:(kb_prev + 1) * 128], pt[pi][:, 0:128], True, False,
                 [b_in[ip], b_pt[pi]], [psb[bo]])
            C.mm(ps[bo][:, oc], vb[ip][:, kb_cur * 128:(kb_cur + 1) * 128], pt[pi][:, 128:256], False, True,
                 [b_in[ip], b_pt[pi]], [psb[bo]], sig=False)
            C.mm(ps[bd][:, oc], ones[:], pt[pi][:, 0:128], True, False, [b_c, b_pt[pi]], [psb[bd]])
            C.mm(ps[bd][:, oc], ones[:], pt[pi][:, 128:256], False, True, [b_c, b_pt[pi]], [psb[bd]], sig=True)
            if tq != 3:
                return
            if d == 1:
                dO = accO[:, t4 * 512:(t4 + 1) * 512]; dD = accD[:, t4 * 512:(t4 + 1) * 512]
                sO = ps[bo][:]; sD = ps[bd][:]
            elif d == 4:
                r0 = t4
                dO = V(accO[:, r0:r0 + 1], (4, 512)); dD = V(accD[:, r0:r0 + 1], (4, 512))
                sO = ps[bo][:]; sD = ps[bd][:]
            else:
                r0 = t4 * 4
                dO = V(accO[:, r0:r0 + 1], (1, 4), (16, 128)); dD = V(accD[:, r0:r0 + 1], (1, 4), (16, 128))
                sO = ps[bo][:].rearrange("p (r i) -> p r i", r=4); sD = ps[bd][:].rearrange("p (r i) -> p r i", r=4)
            if g == 0:
                C.cp(dO, sO, [psb[bo]], [b_acc])
                C.act(dD, sD, AF.Copy, [psb[bd]], [b_acc])
            else:
                C.tt(dO, dO, sO, ALU.add, [psb[bo], b_acc], [b_acc])
                C.tt(dD, dD, sD, ALU.add, [psb[bd], b_acc], [b_acc])
            if tile == 15 and (g == 2 or t["i"] == len(ghs) - 1):
                C.recip(accD[:], accD[:], [b_acc], [b_acc])
                C.tt(otall[:, h * NT:(h + 1) * NT], accO[:], accD[:], ALU.mult, [b_acc], [b_ot])

        load_gh(0)
        for i in range(len(tiles) + 1):
            if i < len(tiles):
                emit_s(tiles[i])
            if i >= 1:
                emit_rest(tiles[i - 1])
            if i < len(tiles) and tiles[i]["tile"] == 0:
                load_gh(tiles[i]["i"] + 1)
        for ti in range(NTILE):
            xp = ti % 2
            C.load(xt[xp][:], x[ti * 128:(ti + 1) * 128, :], [b_x[xp]])
            for nn in range(4):
                bk = 6 + nn % 2
                n0 = nn * 512
                for hh in range(16):
                    C.mm(ps[bk][:], otall[:, hh * NT + ti * 128:hh * NT + (ti + 1) * 128],
                         w_o_sb[:, hh * 2048 + n0:hh * 2048 + n0 + 512], hh == 0, hh == 15, [b_ot, b_w], [psb[bk]])
                C.tt(yt[:, n0:n0 + 512], ps[bk][:], gate[:, n0:n0 + 512], ALU.mult, [psb[bk], b_c], [b_y])
            C.tt(yt[:], yt[:], xt[xp][:], ALU.add, [b_y, b_x[xp]], [b_y])
            C.store(x1[ti * 128:(ti + 1) * 128, :], yt[:], [b_y], [b_out])
        S.finish([b_out])
    return C


def tri2_mask():
    a = np.arange(128)[:, None]
    qi = np.arange(128)[None, :]
    return np.concatenate([(a >= qi), (a <= qi)], axis=1).astype(np.float32)


def dil_exchange(p4res):
    maps = []
    for core in range(8):
        b, j = core // 4, core % 4
        Zc = np.asarray(p4res[core]["Z"]).reshape(NT, 3, 3, 16, 128)
        if j > 0:
            Zp = np.asarray(p4res[core - 1]["Z"]).reshape(NT, 3, 3, 16, 128)
        else:
            Zp = np.zeros_like(Zc)
        dt = Zc.dtype
        QT = np.zeros((48, 128, NT), dt); KT = np.zeros((48, 128, 4096), dt); Vb = np.zeros((48, 128, 4096), dt)
        for g, d in enumerate(DIL):
            nb = 16 // d
            def sub(Zx, which):
                a = Zx[:, g, which]
                return a.reshape(NT // d, d, 16, 128).transpose(1, 0, 2, 3).reshape(d, nb, 128, 16, 128)
            q = sub(Zc, 0); k = sub(Zc, 1); v = sub(Zc, 2)
            kp = sub(Zp, 1)[:, nb - 1:nb]; vp = sub(Zp, 2)[:, nb - 1:nb]
            kk = np.concatenate([kp, k], axis=1)
            vv = np.concatenate([vp, v], axis=1)
            QT[g * 16:(g + 1) * 16] = q.transpose(3, 4, 0, 1, 2).reshape(16, 128, NT)
            KT[g * 16:(g + 1) * 16, :, :NT + 128 * d] = kk.transpose(3, 4, 0, 1, 2).reshape(16, 128, NT + 128 * d)
            Vb[g * 16:(g + 1) * 16, :, :NT + 128 * d] = vv.transpose(3, 2, 0, 1, 4).reshape(16, 128, NT + 128 * d)
        halo = np.full((128, 1), 1.0 if j > 0 else 0.0, np.float32)
        maps.append(dict(QT=QT, KT=KT, Vb=Vb, halo=halo))
    return maps


def _peer_layer(inputs, layer, x_cores, mod):
    C = build_peer()
    pin = peer_inputs(inputs, layer)
    maps = []
    for core in range(8):
        b = core // 4
        m = mod[layer, b]
        d = dict(pin)
        d.update(x=x_cores[core], shift=row(m[6144:8192]), scale=row(m[8192:10240]), gate=row(m[10240:12288]),
                 g=row(inputs["norm_g"][layer, 1]), ident=IDENT)
        maps.append(d)
    res = run(C, maps)
    return [np.asarray(res[c]["x2"]) for c in range(8)]


def kernel(**inputs):
    inputs = {k: np.asarray(v) for k, v in inputs.items()}
    x = inputs["x"].astype(np.float32, copy=False)
    positions = inputs["positions"]
    mod = run_mod(inputs)
    x_cores = []
    for core in range(8):
        b, sl = core_tokens(core)
        x_cores.append(np.ascontiguousarray(x[b, sl]))
    f32 = lambda a: np.ascontiguousarray(np.asarray(a, np.float32))

    C = build_mla_pre()
    maps = []
    for core in range(8):
        b = core // 4
        m = mod[0, b]
        maps.append(dict(x=x_cores[core], pos=pos_layout(positions, core),
                         shift=row(m[0:2048]), scale=row(m[2048:4096]), g=row(inputs["norm_g"][0, 0]),
                         w_in=f32(inputs["mla_w_in"][0]), g_q=row(inputs["mla_g_q"][0]), w_uq=f32(inputs["mla_w_uq"][0]),
                         g_kv=row(inputs["mla_g_kv"][0]), w_ukv=f32(inputs["mla_w_ukv"][0]),
                         g_qn=row(inputs["mla_g_qn"][0]), g_kn=row(inputs["mla_g_kn"][0]),
                         invf=inv_freq(32), ident=IDENT))
    p1 = run(C, maps)
    ex = mla_exchange(p1)
    C = build_mla_attn()
    maps = []
    for core in range(8):
        b = core // 4
        m = mod[0, b]
        d = dict(ex[core])
        d.update(QT=np.asarray(p1[core]["QT"]), tri=tri_mask(), x=x_cores[core], gate=row(m[4096:6144]),
                 w_o=f32(inputs["mla_w_o"][0]), ident=IDENT)
        maps.append(d)
    res = run(C, maps)
    del p1, ex, maps
    x_cores = [np.asarray(res[c]["x1"]) for c in range(8)]
    x_cores = _peer_layer(inputs, 0, x_cores, mod)

    gqk = np.concatenate([np.asarray(inputs["dil_g_qn"][0], np.float32).reshape(-1),
                          np.asarray(inputs["dil_g_kn"][0], np.float32).reshape(-1)]).reshape(1, 768)
    C = build_dil_pre()
    maps = []
    for core in range(8):
        b = core // 4
        m = mod[1, b]
        maps.append(dict(x=x_cores[core], pos=pos_layout(positions, core),
                         shift=row(m[0:2048]), scale=row(m[2048:4096]), g=row(inputs["norm_g"][1, 0]),
                         w_in=f32(inputs["dil_w_in"][0]), gqk=gqk, invf=inv_freq(64), ident=IDENT))
    p4 = run(C, maps)
    ex = dil_exchange(p4)
    del p4
    C = build_dil_attn()
    maps = []
    for core in range(8):
        b = core // 4
        m = mod[1, b]
        d = dict(ex[core])
        d.update(tri2=tri2_mask(), x=x_cores[core], gate=row(m[4096:6144]), w_o=f32(inputs["dil_w_o"][0]), ident=IDENT)
        maps.append(d)
    res = run(C, maps)
    del ex, maps
    x_cores = [np.asarray(res[c]["x1"]) for c in range(8)]
    x_cores = _peer_layer(inputs, 1, x_cores, mod)

    out = np.zeros((2, SEQ, D), np.float32)
    for core in range(8):
        b, sl = core_tokens(core)
        out[b, sl] = x_cores[core]
    return out
```
